# Optimizing a Trainium2 kernel written in Bass

```python
import functools
import jax, jax.numpy as jnp
from jax import lax
import numpy as np

D_MODEL = 2048
BATCH = 1
SEQ = 8192
DEPTH = 1

N_META = 16
HEAD_DIM = 128
Q_BLOCK = 128
ROPE_THETA = 10000.0
EPS = 1e-6
NEG = -1e30
A_HEADS = 8
A_WIDTH = A_HEADS * HEAD_DIM
A_KV_RANK = 512
IDX_HEADS = 16
IDX_DIM = 64
TOPK_MAX = 256
B_HEADS = 8
B_WIDTH = B_HEADS * HEAD_DIM
N_GROUPS = 4
EXPERTS_PER_GROUP = 4
N_EXPERTS = N_GROUPS * EXPERTS_PER_GROUP
TOP_K_IN_GROUP = 2
D_FF_EXPERT = 512
IN_SPLITS = (A_WIDTH, A_KV_RANK, IDX_HEADS * IDX_DIM, IDX_DIM, IDX_HEADS,
             B_WIDTH, B_WIDTH, B_WIDTH, B_HEADS, D_MODEL, D_MODEL)
IN_WIDTH = (A_WIDTH + A_KV_RANK + IDX_HEADS * IDX_DIM + IDX_DIM + IDX_HEADS
            + 3 * B_WIDTH + B_HEADS + 2 * D_MODEL)

kernel_name = 'hybrid_dsa_fox_hier_moe'


def rms_norm(x, g):
    xf = x.astype(jnp.float32)
    y = xf * lax.rsqrt(jnp.mean(xf * xf, axis=-1, keepdims=True) + EPS)
    return (y * g.astype(jnp.float32)).astype(x.dtype)


def rope(x, pos):
    half = x.shape[-1] // 2
    inv_freq = ROPE_THETA ** (-jnp.arange(half, dtype=jnp.float32) / half)
    ang = pos.astype(jnp.float32)[:, None] * inv_freq[None, :]
    cos = jnp.cos(ang)[:, None, :]
    sin = jnp.sin(ang)[:, None, :]
    xf = x.astype(jnp.float32)
    x1, x2 = xf[..., :half], xf[..., half:]
    return jnp.concatenate([x1 * cos - x2 * sin, x2 * cos + x1 * sin], axis=-1).astype(x.dtype)


def _dsa_block(q, qi, wi, pos, k, v, ki, topk):
    L = k.shape[1]
    causal = jnp.arange(L)[None, :] <= pos[:, None]
    dots = jnp.einsum('bqhd,bsd->bqhs', qi, ki).astype(jnp.float32)
    score = jnp.einsum('bqh,bqhs->bqs', wi.astype(jnp.float32), jax.nn.relu(dots)) * (IDX_DIM ** -0.5)
    score = jnp.where(causal[None], score, NEG)
    _, sel = lax.top_k(score, topk)
    valid = sel <= pos[None, :, None]
    k_sel = jax.vmap(lambda kb, sb: kb[sb])(k, sel)
    v_sel = jax.vmap(lambda vb, sb: vb[sb])(v, sel)
    logits = jnp.einsum('bqhd,bqkhd->bhqk', q, k_sel).astype(jnp.float32) * (HEAD_DIM ** -0.5)
    logits = jnp.where(valid[:, None], logits, NEG)
    p = jax.nn.softmax(logits, axis=-1).astype(v.dtype)
    return jnp.einsum('bhqk,bqkhd->bqhd', p, v_sel)


def _fox_block(q, cq, pos, k, v, c):
    L = k.shape[1]
    causal = jnp.arange(L)[None, :] <= pos[:, None]
    logits = jnp.einsum('bqhd,bshd->bhqs', q, k).astype(jnp.float32) * (HEAD_DIM ** -0.5)
    decay = jnp.transpose(cq, (0, 2, 1))[:, :, :, None] - jnp.transpose(c, (0, 2, 1))[:, :, None, :]
    logits = jnp.where(causal[None, None], logits + decay, NEG)
    p = jax.nn.softmax(logits, axis=-1).astype(v.dtype)
    return jnp.einsum('bhqs,bshd->bqhd', p, v)


def _sweep(block_fn, q_args, kv_args):
    B, L = q_args[0].shape[:2]
    n_real = L - N_META
    nb = n_real // Q_BLOCK
    pos = jnp.arange(L)
    meta_out = block_fn(*[a[:, :N_META] for a in q_args], pos[:N_META], *kv_args)

    def to_blocks(a):
        a = a[:, N_META:]
        a = a.reshape((B, nb, Q_BLOCK) + a.shape[2:])
        return jnp.moveaxis(a, 1, 0)

    blocks = tuple(to_blocks(a) for a in q_args)
    pos_blocks = pos[N_META:].reshape(nb, Q_BLOCK)
    out = lax.map(lambda xs: block_fn(*xs[0], xs[1], *kv_args), (blocks, pos_blocks))
    out = jnp.moveaxis(out, 0, 1).reshape((B, n_real) + out.shape[3:])
    return jnp.concatenate([meta_out, out], axis=1)


def token_mixer(x, g_mix, w_in, g_kv, w_kv_up, g_idx_k, b_f, w_branch_a, w_branch_b, w_out):
    B, L, _ = x.shape
    pos = jnp.arange(L)
    h = rms_norm(x, g_mix)
    proj = h @ w_in
    split_idx = np.cumsum(IN_SPLITS)[:-1].tolist()
    qa, ckv, qi, ki, wi, qb, kb, vb, fb, ga, gb = jnp.split(proj, split_idx, axis=-1)

    qa = rope(qa.reshape(B, L, A_HEADS, HEAD_DIM), pos)
    kva = rms_norm(ckv, g_kv) @ w_kv_up
    ka, va = jnp.split(kva, 2, axis=-1)
    ka = rope(ka.reshape(B, L, A_HEADS, HEAD_DIM), pos)
    va = va.reshape(B, L, A_HEADS, HEAD_DIM)
    qi = rope(qi.reshape(B, L, IDX_HEADS, IDX_DIM), pos)
    ki = rope(rms_norm(ki, g_idx_k)[:, :, None, :], pos)[:, :, 0, :]
    wi = wi * (IDX_HEADS ** -0.5)
    topk = min(TOPK_MAX, L // 4)
    ya = _sweep(functools.partial(_dsa_block, topk=topk), (qa, qi, wi), (ka, va, ki))
    ya = ya.reshape(B, L, A_WIDTH)

    logf = jax.nn.log_sigmoid(fb.astype(jnp.float32) + b_f.astype(jnp.float32))
    c = jnp.cumsum(logf, axis=1)
    qb = qb.reshape(B, L, B_HEADS, HEAD_DIM)
    kb = kb.reshape(B, L, B_HEADS, HEAD_DIM)
    vb = vb.reshape(B, L, B_HEADS, HEAD_DIM)
    yb = _sweep(_fox_block, (qb, c), (kb, vb, c)).reshape(B, L, B_WIDTH)

    merged = jax.nn.sigmoid(ga) * (ya @ w_branch_a) + jax.nn.sigmoid(gb) * (yb @ w_branch_b)
    return merged @ w_out


def hier_moe(x, g_ffn, w_group, b_group, w_expert, b_expert, w_gate_e, w_up_e, w_down_e):
    B, L, D = x.shape
    t = rms_norm(x, g_ffn).reshape(B * L, D)
    T = t.shape[0]
    g_logits = (t @ w_group).astype(jnp.float32) + b_group.astype(jnp.float32)
    g_prob = jax.nn.softmax(g_logits, axis=-1)
    g_sel = jnp.argmax(g_logits, axis=-1)
    p_group = jnp.take_along_axis(g_prob, g_sel[:, None], axis=-1)
    e_logits = ((t @ w_expert).astype(jnp.float32) + b_expert.astype(jnp.float32)).reshape(T, N_GROUPS, EXPERTS_PER_GROUP)
    e_logits = jnp.take_along_axis(e_logits, g_sel[:, None, None], axis=1)[:, 0]
    e_prob = jax.nn.softmax(e_logits, axis=-1)
    top_p, top_i = lax.top_k(e_prob, TOP_K_IN_GROUP)
    top_p = top_p / jnp.sum(top_p, axis=-1, keepdims=True)
    weight = p_group * top_p
    expert_id = g_sel[:, None] * EXPERTS_PER_GROUP + top_i
    gates = jnp.sum(jax.nn.one_hot(expert_id, N_EXPERTS, dtype=jnp.float32) * weight[..., None], axis=1)
    hg = jnp.einsum('td,edf->etf', t, w_gate_e)
    hu = jnp.einsum('td,edf->etf', t, w_up_e)
    act = jax.nn.silu(hg) * hu * gates.T[:, :, None].astype(hu.dtype)
    y = jnp.einsum('etf,efd->td', act, w_down_e)
    return y.reshape(B, L, D)


def setup_inputs(seed: int = 0) -> dict:
    key = jax.random.key(seed)
    ks = jax.random.split(key, 24)
    f32 = jnp.float32

    def nrm(k, shape, scale):
        return jax.random.normal(k, shape, f32) * scale

    def gain(k, shape):
        return 1.0 + 0.05 * jax.random.normal(k, shape, f32)

    return {
        'x': nrm(ks[0], (BATCH, SEQ, D_MODEL), 1.0),
        'meta_tokens': nrm(ks[1], (N_META, D_MODEL), 1.0),
        'g_mix': gain(ks[2], (DEPTH, D_MODEL)),
        'w_in': nrm(ks[3], (DEPTH, D_MODEL, IN_WIDTH), D_MODEL ** -0.5),
        'g_kv': gain(ks[4], (DEPTH, A_KV_RANK)),
        'w_kv_up': nrm(ks[5], (DEPTH, A_KV_RANK, 2 * A_WIDTH), A_KV_RANK ** -0.5),
        'g_idx_k': gain(ks[6], (DEPTH, IDX_DIM)),
        'b_f': 3.0 + nrm(ks[7], (DEPTH, B_HEADS), 0.5),
        'w_branch_a': nrm(ks[8], (DEPTH, A_WIDTH, D_MODEL), A_WIDTH ** -0.5),
        'w_branch_b': nrm(ks[9], (DEPTH, B_WIDTH, D_MODEL), B_WIDTH ** -0.5),
        'w_out': nrm(ks[10], (DEPTH, D_MODEL, D_MODEL), D_MODEL ** -0.5),
        'g_ffn': gain(ks[11], (DEPTH, D_MODEL)),
        'w_group': nrm(ks[12], (DEPTH, D_MODEL, N_GROUPS), D_MODEL ** -0.5),
        'b_group': nrm(ks[13], (DEPTH, N_GROUPS), 0.01),
        'w_expert': nrm(ks[14], (DEPTH, D_MODEL, N_EXPERTS), D_MODEL ** -0.5),
        'b_expert': nrm(ks[15], (DEPTH, N_EXPERTS), 0.01),
        'w_gate_e': nrm(ks[16], (DEPTH, N_EXPERTS, D_MODEL, D_FF_EXPERT), D_MODEL ** -0.5),
        'w_up_e': nrm(ks[17], (DEPTH, N_EXPERTS, D_MODEL, D_FF_EXPERT), D_MODEL ** -0.5),
        'w_down_e': nrm(ks[18], (DEPTH, N_EXPERTS, D_FF_EXPERT, D_MODEL), D_FF_EXPERT ** -0.5),
        'g_final': gain(ks[19], (D_MODEL,)),
    }


def reference(x, meta_tokens, g_mix, w_in, g_kv, w_kv_up, g_idx_k, b_f, w_branch_a, w_branch_b,
              w_out, g_ffn, w_group, b_group, w_expert, b_expert, w_gate_e, w_up_e, w_down_e, g_final):
    B = x.shape[0]
    meta = jnp.broadcast_to(meta_tokens[None].astype(x.dtype), (B, N_META, x.shape[-1]))
    h = jnp.concatenate([meta, x], axis=1)
    for l in range(DEPTH):
        h = h + token_mixer(h, g_mix[l], w_in[l], g_kv[l], w_kv_up[l], g_idx_k[l], b_f[l],
                            w_branch_a[l], w_branch_b[l], w_out[l])
        h = h + hier_moe(h, g_ffn[l], w_group[l], b_group[l], w_expert[l], b_expert[l],
                         w_gate_e[l], w_up_e[l], w_down_e[l])
    h = rms_norm(h, g_final)
    return h[:, N_META:]
```

```python
import contextlib
import os
LVL = int(os.environ.get('LVL', '9'))
import numpy as np
import ml_dtypes
import concourse.bass as bass
import concourse.mybir as mybir
from concourse.bass_utils import run_bass_kernel_spmd

F32 = mybir.dt.float32
BF16 = mybir.dt.bfloat16
ALU = mybir.AluOpType
AF = mybir.ActivationFunctionType
AX = mybir.AxisListType

NCORES = 8
D = 2048
SEQ = 8192
NMETA = 16
L = SEQ + NMETA
NKB_ALL = 65
HD = 128
EPS = 1e-6
TOPK = 256
NBISECT = 15
C_QA, C_CKV, C_QI, C_KI, C_WI, C_QB, C_KB, C_VB, C_FB, C_GA, C_GB = (
    0, 1024, 1536, 2560, 2624, 2640, 3664, 4688, 5712, 5720, 7768)


class Buf:
    __slots__ = ("name", "writers", "readers", "excl")

    def __init__(self, name="", excl=False):
        self.name = name
        self.writers = []
        self.readers = []
        self.excl = excl


class _Eng:
    def __init__(self, name, sem):
        self.name = name
        self.sem = sem
        self.count = 0
        self.ops = []
        self.seen = {}
        self.dsems = []
        self.dcount = []
        self.dnext = 0


class Sched:
    ENG_NAMES = ("pe", "act", "dve", "pool", "sp")

    def __init__(self, nc, stack, n_dsem=24):
        self.nc = nc
        self.eng = {}
        self.clock = {}
        for n in self.ENG_NAMES:
            sem = stack.enter_context(nc.semaphore("s_" + n))
            self.eng[n] = _Eng(n, sem)
        for n in ("sp", "pool"):
            e = self.eng[n]
            for i in range(n_dsem):
                e.dsems.append(stack.enter_context(nc.semaphore("d_%s_%d" % (n, i))))
                e.dcount.append(0)
        self.semobj = {}
        for n in self.ENG_NAMES:
            self.semobj[("e", n)] = self.eng[n].sem
        for n in ("sp", "pool"):
            for i, s in enumerate(self.eng[n].dsems):
                self.semobj[("d", n, i)] = s
        self.nwaits = 0

    def _need(self, E, ev, out):
        key, val = ev
        if E.seen.get(key, 0) >= val:
            return
        if out.get(key, 0) < val:
            out[key] = val

    def _apply_waits(self, E, need):
        for key, val in need.items():
            if E.seen.get(key, 0) >= val:
                continue
            E.ops.append(("w", self.semobj[key], val))
            self.nwaits += 1
            ck = self.clock.get((key, val))
            if ck:
                for k2, v2 in ck.items():
                    if E.seen.get(k2, 0) < v2:
                        E.seen[k2] = v2
            if E.seen.get(key, 0) < val:
                E.seen[key] = val

    def _deps(self, E, reads, writes, same_ok):
        need = {}
        me = ("e", E.name)
        for b in reads:
            for ev in b.writers:
                if same_ok and ev[0] == me:
                    continue
                self._need(E, ev, need)
            if b.excl:
                for ev in b.readers:
                    if ev[0] != me:
                        self._need(E, ev, need)
        for b in writes:
            for ev in b.writers:
                if same_ok and ev[0] == me:
                    continue
                self._need(E, ev, need)
            for ev in b.readers:
                if same_ok and ev[0] == me:
                    continue
                self._need(E, ev, need)
        self._apply_waits(E, need)

    def _record(self, ev, reads, writes):
        for b in reads:
            b.readers.append(ev)
        for b in writes:
            b.writers = [ev]
            b.readers = []

    def op(self, eng, fn, reads=(), writes=()):
        E = self.eng[eng]
        self._deps(E, reads, writes, same_ok=(eng == "pe"))
        E.count += 1
        ev = (("e", eng), E.count)
        E.ops.append(("i", fn, E.sem, 1))
        ck = dict(E.seen)
        ck[("e", eng)] = E.count
        self.clock[ev] = ck
        self._record(ev, reads, writes)
        return ev

    def dma(self, eng, fn, reads=(), writes=(), fresh=False):
        E = self.eng[eng]
        self._deps(E, reads, () if fresh else writes, same_ok=False)
        i = E.dnext
        E.dnext = (E.dnext + 1) % len(E.dsems)
        key = ("d", eng, i)
        if E.dcount[i] > 0:
            need = {}
            self._need(E, (key, E.dcount[i]), need)
            self._apply_waits(E, need)
        E.dcount[i] += 16
        ev = (key, E.dcount[i])
        E.ops.append(("i", fn, E.dsems[i], 16))
        self.clock[ev] = dict(E.seen)
        if fresh:
            self._record(ev, reads, ())
            for b in writes:
                b.writers.append(ev)
        else:
            self._record(ev, reads, writes)
        return ev

    def barrier(self):
        for n in ("sp", "pool"):
            E = self.eng[n]
            need = {}
            for i in range(len(E.dsems)):
                if E.dcount[i] > 0:
                    self._need(E, (("d", n, i), E.dcount[i]), need)
            self._apply_waits(E, need)
        evs = []
        for n in self.ENG_NAMES:
            E = self.eng[n]
            if E.count > 0:
                need = {}
                self._need(E, (("e", n), E.count), need)
                self._apply_waits(E, need)
            evs.append(self.op(n, lambda e: e.nop(nofuse=True), (), ()))
        for n in self.ENG_NAMES:
            E = self.eng[n]
            need = {}
            for ev in evs:
                self._need(E, ev, need)
            self._apply_waits(E, need)

    def emit(self, block):
        def run(E, e):
            for o in E.ops:
                if o[0] == "w":
                    e.wait_ge(o[1], o[2])
                else:
                    o[1](e).then_inc(o[2], o[3])

        S = self

        @block.tensor
        def _(e):
            run(S.eng["pe"], e)

        @block.scalar
        def _(e):
            run(S.eng["act"], e)

        @block.vector
        def _(e):
            run(S.eng["dve"], e)

        @block.gpsimd
        def _(e):
            run(S.eng["pool"], e)

        @block.sync
        def _(e):
            run(S.eng["sp"], e)


class SBAlloc:
    def __init__(self, nc, nbytes=200 * 1024):
        self.arena = nc.alloc_sbuf_tensor("arena", [128, nbytes // 4], F32)
        self.off = 0
        self.limit = nbytes
        self.full = nbytes
        self.peak = 0

    def alloc(self, shape, dtype):
        esz = 2 if dtype == BF16 else 4
        n = 1
        for s in shape[1:]:
            n *= s
        fb = (n * esz + 63) // 64 * 64
        off = self.off
        assert off + fb <= self.limit, "SBUF overflow %d+%d" % (off, fb)
        self.off = off + fb
        self.peak = max(self.peak, self.off)
        v = self.arena[0:shape[0], off // 4:(off + fb) // 4]
        if dtype != F32:
            v = v.bitcast(dtype)
        v = v[:, 0:n]
        if len(shape) == 3:
            v = v.rearrange("p (a b) -> p a b", a=shape[1])
        elif len(shape) == 4:
            v = v.rearrange("p (a b c) -> p a b c", a=shape[1], b=shape[2])
        return v

    def alloc_top(self, shape, dtype):
        esz = 2 if dtype == BF16 else 4
        n = 1
        for s in shape[1:]:
            n *= s
        fb = (n * esz + 63) // 64 * 64
        self.limit -= fb
        assert self.off <= self.limit
        off = self.limit
        v = self.arena[0:shape[0], off // 4:(off + fb) // 4]
        if dtype != F32:
            v = v.bitcast(dtype)
        v = v[:, 0:n]
        if len(shape) == 3:
            v = v.rearrange("p (a b) -> p a b", a=shape[1])
        return v

    def mark(self):
        return self.off

    def release(self, m):
        self.off = m


def build_program(stop_after=None, debug=False, only=None, npairs=4, NEXP=16):
    def on(part):
        return only is None or part in only
    nc = bass.Bass("TRN2", target_bir_lowering=False)

    def din(name, shape, dt=F32):
        return nc.dram_tensor(name, list(shape), dt, kind="ExternalInput").ap()

    def dscr(name, shape, dt=BF16):
        if debug:
            return nc.dram_tensor(name, list(shape), dt, kind="ExternalOutput").ap()
        return nc.dram_tensor(name, list(shape), dt).ap()

    xall = din("xall", [L, D])
    xown = din("xown", [1024, D])
    w_in = din("w_in", [D, 9816])
    w_kvup = din("w_kvup", [512, 2048])
    w_bra = din("w_bra", [1024, D])
    w_brb = din("w_brb", [1024, D])
    w_out = din("w_out", [D, D])
    w_ge = din("w_ge", [D, 20])
    w_gate = din("w_gate", [16, D, 512])
    w_up = din("w_up", [16, D, 512])
    w_down = din("w_down", [16, 512, D])
    g_mix = din("g_mix", [1, D])
    g_ffn = din("g_ffn", [1, D])
    g_final = din("g_final", [1, D])
    smallc = din("smallc", [128, 64])
    cosA_all = din("cosA_all", [128, L])
    sinA_all = din("sinA_all", [128, L])
    cosI_all = din("cosI_all", [128, L])
    sinI_all = din("sinI_all", [128, L])
    cosA_own = din("cosA_own", [128, 1024])
    sinA_own = din("sinA_own", [128, 1024])
    cosI_own = din("cosI_own", [128, 1024])
    sinI_own = din("sinI_own", [128, 1024])
    maskT_d = din("maskT", [128, 16 * 256])
    negb_d = din("negb", [128, 2 * 2048])
    ohsel_d = din("ohsel", [128, 8 * 66])
    cmat = din("cmat", [128, 4 * 128])
    out_d = nc.dram_tensor("out", [1024, D], F32, kind="ExternalOutput").ap()

    KaT = dscr("KaT", [8, 128, L])
    KbT = dscr("KbT", [8, 128, L])
    KiT_d = dscr("KiT", [128, L])
    Va = dscr("Va", [L, 1024])
    Vb = dscr("Vb", [L, 1024])
    QaT = dscr("QaT", [8, 128, 1024])
    QbT = dscr("QbT", [8, 128, 1024])
    QiT = dscr("QiT", [8, 128, 1024])
    HnO = dscr("HnO", [128, 16, 1024])
    dbg = {}
    if debug:
        for nm, shp in (("d_hn", [128, 16, 1024]), ("d_L", [128, 8 * 66]), ("d_ya", [128, 8 * 1024]),
                        ("d_yb", [128, 8 * 1024]), ("d_h2", [1024, D]), ("d_thr", [8, 128]),
                        ("d_gates", [128, 8 * 16]), ("d_score", [128, 8208])):
            dbg[nm] = nc.dram_tensor(nm, shp, F32, kind="ExternalOutput").ap()

    with contextlib.ExitStack() as st:
        S = Sched(nc, st)
        A = SBAlloc(nc)
        banks = [nc.alloc_psum_tensor("psb%d" % i, [128, 512], F32) for i in range(8)]
        Bbank = [Buf("bank%d" % i, excl=True) for i in range(8)]
        rr = {"i": 0, "lo": 0, "hi": 8}

        def nextbank():
            i = rr["i"]
            rr["i"] = rr["lo"] + (i + 1 - rr["lo"]) % (rr["hi"] - rr["lo"])
            return banks[i], Bbank[i]

        def setbanks(lo, hi):
            rr["lo"], rr["hi"], rr["i"] = lo, hi, lo

        def mm(out, lhsT, rhs, start, stop, R, W):
            S.op("pe", lambda e: e.matmul(out, lhsT=lhsT, rhs=rhs, start=start, stop=stop), R, W)

        def tr(out, in_, ident, R, W):
            S.op("pe", lambda e: e.transpose(out=out, in_=in_, identity=ident), R, W)

        def act(out, in_, func, R, W, bias=None, scale=None, accum=None, eng="act"):
            kw = {}
            if bias is not None:
                kw["bias"] = bias
            if scale is not None:
                kw["scale"] = scale
            if accum is not None:
                kw["accum_out"] = accum
            S.op(eng, lambda e: e.activation(out=out, in_=in_, func=func, **kw), R, W)

        def ts(eng, out, in0, s1, s2, op0, op1, R, W, accum=None):
            kw = {}
            if op1 is not None:
                kw["op1"] = op1
            if accum is not None:
                kw["accum_out"] = accum
            S.op(eng, lambda e: e.tensor_scalar(out=out, in0=in0, scalar1=s1, scalar2=s2, op0=op0, **kw), R, W)

        def tt(eng, out, in0, in1, op, R, W):
            S.op(eng, lambda e: e.tensor_tensor(out=out, in0=in0, in1=in1, op=op), R, W)

        def stt(eng, out, in0, scalar, in1, op0, op1, R, W):
            S.op(eng, lambda e: e.scalar_tensor_tensor(out=out, in0=in0, scalar=scalar, in1=in1, op0=op0, op1=op1), R, W)

        def cp(eng, out, in_, R, W):
            if eng == "act":
                S.op("act", lambda e: e.copy(out=out, in_=in_), R, W)
            else:
                S.op(eng, lambda e: e.tensor_copy(out=out, in_=in_), R, W)

        def red(eng, out, in_, op, R, W):
            S.op(eng, lambda e: e.tensor_reduce(out=out, in_=in_, axis=AX.X, op=op), R, W)

        def recip(out, in_, R, W):
            S.op("dve", lambda e: e.reciprocal(out=out, in_=in_), R, W)

        def mset(eng, ap, val, W):
            S.op(eng, lambda e: e.memset(ap, val), (), W)

        def dma(eng, out, in_, R, W, fresh=False):
            S.dma(eng, lambda e: e.dma_start(out=out, in_=in_), R, W, fresh=fresh)

        def ring(n, shape, dtype, name="r"):
            return [(A.alloc(shape, dtype), Buf("%s%d" % (name, i))) for i in range(n)]

        wst = {"ring": None, "i": 0, "engs": ("pool",)}

        def wstage_alloc(n=3, engs=("pool",)):
            wst["ring"] = ring(n, [128, 2048], F32, "wstg")
            wst["i"] = 0
            wst["engs"] = engs

        def wload(dst, src, Bdst, cast_eng=None):
            shp = list(dst.shape)
            if len(shp) == 2:
                pieces = [(dst, src, shp[1], None)]
            else:
                a, b = shp[1], shp[2]
                step = max(1, 2048 // b)
                pieces = []
                for a0 in range(0, a, step):
                    a1 = min(a, a0 + step)
                    pieces.append((dst[:, a0:a1, :], src[:, a0:a1, :], (a1 - a0) * b, a1 - a0))
            for (d_, s_, n_, na) in pieces:
                stg, Bstg = wst["ring"][wst["i"] % len(wst["ring"])]
                wst["i"] += 1
                v = stg[:, 0:n_]
                if na is not None:
                    v = v.rearrange("p (a b) -> p a b", a=na)
                dma("sp", v, s_, (), (Bstg,))
                ce = cast_eng or wst["engs"][wst["i"] % len(wst["engs"])]
                cp(ce, d_, v, (Bstg,), (Bdst,))

        cm = A.alloc([128, 512], F32)
        Bcm = Buf("cm")
        dma("sp", cm, cmat[:, :], (), (Bcm,))
        ident_f = cm[:, 0:128]
        triU_f = cm[:, 128:256]
        ones_f = cm[:, 256:384]
        cmb = A.alloc([128, 512], BF16)
        Bcmb = Buf("cmb")
        cp("dve", cmb, cm, (Bcm,), (Bcmb,))
        ident_b = cmb[:, 0:128]
        ones_b = cmb[:, 256:384]
        blk64_b = cmb[:, 384:512]
        smc = A.alloc([128, 64], F32)
        Bsmc = Buf("smc")
        dma("sp", smc, smallc[:, :], (), (Bsmc,))
        Lall = A.alloc([128, 8, 66], F32)
        Tall = A.alloc([128, 8, 66], F32)
        BL = Buf("Lall")
        BT = Buf("Tall")
        mset("pool", Lall, 0.0, (BL,))
        mset("pool", Tall, 0.0, (BT,))
        wiabs = A.alloc([128, 8, 16], F32)
        wisgn = A.alloc([128, 8, 16], F32)
        Bwi = Buf("wi")
        persist_mark = A.mark()

        def rstd_from_ss(ss, n, R, W, tmp=None):
            ts("dve", ss, ss, 1.0 / n, EPS, ALU.mult, ALU.add, R, W)
            act(ss, ss, AF.Sqrt, W, W)
            recip(ss, ss, W, W)

        def norm_A(src_rows_ap, rows, gbc, Bg, xr, xnr, statr, it):
            xb, Bx = xr[it % len(xr)]
            xn, Bxn = xnr[it % len(xnr)]
            stt_, Bst = statr[it % len(statr)]
            dma("sp", xb[0:rows, :], src_rows_ap, (), (Bx,))
            mset("dve", stt_[0:rows, :], 0.0, (Bst,))
            act(xn[0:rows, :], xb[0:rows, :], AF.Square, (Bx, Bst), (Bst, Bxn), accum=stt_[0:rows, 0:1])
            rstd_from_ss(stt_[0:rows, 0:1], D, (Bst,), (Bst,))
            stt("dve", xn[0:rows, :], xb[0:rows, :], stt_[0:rows, 0:1], gbc[0:rows, :], ALU.mult, ALU.mult,
                (Bx, Bst, Bg), (Bxn,))
            return xn, Bxn

        def norm_B(xn, Bxn, rows, hnT, BhnT, col0):
            for half in range(2):
                pb, Bp = nextbank()
                pbb = pb[:, :].bitcast(BF16)
                for i in range(8):
                    c = half * 8 + i
                    tr(pbb[:, i * 128:i * 128 + rows], xn[0:rows, c * 128:(c + 1) * 128], ident_b[0:rows, 0:rows],
                       (Bxn, Bcmb), (Bp,))
                src = pbb.rearrange("p (a b) -> p a b", a=8)[:, :, 0:rows]
                cp("act", hnT[:, half * 8:half * 8 + 8, col0:col0 + rows], src, (Bp,), (BhnT,))

        def norm_block(src_rows_ap, rows, gbc, Bg, xr, xnr, statr, hnT, BhnT, col0, it):
            xn, Bxn = norm_A(src_rows_ap, rows, gbc, Bg, xr, xnr, statr, it)
            norm_B(xn, Bxn, rows, hnT, BhnT, col0)

        m0 = A.mark()
        gbc = A.alloc([128, 2048], F32)
        Bg = Buf("gmix")
        dma("sp", gbc, g_mix.partition_broadcast(128), (), (Bg,))
        Wckv = A.alloc([128, 16, 512], BF16)
        Wki = A.alloc([128, 16, 128], BF16)
        Wkis = A.alloc([128, 16, 128], BF16)
        Wkb = A.alloc([128, 16, 1024], BF16)
        Wvb = A.alloc([128, 16, 1024], BF16)
        Wfb = A.alloc([128, 16, 8], BF16)
        Wkk = A.alloc([128, 4, 1024], BF16)
        Wkks = A.alloc([128, 4, 1024], BF16)
        Wkv = A.alloc([128, 4, 1024], BF16)
        BW = Buf("W0")

        def wv(c0, n):
            return w_in[:, c0:c0 + n].rearrange("(c p) n -> p c n", p=128)

        def wv1(c0, n, c):
            return w_in[c * 128:(c + 1) * 128, c0:c0 + n]

        BW2 = Buf("W0b")
        BW3 = Buf("W0c")
        BW4 = Buf("W0d")
        BW5 = Buf("W0e")
        BW6 = Buf("W0f")
        for c in range(16):
            dma("pool", Wckv[:, c, :], wv1(C_CKV, 512, c), (), (BW,), fresh=True)
            for d0 in (0, 64):
                dma("pool", Wki[:, c, d0:d0 + 64], wv1(C_KI, 64, c), (), (BW2,), fresh=True)
                dma("pool", Wkis[:, c, d0:d0 + 32], wv1(C_KI + 32, 32, c), (), (BW2,), fresh=True)
                dma("pool", Wkis[:, c, d0 + 32:d0 + 64], wv1(C_KI, 32, c), (), (BW2,), fresh=True)
            dma("pool", Wkb[:, c, :], wv1(C_KB, 1024, c), (), (BW3,), fresh=True)
            dma("pool", Wvb[:, c, :], wv1(C_VB, 1024, c), (), (BW4,), fresh=True)
            dma("pool", Wfb[:, c, :], wv1(C_FB, 8, c), (), (BW5,), fresh=True)
        for c in range(4):
            kvr = w_kvup[c * 128:(c + 1) * 128, :]
            dma("pool", Wkk[:, c, :], kvr[:, 0:1024], (), (BW6,), fresh=True)
            dma("pool", Wkv[:, c, :], kvr[:, 1024:2048], (), (BW6,), fresh=True)
            src = kvr[:, 0:1024].rearrange("p (h t j) -> p h t j", h=8, t=2)
            dst = Wkks[:, c, :].rearrange("p (h t j) -> p h t j", h=8, t=2)
            dma("pool", dst[:, :, 0, :], src[:, :, 1, :], (), (BW6,), fresh=True)
            dma("pool", dst[:, :, 1, :], src[:, :, 0, :], (), (BW6,), fresh=True)
        if stop_after == "w0":
            S.barrier()
            return _finish(nc, S, out_d)
        Wall = (BW, BW2, BW3, BW4, BW5, BW6)

        xr = ring(1, [128, 2048], F32, "x")
        xnr = ring(2, [128, 2048], BF16, "xn")
        statr = ring(4, [128, 8], F32, "st")
        hnr = ring(2, [128, 16, 256], BF16, "hn")
        tabr = ring(1, [128, 4, 256], F32, "tab")
        sqr = ring(1, [128, 4, 256], BF16, "sq")
        ckvTr = ring(1, [128, 4, 256], F32, "ckvT")
        rstdr = ring(2, [128, 256], F32, "rstd")
        ckvnr = ring(2, [128, 4, 256], BF16, "ckvn")
        t1r = ring(2, [128, 256], F32, "t1")
        t2r = ring(2, [128, 256], F32, "t2")
        kst = ring(4, [128, 256], BF16, "kst")
        vst = ring(3, [128, 1024], BF16, "vst")
        ksqr = ring(1, [128, 256], BF16, "ksq")
        kiAr = ring(1, [128, 256], F32, "kiA")
        kiBr = ring(1, [128, 256], F32, "kiB")
        lr = ring(2, [128, 8], F32, "l")
        zr = ring(2, [128, 8], F32, "z")

        cnt = {"blk": 0, "k": 0, "v": 0, "t": 0}
        groups = [(0, 16)] + [(16 + 256 * g, 256) for g in range(32)]
        groups = groups[:1 + 8 * npairs]
        if stop_after == "p0small":
            groups = groups[1:3] if only and "nometa" in only else groups[:3]
        pendB = []

        def do_norm_A(gi_):
            p0_, T_ = groups[gi_]
            hnT_, BhnT_ = hnr[gi_ % 2]
            for j_ in range(1 if T_ == 16 else 2):
                rows_ = 16 if T_ == 16 else 128
                xn_, Bxn_ = norm_A(xall[p0_ + 128 * j_:p0_ + 128 * j_ + rows_, :], rows_, gbc, Bg, xr, xnr, statr,
                                   cnt["blk"])
                cnt["blk"] += 1
                pendB.append((xn_, Bxn_, rows_, hnT_, BhnT_, 128 * j_))

        def do_norm_B():
            while pendB:
                norm_B(*pendB.pop(0))

        deferred = []
        do_norm_A(0)
        do_norm_B()
        for gi, (p0, T) in enumerate(groups):
            hnT, BhnT = hnr[gi % 2]
            tab, Btab = tabr[0]
            nblk = 1 if T == 16 else 2
            for ti, tsrc in enumerate((cosA_all, sinA_all, cosI_all, sinI_all)):
                dma("sp", tab[:, ti, 0:T], tsrc[:, p0:p0 + T], (), (Btab,))
            sq, Bsq = sqr[0]
            ckvT, BckvT = ckvTr[0]
            rs, Brs = rstdr[gi % 2]
            rs2, Brs2 = rstdr[(gi + 1) % 2]
            ckvn, Bckvn = ckvnr[gi % 2]
            for n in range(4):
                pb, Bp = nextbank()
                for c in range(16):
                    mm(pb[:, 0:T], Wckv[:, c, n * 128:(n + 1) * 128], hnT[:, c, 0:T], c == 0, c == 15,
                       (BW, BhnT), (Bp,))
                act(sq[:, n, 0:T], pb[:, 0:T], AF.Square, (Bp,), (Bsq,))
                cp("dve", ckvT[:, n, 0:T], pb[:, 0:T], (Bp,), (BckvT,))
            pki, Bpki = nextbank()
            pki2, Bpki2 = nextbank()
            for c in range(16):
                mm(pki[:, 0:T], Wki[:, c, :], hnT[:, c, 0:T], c == 0, c == 15, (BW2, BhnT), (Bpki,))
            for c in range(16):
                mm(pki2[:, 0:T], Wkis[:, c, :], hnT[:, c, 0:T], c == 0, c == 15, (BW2, BhnT), (Bpki2,))
            ksq, Bksq = ksqr[0]
            act(ksq[:, 0:T], pki[:, 0:T], AF.Square, (Bpki,), (Bksq,))
            kiA, BkiA = kiAr[0]
            kiB, BkiB = kiBr[0]
            ts("dve", kiA[:, 0:T], pki[:, 0:T], smc[:, 4:5], None, ALU.mult, None, (Bpki, Bsmc), (BkiA,))
            ts("dve", kiB[:, 0:T], pki2[:, 0:T], smc[:, 5:6], None, ALU.mult, None, (Bpki2, Bsmc), (BkiB,))
            for h in range(8):
                pb, Bp = nextbank()
                for c in range(16):
                    mm(pb[:, 0:T], Wkb[:, c, h * 128:(h + 1) * 128], hnT[:, c, 0:T], c == 0, c == 15,
                       (BW3, BhnT), (Bp,))
                ks, Bks = kst[cnt["k"] % 4]
                cnt["k"] += 1
                cp("act", ks[:, 0:T], pb[:, 0:T], (Bp,), (Bks,))
                dma("sp", KbT[h, :, p0:p0 + T], ks[:, 0:T], (Bks,), ())
                if h == 3:
                    pss, Bpss = nextbank()
                    for n in range(4):
                        mm(pss[:, 0:T], ones_b, sq[:, n, 0:T], n == 0, n == 3, (Bcmb, Bsq), (Bpss,))
                    mm(pss[:, 256:256 + T], blk64_b, ksq[:, 0:T], True, True, (Bcmb, Bksq), (Bpss,))
                    cp("dve", rs[:, 0:T], pss[:, 0:T], (Bpss,), (Brs,))
                    cp("dve", rs2[:, 0:T], pss[:, 256:256 + T], (Bpss,), (Brs2,))
                    rstd_from_ss(rs[:, 0:T], 512, (Brs,), (Brs,))
                    rstd_from_ss(rs2[:, 0:T], 64, (Brs2,), (Brs2,))
                    for n in range(4):
                        stt("dve", ckvn[:, n, 0:T], ckvT[:, n, 0:T], smc[:, n:n + 1], rs[:, 0:T], ALU.mult, ALU.mult,
                            (BckvT, Bsmc, Brs), (Bckvn,))
                    if gi + 1 < len(groups):
                        do_norm_A(gi + 1)
            for j in range(nblk):
                rows = 16 if T == 16 else 128
                b = 0 if T == 16 else 1 + 2 * (gi - 1) + j
                vs, Bvs = vst[cnt["v"] % 3]
                cnt["v"] += 1
                for half in range(2):
                    pb, Bp = nextbank()
                    for c in range(16):
                        mm(pb[0:rows, :], hnT[:, c, 128 * j:128 * j + rows], Wvb[:, c, half * 512:(half + 1) * 512],
                           c == 0, c == 15, (BW4, BhnT), (Bp,))
                    cp("act", vs[0:rows, half * 512:(half + 1) * 512], pb[0:rows, :], (Bp,), (Bvs,))
                dma("sp", Vb[p0 + 128 * j:p0 + 128 * j + rows, :], vs[0:rows, :], (Bvs,), ())
                pb, Bp = nextbank()
                for c in range(16):
                    mm(pb[0:rows, 0:8], hnT[:, c, 128 * j:128 * j + rows], Wfb[:, c, 0:8], c == 0, c == 15,
                       (BW5, BhnT), (Bp,))
                z, Bz = zr[b % 2]
                l, Bl = lr[b % 2]
                tt("dve", z[0:rows, :], pb[0:rows, 0:8], smc[0:rows, 8:16], ALU.add, (Bp, Bsmc), (Bz,))
                act(z[0:rows, :], z[0:rows, :], AF.Exp, (Bz,), (Bz,), scale=-1.0)
                act(l[0:rows, :], z[0:rows, :], AF.Ln, (Bz,), (Bl,), bias=1.0)
                def cums(rows=rows, l=l, Bl=Bl, b=b):
                    pb, Bp = nextbank()
                    mm(pb[0:rows, 0:8], triU_f[0:rows, 0:rows], l[0:rows, :], True, True, (Bcm, Bl), (Bp,))
                    mm(pb[:, 8:16], ones_f[0:rows, :], l[0:rows, :], True, True, (Bcm, Bl), (Bp,))
                    tt("dve", Lall[0:rows, :, b], pb[0:rows, 0:8], Tall[0:rows, :, b], ALU.add, (Bp, BT), (BL,))
                    tt("dve", Tall[:, :, b + 1], pb[:, 8:16], Tall[:, :, b], ALU.add, (Bp, BT), (BT,))

                deferred.append(cums)
            do_norm_B()
            tt("pool", kiA[:, 0:T], kiA[:, 0:T], rs2[:, 0:T], ALU.mult, (BkiA, Brs2), (BkiA,))
            tt("pool", kiB[:, 0:T], kiB[:, 0:T], rs2[:, 0:T], ALU.mult, (BkiB, Brs2), (BkiB,))
            tt("pool", kiA[:, 0:T], kiA[:, 0:T], tab[:, 2, 0:T], ALU.mult, (BkiA, Btab), (BkiA,))
            tt("pool", kiB[:, 0:T], kiB[:, 0:T], tab[:, 3, 0:T], ALU.mult, (BkiB, Btab), (BkiB,))
            ks, Bks = kst[cnt["k"] % 4]
            cnt["k"] += 1
            tt("pool", ks[:, 0:T], kiA[:, 0:T], kiB[:, 0:T], ALU.add, (BkiA, BkiB), (Bks,))
            dma("sp", KiT_d[:, p0:p0 + T], ks[:, 0:T], (Bks,), ())
            for h in range(8):
                pb, Bp = nextbank()
                pb2, Bp2 = nextbank()
                for c in range(4):
                    mm(pb[:, 0:T], Wkk[:, c, h * 128:(h + 1) * 128], ckvn[:, c, 0:T], c == 0, c == 3,
                       (BW6, Bckvn), (Bp,))
                for c in range(4):
                    mm(pb2[:, 0:T], Wkks[:, c, h * 128:(h + 1) * 128], ckvn[:, c, 0:T], c == 0, c == 3,
                       (BW6, Bckvn), (Bp2,))
                t1, Bt1 = t1r[cnt["t"] % 2]
                t2, Bt2 = t2r[cnt["t"] % 2]
                cnt["t"] += 1
                tt("dve", t1[:, 0:T], pb[:, 0:T], tab[:, 0, 0:T], ALU.mult, (Bp, Btab), (Bt1,))
                tt("dve", t2[:, 0:T], pb2[:, 0:T], tab[:, 1, 0:T], ALU.mult, (Bp2, Btab), (Bt2,))
                ks, Bks = kst[cnt["k"] % 4]
                cnt["k"] += 1
                tt("pool", ks[:, 0:T], t1[:, 0:T], t2[:, 0:T], ALU.add, (Bt1, Bt2), (Bks,))
                dma("sp", KaT[h, :, p0:p0 + T], ks[:, 0:T], (Bks,), ())
            for j in range(nblk):
                rows = 16 if T == 16 else 128
                vs, Bvs = vst[cnt["v"] % 3]
                cnt["v"] += 1
                for half in range(2):
                    pb, Bp = nextbank()
                    for c in range(4):
                        mm(pb[0:rows, :], ckvn[:, c, 128 * j:128 * j + rows], Wkv[:, c, half * 512:(half + 1) * 512],
                           c == 0, c == 3, (BW6, Bckvn), (Bp,))
                    cp("act", vs[0:rows, half * 512:(half + 1) * 512], pb[0:rows, :], (Bp,), (Bvs,))
                dma("sp", Va[p0 + 128 * j:p0 + 128 * j + rows, :], vs[0:rows, :], (Bvs,), ())
            while deferred:
                deferred.pop(0)()
        if debug:
            dma("sp", dbg["d_L"], Lall.rearrange("p a b -> p (a b)"), (BL,), ())
        S.barrier()
        A.release(m0)
        if stop_after in ("p0", "p0small"):
            return _finish(nc, S, out_d)
        NP_ = npairs
        NS = 2 * NP_
        NT = 128 * NS
        TG = min(512, NT)
        NTG = NT // TG

        def mkap(base, off_elems, dims):
            pstride = base.ap[0][0]
            return bass.AP(base.tensor, base.offset + off_elems, [(pstride, dims[0])] + [tuple(d) for d in dims[1:]])

        yaT = A.alloc([128, 8, NT], BF16)
        ByaT = Buf("yaT")
        mq = A.mark()
        gbc = A.alloc([128, 2048], F32)
        Bg = Buf("gmix2")
        dma("sp", gbc, g_mix.partition_broadcast(128), (), (Bg,))
        hnO = A.alloc([128, 16, NT], BF16)
        BhnO = Buf("hnO")
        xr = ring(2, [128, 2048], F32, "qx")
        xnr = ring(2, [128, 2048], BF16, "qxn")
        statr = ring(4, [128, 8], F32, "qst")
        for s_ in range(NS):
            norm_block(xown[128 * s_:128 * s_ + 128, :], 128, gbc, Bg, xr, xnr, statr, hnO, BhnO, 128 * s_, s_)
        dma("sp", HnO[:, :, 0:NT], hnO, (BhnO,), ())
        tabq = A.alloc([128, 4, NT], F32)
        Btabq = Buf("tabq")
        for ti, tsrc in enumerate((cosA_own, sinA_own, cosI_own, sinI_own)):
            dma("sp", tabq[:, ti, :], tsrc[:, 0:NT], (), (Btabq,), fresh=True)
        wstage_alloc(3, ("dve",))
        wr = ring(3, [128, 16, 128], BF16, "qw")
        wsr = ring(3, [128, 16, 128], BF16, "qws")
        qst = ring(3, [128, NT], BF16, "qstage")
        t1r = ring(2, [128, TG], F32, "qt1")
        t2r = ring(2, [128, TG], F32, "qt2")
        specs = [("qa", h, C_QA + 128 * h, QaT) for h in range(8)] + \
                [("qi", t, C_QI + 128 * t, QiT) for t in range(8)] + \
                [("qb", h, C_QB + 128 * h, QbT) for h in range(8)]
        def q_load(i):
            kind, idx, col0, dst = specs[i]
            W, BWt = wr[i % 3]
            Ws, BWs = wsr[i % 3]
            wsrc = w_in[:, col0:col0 + 128].rearrange("(c p) n -> p c n", p=128)
            wload(W, wsrc, BWt)
            if kind != "qb":
                hw = 64 if kind == "qa" else 32
                for j in range(128 // hw):
                    jo = j ^ 1
                    wload(Ws[:, :, j * hw:(j + 1) * hw], wsrc[:, :, jo * hw:(jo + 1) * hw], BWs)

        q_load(0)
        q_load(1)
        for i, (kind, idx, col0, dst) in enumerate(specs):
            W, BWt = wr[i % 3]
            Ws, BWs = wsr[i % 3]
            if i + 2 < len(specs):
                q_load(i + 2)
            st_, Bst_ = qst[i % 3]
            for tg in range(NTG):
                cols = slice(tg * TG, (tg + 1) * TG)
                pb, Bp = nextbank()
                for c in range(16):
                    mm(pb[:, 0:TG], W[:, c, :], hnO[:, c, cols], c == 0, c == 15, (BWt, BhnO), (Bp,))
                if kind == "qb":
                    act(st_[:, cols], pb[:, 0:TG], AF.Copy, (Bp,), (Bst_,), scale=float(128 ** -0.5))
                else:
                    pb2, Bp2 = nextbank()
                    for c in range(16):
                        mm(pb2[:, 0:TG], Ws[:, c, :], hnO[:, c, cols], c == 0, c == 15, (BWs, BhnO), (Bp2,))
                    ci = 0 if kind == "qa" else 2
                    t1, Bt1 = t1r[tg % 2]
                    t2, Bt2 = t2r[tg % 2]
                    tt("dve", t1, pb[:, 0:TG], tabq[:, ci, cols], ALU.mult, (Bp, Btabq), (Bt1,))
                    tt("dve", t2, pb2[:, 0:TG], tabq[:, ci + 1, cols], ALU.mult, (Bp2, Btabq), (Bt2,))
                    tt("pool", st_[:, cols], t1, t2, ALU.add, (Bt1, Bt2), (Bst_,))
            dma("sp", dst[idx, :, 0:NT], st_, (Bst_,), ())
        Wwi = A.alloc([128, 16, 16], BF16)
        BWwi = Buf("Wwi")
        dma("pool", Wwi, w_in[:, C_WI:C_WI + 16].rearrange("(c p) n -> p c n", p=128), (), (BWwi,))
        wtmp = A.alloc([128, 16], F32)
        Bwtmp = Buf("wtmp")
        for s_ in range(NS):
            pb, Bp = nextbank()
            for c in range(16):
                mm(pb[:, 0:16], hnO[:, c, 128 * s_:128 * s_ + 128], Wwi[:, c, :], c == 0, c == 15, (BWwi, BhnO), (Bp,))
            ts("dve", wtmp, pb[:, 0:16], 0.25 * 0.125, None, ALU.mult, None, (Bp,), (Bwtmp,))
            ts("dve", wisgn[:, s_, :], wtmp, 0.0, 2.0, ALU.is_ge, ALU.mult, (Bwtmp,), (Bwi,))
            ts("dve", wisgn[:, s_, :], wisgn[:, s_, :], -1.0, None, ALU.add, None, (Bwi,), (Bwi,))
            tt("dve", wiabs[:, s_, :], wtmp, wisgn[:, s_, :], ALU.mult, (Bwtmp, Bwi), (Bwi,))
        if debug:
            pass
        S.barrier()
        A.release(mq)
        if stop_after == "q":
            return _finish(nc, S, out_d)

        def kb_range(kb):
            return (0, 16) if kb == 0 else (16 + 128 * (kb - 1), 128)

        LOOK = 3
        pending_fin = []

        prefetched = {}

        def attention(m, h, mixer, Qp, BQp, KT_d, V_d, Kr, Vr, PTr, onr, rsr, yT, ByT, cnt,
                      selT=None, BselT=None, maskT_sb=None, Bmask=None, biasAB=None, Bbias=None, nxt=None):
            NKB = 16 * m + 17
            par = cnt["o"] % 2
            cnt["o"] += 1
            oA, BoA = banks[4 + 2 * par], Bbank[4 + 2 * par]
            oB, BoB = banks[5 + 2 * par], Bbank[5 + 2 * par]
            chunks = {}
            nch_tot = (NKB + 15) // 16

            def load_chunk(ch, mm_=m, hh_=h, NKB_=NKB):
                key = (mixer, mm_, hh_, ch)
                if key in prefetched:
                    chunks[ch] = prefetched.pop(key)
                    return
                NKBx = NKB_
                kb0 = 16 * ch
                kb1 = min(NKBx, kb0 + 16)
                klo = kb_range(kb0)[0]
                khi = kb_range(kb1 - 1)[0] + kb_range(kb1 - 1)[1]
                Kc, BKc = Kr[cnt["kv"] % len(Kr)]
                Vc, BVc = Vr[cnt["kv"] % len(Vr)]
                cnt["kv"] += 1
                dma("sp", Kc[:, 0:khi - klo], KT_d[hh_, :, klo:khi], (), (BKc,))
                j0 = 0
                if kb0 == 0:
                    dma("sp", Vc[0:16, 0, 0:128], V_d[0:16, hh_ * 128:(hh_ + 1) * 128], (), (BVc,))
                    j0 = 1
                nreal = (kb1 - kb0) - j0
                r0 = kb_range(kb0 + j0)[0]
                dma("sp", Vc[:, j0:j0 + nreal, 0:128],
                    V_d[r0:r0 + 128 * nreal, hh_ * 128:(hh_ + 1) * 128].rearrange("(j p) d -> p j d", p=128),
                    (), (BVc,), fresh=(j0 == 1))
                rec = (Kc, BKc, Vc, BVc, klo, kb0)
                if (mm_, hh_) == (m, h):
                    chunks[ch] = rec
                else:
                    prefetched[key] = rec

            pts = {}

            def emit_S(kb):
                ch = kb // 16
                if ch not in chunks:
                    load_chunk(ch)
                if kb % 16 == 0:
                    if ch + 1 < nch_tot:
                        if (ch + 1) not in chunks:
                            load_chunk(ch + 1)
                    elif nxt is not None:
                        load_chunk(0, nxt[0], nxt[1], 16 * nxt[0] + 17)
                Kc, BKc, Vc, BVc, klo, kb0 = chunks[ch]
                k0, ksz = kb_range(kb)
                pb, Bp = nextbank()
                mm(pb[0:ksz, 0:256], Kc[:, k0 - klo:k0 - klo + ksz], Qp[:, h, :], True, True, (BKc, BQp), (Bp,))
                PT, BPT = PTr[cnt["pt"] % len(PTr)]
                cnt["pt"] += 1
                if mixer == "a":
                    act(PT[0:ksz, :], pb[0:ksz, 0:256], AF.Exp, (Bp,), (BPT,))
                    tt("pool" if kb % 3 == 2 else "dve", PT[0:ksz, :], PT[0:ksz, :], selT[0:ksz, kb, :], ALU.mult,
                       (BPT, BselT), (BPT,))
                else:
                    for half in range(2):
                        act(PT[0:ksz, half * 128:(half + 1) * 128], pb[0:ksz, half * 128:(half + 1) * 128], AF.Exp,
                            (Bp, Bbias), (BPT,), bias=biasAB[half][0:ksz, kb:kb + 1])
                    t = kb - (NKB - 16)
                    if t >= 0:
                        tt("dve", PT[0:ksz, :], PT[0:ksz, :], maskT_sb[0:ksz, t, :], ALU.mult, (BPT, Bmask), (BPT,))
                pts[kb] = (PT, BPT)

            def emit_PV(kb):
                ch = kb // 16
                Kc, BKc, Vc, BVc, klo, kb0 = chunks[ch]
                k0, ksz = kb_range(kb)
                j = kb - kb0
                PT, BPT = pts.pop(kb)
                mm(oA[:, 0:129], PT[0:ksz, 0:128], Vc[0:ksz, j, :], kb == 0, kb == NKB - 1, (BPT, BVc), (BoA,))
                mm(oB[:, 0:129], PT[0:ksz, 128:256], Vc[0:ksz, j, :], kb == 0, kb == NKB - 1, (BPT, BVc), (BoB,))

            for kb in range(min(LOOK, NKB)):
                emit_S(kb)
            while pending_fin:
                pending_fin.pop(0)()
            for kb in range(NKB):
                emit_PV(kb)
                if kb + LOOK < NKB:
                    emit_S(kb + LOOK)

            def fin():
                for half, (o_, Bo_) in enumerate(((oA, BoA), (oB, BoB))):
                    s_ = 2 * m + half
                    rs_, Brs_ = rsr[cnt["fin"] % len(rsr)]
                    on_, Bon_ = onr[cnt["fin"] % len(onr)]
                    cnt["fin"] += 1
                    recip(rs_[:, 0:1], o_[:, 128:129], (Bo_,), (Brs_,))
                    ts("dve", on_, o_[:, 0:128], rs_[:, 0:1], None, ALU.mult, None, (Bo_, Brs_), (Bon_,))
                    pb, Bp = nextbank()
                    pbb = pb[:, :].bitcast(BF16)
                    tr(pbb[:, 0:128], on_, ident_b, (Bon_, Bcmb), (Bp,))
                    cp("act", yT[:, h, 128 * s_:128 * s_ + 128], pbb[:, 0:128], (Bp,), (ByT,))

            pending_fin.append(fin)

        def flush_fin():
            while pending_fin:
                pending_fin.pop(0)()

        def kv_rings():
            Kr = ring(3, [128, 2048], BF16, "Kc")
            Vr = ring(3, [128, 16, 129], BF16, "Vc")
            for Vc, BVc in Vr:
                mset("pool", Vc[:, :, 128:129], 1.0, (BVc,))
            PTr = ring(5, [128, 256], BF16, "PT")
            onr = ring(4, [128, 128], BF16, "on")
            rsr = ring(4, [128, 8], F32, "rs")
            return Kr, Vr, PTr, onr, rsr

        md = A.mark()
        setbanks(0, 4)
        KiT_sb = A.alloc([128, L], BF16)
        BKi = Buf("KiT_sb")
        NKTOT = 16 + 128 * 16 * NP_
        dma("sp", KiT_sb[:, 0:NKTOT], KiT_d[:, 0:NKTOT], (), (BKi,))
        maskT_sb = A.alloc([128, 16, 256], BF16)
        Bmask = Buf("maskT")
        dma("pool", maskT_sb, maskT_d.rearrange("p (t q) -> p t q", t=16), (), (Bmask,))
        negb_sb = A.alloc([128, 2, 2048], F32)
        Bnegb = Buf("negb")
        dma("sp", negb_sb, negb_d.rearrange("p (a k) -> p a k", a=2), (), (Bnegb,))
        score = A.alloc([128, L], F32)
        Bscore = Buf("score")
        sel = A.alloc([128, L], BF16)
        Bsel = Buf("sel")
        selT = A.alloc([128, 65, 256], BF16)
        BselT = Buf("selT")
        rr_ = ring(4, [128, 512], F32, "relu")
        Qi_p = A.alloc([128, 8, 256], BF16)
        BQi = Buf("Qi_p")
        Qa_p = A.alloc([128, 8, 256], BF16)
        BQa = Buf("Qa_p")
        bst = A.alloc([128, 16], F32)
        Bbst = Buf("bst")
        btab = A.alloc([128, 16], F32)
        bcnt_t = A.alloc([128, 16], F32)
        Bbtab = Buf("btab")
        cntp = A.alloc([128, 8], F32)[:, 0:1]
        Bcntp = Buf("cntp")
        Bsel2 = Buf("sel2")
        Kr, Vr, PTr, onr, rsr = kv_rings()
        acnt = {"o": 0, "kv": 0, "pt": 0, "fin": 0}
        for m in range(NP_):
            NKB = 16 * m + 17
            NK = 16 + 128 * (NKB - 1)
            flush_fin()
            dma("sp", Qi_p, QiT[:, :, 256 * m:256 * m + 256].rearrange("t p q -> p t q"), (), (BQi,))
            dma("sp", Qa_p, QaT[:, :, 256 * m:256 * m + 256].rearrange("h p q -> p h q"), (), (BQa,))
            for half in range(2):
                s_ = 2 * m + half
                nchunk = (NK + 511) // 512
                Bscs = [Buf("sc%d" % i) for i in range(nchunk)]
                for b_ in Bscs:
                    b_.writers = list(Bscore.writers)
                    b_.readers = list(Bscore.readers)
                for h in range(16):
                    po = (h % 2) * 64
                    for ch in range(nchunk):
                        k0 = ch * 512
                        kw = min(512, NK - k0)
                        pb, Bp = nextbank()
                        mm(pb[:, 0:kw], Qi_p[po:po + 64, h // 2, half * 128:(half + 1) * 128],
                           KiT_sb[po:po + 64, k0:k0 + kw], True, True, (BQi, BKi), (Bp,))
                        r_, Br_ = rr_[(h * nchunk + ch) % 4]
                        act(r_[:, 0:kw], pb[:, 0:kw], AF.Relu, (Bp, Bwi), (Br_,), scale=wiabs[:, s_, h:h + 1])
                        ae = "dve"
                        Bsc = Bscs[ch]
                        if h == 0:
                            ts(ae, score[:, k0:k0 + kw], r_[:, 0:kw], wisgn[:, s_, h:h + 1], None, ALU.mult, None,
                               (Br_, Bwi), (Bsc,))
                        else:
                            stt(ae, score[:, k0:k0 + kw], r_[:, 0:kw], wisgn[:, s_, h:h + 1], score[:, k0:k0 + kw],
                                ALU.mult, ALU.add, (Br_, Bwi, Bsc), (Bsc,))
                Bscore.writers = [ev for b_ in Bscs for ev in b_.writers]
                Bscore.readers = [ev for b_ in Bscs for ev in b_.readers]
                if debug and m == 0 and half == 0:
                    dma("sp", dbg["d_score"][:, 0:NK], score[:, 0:NK], (Bscore,), ())
                red("dve", bst[:, 0:1], score[:, 0:NK], ALU.min, (Bscore,), (Bbst,))
                red("dve", bst[:, 1:2], score[:, 0:NK], ALU.max, (Bscore,), (Bbst,))
                tt("dve", score[:, NK - 2048:NK], score[:, NK - 2048:NK], negb_sb[:, half, :], ALU.add,
                   (Bscore, Bnegb), (Bscore,))
                lo, hi, mid, inc = (bst[:, i:i + 1] for i in range(4))
                tt("dve", hi, hi, lo, ALU.subtract, (Bbst,), (Bbst,))
                ts("dve", btab[:, 0:NBISECT], smc[:, 36:36 + NBISECT], hi, None, ALU.mult, None, (Bsmc, Bbst), (Bbtab,))
                mset("dve", bcnt_t, 0.0, (Bbtab,))
                for it in range(NBISECT):
                    tt("dve", mid, lo, btab[:, it:it + 1], ALU.add, (Bbst, Bbtab), (Bbst,))
                    ts("dve", sel[:, 0:NK], score[:, 0:NK], mid, 0.0, ALU.is_ge, ALU.add, (Bscore, Bbst, Bsel), (Bsel, Bbtab),
                       accum=bcnt_t[:, it:it + 1])
                    stt("dve", inc, bcnt_t[:, it:it + 1], float(TOPK) - 0.5, btab[:, it:it + 1], ALU.is_ge, ALU.mult,
                        (Bbtab,), (Bbst,))
                    tt("dve", lo, lo, inc, ALU.add, (Bbst,), (Bbst,))
                ts("dve", sel[:, 0:NK], score[:, 0:NK], lo, None, ALU.is_ge, None, (Bscore, Bbst, Bsel, Bsel2), (Bsel, Bsel2))
                if debug:
                    dma("sp", dbg["d_thr"][s_:s_ + 1, :].rearrange("a p -> p a"), lo, (Bbst,), ())
                pb, Bp = nextbank()
                pbb = pb[:, :].bitcast(BF16)
                tr(pbb[0:16, 0:128], sel[:, 0:16], ident_b, (Bsel, Bcmb), (Bp,))
                cp("act", selT[0:16, 0, half * 128:(half + 1) * 128], pbb[0:16, 0:128], (Bp,), (BselT,))
                for g8 in range((NKB - 1) // 8):
                    pb, Bp = nextbank()
                    pbb = pb[:, :].bitcast(BF16)
                    for i in range(8):
                        kb = 1 + 8 * g8 + i
                        k0 = 16 + 128 * (kb - 1)
                        tr(pbb[:, i * 128:(i + 1) * 128], sel[:, k0:k0 + 128], ident_b, (Bsel, Bcmb), (Bp,))
                    cp("act", selT[:, 1 + 8 * g8:9 + 8 * g8, half * 128:(half + 1) * 128],
                       pbb.rearrange("p (a b) -> p a b", a=8), (Bp,), (BselT,))
            for h in range(8):
                attention(m, h, "a", Qa_p, BQa, KaT, Va, Kr, Vr, PTr, onr, rsr, yaT, ByaT, acnt,
                          selT=selT, BselT=BselT, nxt=((m, h + 1) if h < 7 else None))
        flush_fin()
        if debug:
            dma("pool", dbg["d_ya"][:, 0:8 * NT], yaT.rearrange("p a b -> p (a b)"), (ByaT,), ())
        S.barrier()
        A.release(md)
        if stop_after == "dsa":
            return _finish(nc, S, out_d)

        ybT = A.alloc([128, 8, NT], BF16)
        BybT = Buf("ybT")
        mf = A.mark()
        maskT_sb = A.alloc([128, 16, 256], BF16)
        Bmask = Buf("maskT2")
        dma("pool", maskT_sb, maskT_d.rearrange("p (t q) -> p t q", t=16), (), (Bmask,))
        oh_sb = A.alloc([128, 8, 66], F32)
        Boh = Buf("oh")
        dma("sp", oh_sb, ohsel_d.rearrange("p (s b) -> p s b", s=8), (), (Boh,))
        Lref = A.alloc([128, 8, 8], F32)
        BLref = Buf("Lref")
        tmpL = A.alloc([128, 8, 66], F32)
        BtmpL = Buf("tmpL")
        for s_ in range(NS):
            ohb = mkap(oh_sb, s_ * 66, [128, (0, 8), (1, 66)])
            tt("dve", tmpL, Tall, ohb, ALU.mult, (BT, Boh), (BtmpL,))
            red("dve", Lref[:, s_, :], tmpL, ALU.add, (BtmpL,), (BLref,))
        Qb_p = A.alloc([128, 8, 256], BF16)
        BQb = Buf("Qb_p")
        biasr = [ring(2, [128, 66], F32, "biasA"), ring(2, [128, 66], F32, "biasB")]
        Kr, Vr, PTr, onr, rsr = kv_rings()
        bcnt = {"o": 0, "kv": 0, "pt": 0, "fin": 0}
        it_ = 0
        for m in range(NP_):
            dma("sp", Qb_p, QbT[:, :, 256 * m:256 * m + 256].rearrange("h p q -> p h q"), (), (BQb,))
            for h in range(8):
                bA, BbA = biasr[0][it_ % 2]
                bB, BbB = biasr[1][it_ % 2]
                it_ += 1
                for half, (bt, Bbt) in enumerate(((bA, BbA), (bB, BbB))):
                    s_ = 2 * m + half
                    ts("dve", bt, Lall[:, h, :], Lref[:, s_, h:h + 1], 60.0, ALU.subtract, ALU.min, (BL, BLref), (Bbt,))
                Bbias = Buf("biasjoin")
                Bbias.writers = list(BbA.writers) + list(BbB.writers)
                nx = (m, h + 1) if h < 7 else ((m + 1, 0) if m + 1 < NP_ else None)
                attention(m, h, "b", Qb_p, BQb, KbT, Vb, Kr, Vr, PTr, onr, rsr, ybT, BybT, bcnt,
                          maskT_sb=maskT_sb, Bmask=Bmask, biasAB=(bA, bB), Bbias=Bbias, nxt=nx)
                for ev in Bbias.readers:
                    BbA.readers.append(ev)
                    BbB.readers.append(ev)
        flush_fin()
        if debug:
            dma("pool", dbg["d_yb"][:, 0:8 * NT], ybT.rearrange("p a b -> p (a b)"), (BybT,), ())
        S.barrier()
        A.release(mf)
        setbanks(0, 8)
        if stop_after == "fox":
            return _finish(nc, S, out_d)

        mergedT = A.alloc_top([128, 16, NT], BF16)
        Bmerged = Buf("merged")
        mm_ = A.mark()
        hnO = A.alloc([128, 16, NT], BF16)
        BhnO = Buf("hnO2")
        dma("sp", hnO, HnO[:, :, 0:NT], (), (BhnO,))
        wstage_alloc(3, ("dve", "act"))
        gwr = ring(4, [128, 16, 128], BF16, "gw")
        bwr = ring(4, [128, 8, 128], BF16, "bw")
        sgr = ring(2, [128, TG], F32, "sg")
        m1r = ring(2, [128, TG], F32, "m1")
        for n in range(16):
            Wga, BWga = gwr[(2 * n) % 4]
            Wgb, BWgb = gwr[(2 * n + 1) % 4]
            Wba, BWba = bwr[(2 * n) % 4]
            Wbb, BWbb = bwr[(2 * n + 1) % 4]
            wload(Wga, w_in[:, C_GA + 128 * n:C_GA + 128 * n + 128].rearrange("(c p) n -> p c n", p=128), BWga)
            wload(Wgb, w_in[:, C_GB + 128 * n:C_GB + 128 * n + 128].rearrange("(c p) n -> p c n", p=128), BWgb)
            wload(Wba, w_bra[:, 128 * n:128 * n + 128].rearrange("(c p) n -> p c n", p=128), BWba)
            wload(Wbb, w_brb[:, 128 * n:128 * n + 128].rearrange("(c p) n -> p c n", p=128), BWbb)
            for tg in range(NTG):
                cols = slice(tg * TG, (tg + 1) * TG)
                res = []
                for (Wg, BWg, Wb, BWb, yT_, ByT_) in ((Wga, BWga, Wba, BWba, yaT, ByaT), (Wgb, BWgb, Wbb, BWbb, ybT, BybT)):
                    pg, Bpg = nextbank()
                    for c in range(16):
                        mm(pg[:, 0:TG], Wg[:, c, :], hnO[:, c, cols], c == 0, c == 15, (BWg, BhnO), (Bpg,))
                    pbr, Bpbr = nextbank()
                    for j in range(8):
                        mm(pbr[:, 0:TG], Wb[:, j, :], yT_[:, j, cols], j == 0, j == 7, (BWb, ByT_), (Bpbr,))
                    sg, Bsg = sgr[len(res) % 2]
                    m1, Bm1 = m1r[len(res) % 2]
                    act(sg, pg[:, 0:TG], AF.Sigmoid, (Bpg,), (Bsg,))
                    tt("dve", m1, sg, pbr[:, 0:TG], ALU.mult, (Bsg, Bpbr), (Bm1,))
                    res.append((m1, Bm1))
                tt("pool", mergedT[:, n, cols], res[0][0], res[1][0], ALU.add, (res[0][1], res[1][1]), (Bmerged,))
        S.barrier()
        A.release(persist_mark)
        if stop_after == "mergea":
            return _finish(nc, S, out_d)
        h2 = A.alloc([128, NS, 2048], F32)
        Bh2 = [Buf("h2_%d" % i) for i in range(NS)]
        mo = A.mark()
        for s_ in range(NS):
            dma("sp", h2[:, s_, :], xown[128 * s_:128 * s_ + 128, :], (), (Bh2[s_],))
        wstage_alloc(3, ("dve", "act", "pool"))
        wor = ring(2, [128, 16, 512], BF16, "wo")
        for cg in range(4):
            Wo, BWo = wor[cg % 2]
            wload(Wo, w_out[:, cg * 512:(cg + 1) * 512].rearrange("(c p) n -> p c n", p=128), BWo)
            for s_ in range(NS):
                pb, Bp = nextbank()
                for c in range(16):
                    mm(pb[:, :], mergedT[:, c, 128 * s_:128 * s_ + 128], Wo[:, c, :], c == 0, c == 15, (Bmerged, BWo), (Bp,))
                tt("dve", h2[:, s_, cg * 512:(cg + 1) * 512], h2[:, s_, cg * 512:(cg + 1) * 512], pb[:, :], ALU.add,
                   (Bp, Bh2[s_]), (Bh2[s_],))
        if debug:
            for s_ in range(NS):
                dma("sp", dbg["d_h2"][128 * s_:128 * s_ + 128, :], h2[:, s_, :], (Bh2[s_],), ())
        S.barrier()
        A.release(mo)
        A.limit = A.full
        if stop_after == "merge":
            return _finish(nc, S, out_d)

        tT = A.alloc([128, 16, NT], BF16)
        BtT = Buf("tT")
        gates = A.alloc([128, NS, 16], F32)
        Bgates = Buf("gates")
        mp = A.mark()
        gbc = A.alloc([128, 2048], F32)
        Bg = Buf("gffn")
        dma("sp", gbc, g_ffn.partition_broadcast(128), (), (Bg,))
        Wge = A.alloc([128, 16, 20], F32)
        BWge = Buf("Wge")
        dma("sp", Wge, w_ge.rearrange("(c p) n -> p c n", p=128), (), (BWge,))
        tnr = ring(1, [128, 2048], F32, "tn")
        t32r = ring(1, [128, 16, 128], F32, "t32")
        statr = ring(2, [128, 8], F32, "mst")
        def route_math(rt, R_, s_):
            gmax, ngmax, sume, pgrp = rt[:, 20:21], rt[:, 21:22], rt[:, 22:23], rt[:, 23:24]
            ohg, eg = rt[:, 24:28], rt[:, 28:32]
            red("dve", gmax, rt[:, 0:4], ALU.max, R_, R_)
            ts("dve", ohg, rt[:, 0:4], gmax, None, ALU.is_ge, None, R_, R_)
            ts("dve", ngmax, gmax, -1.0, None, ALU.mult, None, R_, R_)
            mset("dve", sume, 0.0, R_)
            act(eg, rt[:, 0:4], AF.Exp, R_, R_, bias=ngmax, accum=sume)
            recip(pgrp, sume, R_, R_)
            tmp16 = rt[:, 32:48]
            tt("dve", tmp16.rearrange("p (g j) -> p g j", g=4), rt[:, 4:20].rearrange("p (g j) -> p g j", g=4),
               mkap(rt, 24, [128, (1, 4), (0, 4)]), ALU.mult, R_, R_)
            esel = rt[:, 48:52]
            red("dve", esel, mkap(rt, 32, [128, (1, 4), (4, 4)]), ALU.add, R_, R_)
            m1_, mk1, e2_, m2_, mk2 = rt[:, 52:53], rt[:, 53:57], rt[:, 57:61], rt[:, 61:62], rt[:, 0:4]
            red("dve", m1_, esel, ALU.max, R_, R_)
            ts("dve", mk1, esel, m1_, None, ALU.is_ge, None, R_, R_)
            stt("dve", e2_, mk1, -1e30, esel, ALU.mult, ALU.add, R_, R_)
            red("dve", m2_, e2_, ALU.max, R_, R_)
            ts("dve", mk2, e2_, m2_, None, ALU.is_ge, None, R_, R_)
            dd_, ed_, p1_, p2_ = rt[:, 4:5], rt[:, 5:6], rt[:, 6:7], rt[:, 7:8]
            tt("dve", dd_, m2_, m1_, ALU.subtract, R_, R_)
            act(ed_, dd_, AF.Exp, R_, R_)
            ts("dve", p1_, ed_, 1.0, None, ALU.add, None, R_, R_)
            recip(p1_, p1_, R_, R_)
            tt("dve", p2_, ed_, p1_, ALU.mult, R_, R_)
            tt("dve", p1_, p1_, pgrp, ALU.mult, R_, R_)
            tt("dve", p2_, p2_, pgrp, ALU.mult, R_, R_)
            ge_ = rt[:, 8:12]
            ts("dve", ge_, mk1, p1_, None, ALU.mult, None, R_, R_)
            stt("dve", ge_, mk2, p2_, ge_, ALU.mult, ALU.add, R_, R_)
            for g in range(4):
                ts("dve", gates[:, s_, 4 * g:4 * g + 4], ge_, rt[:, 24 + g:25 + g], None, ALU.mult, None, R_, (Bgates,))

        rtr = ring(2, [128, 64], F32, "rt")
        tnr = ring(2, [128, 2048], F32, "tn2") if A.limit - A.off > 24 * 1024 else tnr
        route_q = []
        for s_ in range(NS):
            rt, Brt = rtr[s_ % 2]
            tn, Btn = tnr[s_ % len(tnr)]
            t32, Bt32 = t32r[0]
            st_, Bst_ = statr[s_ % 2]
            mset("dve", st_, 0.0, (Bst_,))
            act(tn, h2[:, s_, :], AF.Square, (Bh2[s_], Bst_), (Btn, Bst_), accum=st_[:, 0:1])
            rstd_from_ss(st_[:, 0:1], D, (Bst_,), (Bst_,))
            stt("dve", tn, h2[:, s_, :], st_[:, 0:1], gbc, ALU.mult, ALU.mult, (Bh2[s_], Bst_, Bg), (Btn,))
            for q4 in range(4):
                pb, Bp = nextbank()
                for i in range(4):
                    c = 4 * q4 + i
                    tr(pb[:, i * 128:(i + 1) * 128], tn[:, c * 128:(c + 1) * 128], ident_f, (Btn, Bcm), (Bp,))
                src = pb[:, :].rearrange("p (a b) -> p a b", a=4)
                cp("act", tT[:, 4 * q4:4 * q4 + 4, 128 * s_:128 * s_ + 128], src, (Bp,), (BtT,))
                cp("dve", t32[:, 4 * q4:4 * q4 + 4, :], src, (Bp,), (Bt32,))
            while route_q:
                route_q.pop(0)()
            pb, Bp = nextbank()
            for c in range(16):
                mm(pb[:, 0:20], t32[:, c, :], Wge[:, c, :], c == 0, c == 15, (Bt32, BWge), (Bp,))
            R_ = (Brt,)
            lg = rt[:, 0:20]
            tt("dve", lg, pb[:, 0:20], smc[:, 16:36], ALU.add, (Bp, Bsmc), R_)
            route_q.append(lambda rt=rt, R_=R_, s_=s_: route_math(rt, R_, s_))
        while route_q:
            route_q.pop(0)()
        if debug:
            dma("sp", dbg["d_gates"][:, 0:NS * 16], gates.rearrange("p a b -> p (a b)"), (Bgates,), ())
        S.barrier()
        A.release(mp)
        wstage_alloc(2, ("pool",))
        wgr = ring(2, [128, 16, 128], BF16, "wg")
        wur = ring(2, [128, 16, 128], BF16, "wu")
        wdr = ring(2, [128, 4, 2048], BF16, "wd")
        actr = ring(2, [128, 4, NT], BF16, "actT")
        sgr = ring(2, [128, TG], F32, "silu")
        wi_ = 0
        for e_ in range(NEXP):
            actT, Bact = actr[e_ % 2]
            Wd, BWd = wdr[e_ % 2]
            for f in range(4):
                wload(Wd[:, f, :], w_down[e_, f * 128:(f + 1) * 128, :], BWd)
            for ft in range(4):
                Wg, BWg = wgr[wi_ % 2]
                Wu, BWu = wur[wi_ % 2]
                wi_ += 1
                wload(Wg, w_gate[e_, :, ft * 128:(ft + 1) * 128].rearrange("(c p) n -> p c n", p=128), BWg)
                wload(Wu, w_up[e_, :, ft * 128:(ft + 1) * 128].rearrange("(c p) n -> p c n", p=128), BWu)
                for tg in range(NTG):
                    cols = slice(tg * TG, (tg + 1) * TG)
                    pg, Bpg = nextbank()
                    for c in range(16):
                        mm(pg[:, 0:TG], Wg[:, c, :], tT[:, c, cols], c == 0, c == 15, (BWg, BtT), (Bpg,))
                    pu, Bpu = nextbank()
                    for c in range(16):
                        mm(pu[:, 0:TG], Wu[:, c, :], tT[:, c, cols], c == 0, c == 15, (BWu, BtT), (Bpu,))
                    sg, Bsg = sgr[tg % 2]
                    act(sg, pg[:, 0:TG], AF.Silu, (Bpg,), (Bsg,))
                    tt("dve", actT[:, ft, cols], sg, pu[:, 0:TG], ALU.mult, (Bsg, Bpu), (Bact,))
            for s_ in range(NS):
                for cg in range(4):
                    pb, Bp = nextbank()
                    for f in range(4):
                        mm(pb[:, :], actT[:, f, 128 * s_:128 * s_ + 128], Wd[:, f, cg * 512:(cg + 1) * 512], f == 0, f == 3,
                           (Bact, BWd), (Bp,))
                    stt("dve", h2[:, s_, cg * 512:(cg + 1) * 512], pb[:, :], gates[:, s_, e_:e_ + 1],
                        h2[:, s_, cg * 512:(cg + 1) * 512], ALU.mult, ALU.add, (Bp, Bgates, Bh2[s_]), (Bh2[s_],))
        S.barrier()
        A.release(mp)
        gbc = A.alloc([128, 2048], F32)
        Bg = Buf("gfinal")
        dma("sp", gbc, g_final.partition_broadcast(128), (), (Bg,))
        outr = ring(2, [128, 2048], F32, "outst")
        statr = ring(2, [128, 8], F32, "fst")
        for s_ in range(NS):
            ot, Bot = outr[s_ % 2]
            st_, Bst_ = statr[s_ % 2]
            mset("dve", st_, 0.0, (Bst_,))
            act(ot, h2[:, s_, :], AF.Square, (Bh2[s_], Bst_), (Bot, Bst_), accum=st_[:, 0:1])
            rstd_from_ss(st_[:, 0:1], D, (Bst_,), (Bst_,))
            stt("dve", ot, h2[:, s_, :], st_[:, 0:1], gbc, ALU.mult, ALU.mult, (Bh2[s_], Bst_, Bg), (Bot,))
            dma("sp", out_d[128 * s_:128 * s_ + 128, :], ot, (Bot,), ())
        S.barrier()
        return _finish(nc, S, out_d)


def _finish(nc, S, out_d):
    with nc.Block() as block:
        S.emit(block)
    return nc


def own_blocks(c):
    out = []
    for m in range(4):
        out += [16*m + c, 16*m + 15 - c]
    return out

def rope_tables(pos, dim):
    half = dim // 2
    inv = (10000.0 ** (-np.arange(half, dtype=np.float32) / half)).astype(np.float32)
    ang = pos.astype(np.float32)[None, :] * inv[:, None]
    cos = np.cos(ang).astype(np.float32); sin = np.sin(ang).astype(np.float32)
    reps = 128 // dim
    cosT = np.concatenate([cos, cos] * reps, axis=0)
    sinT = np.concatenate([-sin, sin] * reps, axis=0)
    return np.ascontiguousarray(cosT), np.ascontiguousarray(sinT)

def _prep(inputs):
    x = np.asarray(inputs['x'], np.float32)[0]
    meta = np.asarray(inputs['meta_tokens'], np.float32)
    xall = np.ascontiguousarray(np.concatenate([meta, x], axis=0))
    posall = np.arange(L)
    cA, sA = rope_tables(posall, 128)
    cI, sI = rope_tables(posall, 64)
    g = lambda k: np.ascontiguousarray(np.asarray(inputs[k], np.float32))
    common = {
        'xall': xall, 'w_in': g('w_in')[0], 'w_kvup': g('w_kv_up')[0], 'w_bra': g('w_branch_a')[0],
        'w_brb': g('w_branch_b')[0], 'w_out': g('w_out')[0],
        'w_ge': np.ascontiguousarray(np.concatenate([g('w_group')[0], g('w_expert')[0]], axis=1)),
        'w_gate': g('w_gate_e')[0], 'w_up': g('w_up_e')[0], 'w_down': g('w_down_e')[0],
        'g_mix': g('g_mix').reshape(1, D), 'g_ffn': g('g_ffn').reshape(1, D), 'g_final': g('g_final').reshape(1, D),
        'cosA_all': cA, 'sinA_all': sA, 'cosI_all': cI, 'sinI_all': sI,
    }
    smallc = np.zeros((128, 64), np.float32)
    smallc[:, 0:4] = g('g_kv')[0].reshape(4, 128).T
    gk = g('g_idx_k')[0]
    p = np.arange(128)
    smallc[:, 4] = gk[p % 64]
    smallc[:, 5] = gk[(p % 64 + 32) % 64]
    smallc[:, 8:16] = g('b_f')[0][None, :]
    smallc[:, 16:20] = g('b_group')[0][None, :]
    smallc[:, 20:36] = g('b_expert')[0][None, :]
    smallc[:, 36:52] = (0.5 ** np.arange(1, 17, dtype=np.float32))[None, :]
    common['smallc'] = smallc
    cm = np.zeros((128, 512), np.float32)
    cm[:, 0:128] = np.eye(128)
    cm[:, 128:256] = np.triu(np.ones((128, 128)))
    cm[:, 256:384] = 1.0
    blk = np.zeros((128, 128)); blk[0:64, 0:64] = 1; blk[64:, 64:] = 1
    cm[:, 384:512] = blk
    common['cmat'] = cm
    maps = []
    tri = (np.arange(128)[:, None] <= np.arange(128)[None, :]).astype(np.float32)
    for c in range(NCORES):
        blks = own_blocks(c)
        rows = np.concatenate([np.arange(128 * b, 128 * b + 128) for b in blks])
        pos = rows + NMETA
        d = dict(common)
        d['xown'] = np.ascontiguousarray(x[rows])
        ca, sa = rope_tables(pos, 128); ci, si = rope_tables(pos, 64)
        sc = np.float32(128 ** -0.5)
        d['cosA_own'] = ca * sc; d['sinA_own'] = sa * sc; d['cosI_own'] = ci; d['sinI_own'] = si
        mT = np.zeros((128, 16, 256), np.float32)
        for t in range(16):
            for half, dg in ((0, c), (1, 15 - c)):
                if t < dg: mT[:, t, half*128:(half+1)*128] = 1.0
                elif t == dg: mT[:, t, half*128:(half+1)*128] = tri
        d['maskT'] = np.ascontiguousarray(mT.reshape(128, 16 * 256))
        nb = np.zeros((128, 2, 16, 128), np.float32)
        for half in range(2):
            m_qk = mT[:, :, half*128:(half+1)*128].transpose(2, 1, 0)
            nb[:, half] = np.where(m_qk > 0, 0.0, -1e30)
        d['negb'] = np.ascontiguousarray(nb.reshape(128, 2 * 2048))
        oh = np.zeros((128, 8, 66), np.float32)
        for s, b in enumerate(blks):
            oh[:, s, 1 + b] = 1.0
        d['ohsel'] = np.ascontiguousarray(oh.reshape(128, 8 * 66))
        maps.append(d)
    return maps

def _assemble(results):
    out = np.zeros((1, SEQ, D), np.float32)
    for c in range(NCORES):
        blks = own_blocks(c)
        o = results[c]['out']
        for s, b in enumerate(blks):
            out[0, 128*b:128*b+128] = o[128*s:128*s+128]
    return out


_PROGRAM = {}


def kernel(**inputs):
    maps = _prep(inputs)
    if "nc" not in _PROGRAM:
        _PROGRAM["nc"] = build_program()
    nc = _PROGRAM["nc"]
    res = run_bass_kernel_spmd(nc, maps, core_ids=list(range(NCORES)))
    return _assemble(res.results)
```

```python
import contextlib
import os
LVL = int(os.environ.get('LVL', '9'))
import numpy as np
import ml_dtypes
import concourse.bass as bass
import concourse.mybir as mybir
from concourse.bass_utils import run_bass_kernel_spmd

F32 = mybir.dt.float32
BF16 = mybir.dt.bfloat16
ALU = mybir.AluOpType
AF = mybir.ActivationFunctionType
AX = mybir.AxisListType

NCORES = 8
D = 2048
SEQ = 8192
NMETA = 16
L = SEQ + NMETA
NKB_ALL = 65
HD = 128
EPS = 1e-6
TOPK = 256
NBISECT = 15
C_QA, C_CKV, C_QI, C_KI, C_WI, C_QB, C_KB, C_VB, C_FB, C_GA, C_GB = (
    0, 1024, 1536, 2560, 2624, 2640, 3664, 4688, 5712, 5720, 7768)


class Buf:
    __slots__ = ("name", "writers", "readers", "excl")

    def __init__(self, name="", excl=False):
        self.name = name
        self.writers = []
        self.readers = []
        self.excl = excl


class _Eng:
    def __init__(self, name, sem):
        self.name = name
        self.sem = sem
        self.count = 0
        self.ops = []
        self.seen = {}
        self.dsems = []
        self.dcount = []
        self.dnext = 0


class Sched:
    ENG_NAMES = ("pe", "act", "dve", "pool", "sp")

    def __init__(self, nc, stack, n_dsem=24):
        self.nc = nc
        self.eng = {}
        self.clock = {}
        for n in self.ENG_NAMES:
            sem = stack.enter_context(nc.semaphore("s_" + n))
            self.eng[n] = _Eng(n, sem)
        for n in ("sp", "pool"):
            e = self.eng[n]
            for i in range(n_dsem):
                e.dsems.append(stack.enter_context(nc.semaphore("d_%s_%d" % (n, i))))
                e.dcount.append(0)
        self.semobj = {}
        for n in self.ENG_NAMES:
            self.semobj[("e", n)] = self.eng[n].sem
        for n in ("sp", "pool"):
            for i, s in enumerate(self.eng[n].dsems):
                self.semobj[("d", n, i)] = s
        self.nwaits = 0

    def _need(self, E, ev, out):
        key, val = ev
        if E.seen.get(key, 0) >= val:
            return
        if out.get(key, 0) < val:
            out[key] = val

    def _apply_waits(self, E, need):
        for key, val in need.items():
            if E.seen.get(key, 0) >= val:
                continue
            E.ops.append(("w", self.semobj[key], val))
            self.nwaits += 1
            ck = self.clock.get((key, val))
            if ck:
                for k2, v2 in ck.items():
                    if E.seen.get(k2, 0) < v2:
                        E.seen[k2] = v2
            if E.seen.get(key, 0) < val:
                E.seen[key] = val

    def _deps(self, E, reads, writes, same_ok):
        need = {}
        me = ("e", E.name)
        for b in reads:
            for ev in b.writers:
                if same_ok and ev[0] == me:
                    continue
                self._need(E, ev, need)
            if b.excl:
                for ev in b.readers:
                    if ev[0] != me:
                        self._need(E, ev, need)
        for b in writes:
            for ev in b.writers:
                if same_ok and ev[0] == me:
                    continue
                self._need(E, ev, need)
            for ev in b.readers:
                if same_ok and ev[0] == me:
                    continue
                self._need(E, ev, need)
        self._apply_waits(E, need)

    def _record(self, ev, reads, writes):
        for b in reads:
            b.readers.append(ev)
        for b in writes:
            b.writers = [ev]
            b.readers = []

    def op(self, eng, fn, reads=(), writes=()):
        E = self.eng[eng]
        self._deps(E, reads, writes, same_ok=(eng == "pe"))
        E.count += 1
        ev = (("e", eng), E.count)
        E.ops.append(("i", fn, E.sem, 1))
        ck = dict(E.seen)
        ck[("e", eng)] = E.count
        self.clock[ev] = ck
        self._record(ev, reads, writes)
        return ev

    def dma(self, eng, fn, reads=(), writes=(), fresh=False):
        E = self.eng[eng]
        self._deps(E, reads, () if fresh else writes, same_ok=False)
        i = E.dnext
        E.dnext = (E.dnext + 1) % len(E.dsems)
        key = ("d", eng, i)
        if E.dcount[i] > 0:
            need = {}
            self._need(E, (key, E.dcount[i]), need)
            self._apply_waits(E, need)
        E.dcount[i] += 16
        ev = (key, E.dcount[i])
        E.ops.append(("i", fn, E.dsems[i], 16))
        self.clock[ev] = dict(E.seen)
        if fresh:
            self._record(ev, reads, ())
            for b in writes:
                b.writers.append(ev)
        else:
            self._record(ev, reads, writes)
        return ev

    def barrier(self):
        for n in ("sp", "pool"):
            E = self.eng[n]
            need = {}
            for i in range(len(E.dsems)):
                if E.dcount[i] > 0:
                    self._need(E, (("d", n, i), E.dcount[i]), need)
            self._apply_waits(E, need)
        evs = []
        for n in self.ENG_NAMES:
            E = self.eng[n]
            if E.count > 0:
                need = {}
                self._need(E, (("e", n), E.count), need)
                self._apply_waits(E, need)
            evs.append(self.op(n, lambda e: e.nop(nofuse=True), (), ()))
        for n in self.ENG_NAMES:
            E = self.eng[n]
            need = {}
            for ev in evs:
                self._need(E, ev, need)
            self._apply_waits(E, need)

    def emit(self, block):
        def run(E, e):
            for o in E.ops:
                if o[0] == "w":
                    e.wait_ge(o[1], o[2])
                else:
                    o[1](e).then_inc(o[2], o[3])

        S = self

        @block.tensor
        def _(e):
            run(S.eng["pe"], e)

        @block.scalar
        def _(e):
            run(S.eng["act"], e)

        @block.vector
        def _(e):
            run(S.eng["dve"], e)

        @block.gpsimd
        def _(e):
            run(S.eng["pool"], e)

        @block.sync
        def _(e):
            run(S.eng["sp"], e)


class SBAlloc:
    def __init__(self, nc, nbytes=200 * 1024):
        self.arena = nc.alloc_sbuf_tensor("arena", [128, nbytes // 4], F32)
        self.off = 0
        self.limit = nbytes
        self.full = nbytes
        self.peak = 0

    def alloc(self, shape, dtype):
        esz = 2 if dtype == BF16 else 4
        n = 1
        for s in shape[1:]:
            n *= s
        fb = (n * esz + 63) // 64 * 64
        off = self.off
        assert off + fb <= self.limit, "SBUF overflow %d+%d" % (off, fb)
        self.off = off + fb
        self.peak = max(self.peak, self.off)
        v = self.arena[0:shape[0], off // 4:(off + fb) // 4]
        if dtype != F32:
            v = v.bitcast(dtype)
        v = v[:, 0:n]
        if len(shape) == 3:
            v = v.rearrange("p (a b) -> p a b", a=shape[1])
        elif len(shape) == 4:
            v = v.rearrange("p (a b c) -> p a b c", a=shape[1], b=shape[2])
        return v

    def alloc_top(self, shape, dtype):
        esz = 2 if dtype == BF16 else 4
        n = 1
        for s in shape[1:]:
            n *= s
        fb = (n * esz + 63) // 64 * 64
        self.limit -= fb
        assert self.off <= self.limit
        off = self.limit
        v = self.arena[0:shape[0], off // 4:(off + fb) // 4]
        if dtype != F32:
            v = v.bitcast(dtype)
        v = v[:, 0:n]
        if len(shape) == 3:
            v = v.rearrange("p (a b) -> p a b", a=shape[1])
        return v

    def mark(self):
        return self.off

    def release(self, m):
        self.off = m


def build_program(stop_after=None, debug=False, only=None, npairs=4, NEXP=16):
    def on(part):
        return only is None or part in only
    nc = bass.Bass("TRN2", target_bir_lowering=False)

    def din(name, shape, dt=F32):
        return nc.dram_tensor(name, list(shape), dt, kind="ExternalInput").ap()

    def dscr(name, shape, dt=BF16):
        if debug:
            return nc.dram_tensor(name, list(shape), dt, kind="ExternalOutput").ap()
        return nc.dram_tensor(name, list(shape), dt).ap()

    xall = din("xall", [L, D])
    xown = din("xown", [1024, D])
    w_in = din("w_in", [D, 9816])
    w_kvup = din("w_kvup", [512, 2048])
    w_bra = din("w_bra", [1024, D])
    w_brb = din("w_brb", [1024, D])
    w_out = din("w_out", [D, D])
    w_ge = din("w_ge", [D, 20])
    w_gate = din("w_gate", [16, D, 512])
    w_up = din("w_up", [16, D, 512])
    w_down = din("w_down", [16, 512, D])
    g_mix = din("g_mix", [1, D])
    g_ffn = din("g_ffn", [1, D])
    g_final = din("g_final", [1, D])
    smallc = din("smallc", [128, 64])
    cosA_all = din("cosA_all", [128, L])
    sinA_all = din("sinA_all", [128, L])
    cosI_all = din("cosI_all", [128, L])
    sinI_all = din("sinI_all", [128, L])
    cosA_own = din("cosA_own", [128, 1024])
    sinA_own = din("sinA_own", [128, 1024])
    cosI_own = din("cosI_own", [128, 1024])
    sinI_own = din("sinI_own", [128, 1024])
    maskT_d = din("maskT", [128, 16 * 256])
    negb_d = din("negb", [128, 2 * 2048])
    ohsel_d = din("ohsel", [128, 8 * 66])
    cmat = din("cmat", [128, 4 * 128])
    out_d = nc.dram_tensor("out", [1024, D], F32, kind="ExternalOutput").ap()

    KaT = dscr("KaT", [8, 128, L])
    KbT = dscr("KbT", [8, 128, L])
    KiT_d = dscr("KiT", [128, L])
    Va = dscr("Va", [L, 1024])
    Vb = dscr("Vb", [L, 1024])
    QaT = dscr("QaT", [8, 128, 1024])
    QbT = dscr("QbT", [8, 128, 1024])
    QiT = dscr("QiT", [8, 128, 1024])
    HnO = dscr("HnO", [128, 16, 1024])
    dbg = {}
    if debug:
        for nm, shp in (("d_hn", [128, 16, 1024]), ("d_L", [128, 8 * 66]), ("d_ya", [128, 8 * 1024]),
                        ("d_yb", [128, 8 * 1024]), ("d_h2", [1024, D]), ("d_thr", [8, 128]),
                        ("d_gates", [128, 8 * 16]), ("d_score", [128, 8208])):
            dbg[nm] = nc.dram_tensor(nm, shp, F32, kind="ExternalOutput").ap()

    with contextlib.ExitStack() as st:
        S = Sched(nc, st)
        A = SBAlloc(nc)
        banks = [nc.alloc_psum_tensor("psb%d" % i, [128, 512], F32) for i in range(8)]
        Bbank = [Buf("bank%d" % i, excl=True) for i in range(8)]
        rr = {"i": 0, "lo": 0, "hi": 8}

        def nextbank():
            i = rr["i"]
            rr["i"] = rr["lo"] + (i + 1 - rr["lo"]) % (rr["hi"] - rr["lo"])
            return banks[i], Bbank[i]

        def setbanks(lo, hi):
            rr["lo"], rr["hi"], rr["i"] = lo, hi, lo

        def mm(out, lhsT, rhs, start, stop, R, W):
            S.op("pe", lambda e: e.matmul(out, lhsT=lhsT, rhs=rhs, start=start, stop=stop), R, W)

        def tr(out, in_, ident, R, W):
            S.op("pe", lambda e: e.transpose(out=out, in_=in_, identity=ident), R, W)

        def act(out, in_, func, R, W, bias=None, scale=None, accum=None, eng="act"):
            kw = {}
            if bias is not None:
                kw["bias"] = bias
            if scale is not None:
                kw["scale"] = scale
            if accum is not None:
                kw["accum_out"] = accum
            S.op(eng, lambda e: e.activation(out=out, in_=in_, func=func, **kw), R, W)

        def ts(eng, out, in0, s1, s2, op0, op1, R, W, accum=None):
            kw = {}
            if op1 is not None:
                kw["op1"] = op1
            if accum is not None:
                kw["accum_out"] = accum
            S.op(eng, lambda e: e.tensor_scalar(out=out, in0=in0, scalar1=s1, scalar2=s2, op0=op0, **kw), R, W)

        def tt(eng, out, in0, in1, op, R, W):
            S.op(eng, lambda e: e.tensor_tensor(out=out, in0=in0, in1=in1, op=op), R, W)

        def stt(eng, out, in0, scalar, in1, op0, op1, R, W):
            S.op(eng, lambda e: e.scalar_tensor_tensor(out=out, in0=in0, scalar=scalar, in1=in1, op0=op0, op1=op1), R, W)

        def cp(eng, out, in_, R, W):
            if eng == "act":
                S.op("act", lambda e: e.copy(out=out, in_=in_), R, W)
            else:
                S.op(eng, lambda e: e.tensor_copy(out=out, in_=in_), R, W)

        def red(eng, out, in_, op, R, W):
            S.op(eng, lambda e: e.tensor_reduce(out=out, in_=in_, axis=AX.X, op=op), R, W)

        def recip(out, in_, R, W):
            S.op("dve", lambda e: e.reciprocal(out=out, in_=in_), R, W)

        def mset(eng, ap, val, W):
            S.op(eng, lambda e: e.memset(ap, val), (), W)

        def dma(eng, out, in_, R, W, fresh=False):
            S.dma(eng, lambda e: e.dma_start(out=out, in_=in_), R, W, fresh=fresh)

        def ring(n, shape, dtype, name="r"):
            return [(A.alloc(shape, dtype), Buf("%s%d" % (name, i))) for i in range(n)]

        wst = {"ring": None, "i": 0, "engs": ("pool",)}

        def wstage_alloc(n=3, engs=("pool",)):
            wst["ring"] = ring(n, [128, 2048], F32, "wstg")
            wst["i"] = 0
            wst["engs"] = engs

        def wload(dst, src, Bdst, cast_eng=None):
            shp = list(dst.shape)
            if len(shp) == 2:
                pieces = [(dst, src, shp[1], None)]
            else:
                a, b = shp[1], shp[2]
                step = max(1, 2048 // b)
                pieces = []
                for a0 in range(0, a, step):
                    a1 = min(a, a0 + step)
                    pieces.append((dst[:, a0:a1, :], src[:, a0:a1, :], (a1 - a0) * b, a1 - a0))
            for (d_, s_, n_, na) in pieces:
                stg, Bstg = wst["ring"][wst["i"] % len(wst["ring"])]
                wst["i"] += 1
                v = stg[:, 0:n_]
                if na is not None:
                    v = v.rearrange("p (a b) -> p a b", a=na)
                dma("sp", v, s_, (), (Bstg,))
                ce = cast_eng or wst["engs"][wst["i"] % len(wst["engs"])]
                cp(ce, d_, v, (Bstg,), (Bdst,))

        cm = A.alloc([128, 512], F32)
        Bcm = Buf("cm")
        dma("sp", cm, cmat[:, :], (), (Bcm,))
        ident_f = cm[:, 0:128]
        triU_f = cm[:, 128:256]
        ones_f = cm[:, 256:384]
        cmb = A.alloc([128, 512], BF16)
        Bcmb = Buf("cmb")
        cp("dve", cmb, cm, (Bcm,), (Bcmb,))
        ident_b = cmb[:, 0:128]
        ones_b = cmb[:, 256:384]
        blk64_b = cmb[:, 384:512]
        smc = A.alloc([128, 64], F32)
        Bsmc = Buf("smc")
        dma("sp", smc, smallc[:, :], (), (Bsmc,))
        Lall = A.alloc([128, 8, 66], F32)
        Tall = A.alloc([128, 8, 66], F32)
        BL = Buf("Lall")
        BT = Buf("Tall")
        mset("pool", Lall, 0.0, (BL,))
        mset("pool", Tall, 0.0, (BT,))
        wiabs = A.alloc([128, 8, 16], F32)
        wisgn = A.alloc([128, 8, 16], F32)
        Bwi = Buf("wi")
        persist_mark = A.mark()

        def rstd_from_ss(ss, n, R, W, tmp=None):
            ts("dve", ss, ss, 1.0 / n, EPS, ALU.mult, ALU.add, R, W)
            act(ss, ss, AF.Sqrt, W, W)
            recip(ss, ss, W, W)

        def norm_A(src_rows_ap, rows, gbc, Bg, xr, xnr, statr, it):
            xb, Bx = xr[it % len(xr)]
            xn, Bxn = xnr[it % len(xnr)]
            stt_, Bst = statr[it % len(statr)]
            dma("sp", xb[0:rows, :], src_rows_ap, (), (Bx,))
            mset("dve", stt_[0:rows, :], 0.0, (Bst,))
            act(xn[0:rows, :], xb[0:rows, :], AF.Square, (Bx, Bst), (Bst, Bxn), accum=stt_[0:rows, 0:1])
            rstd_from_ss(stt_[0:rows, 0:1], D, (Bst,), (Bst,))
            stt("dve", xn[0:rows, :], xb[0:rows, :], stt_[0:rows, 0:1], gbc[0:rows, :], ALU.mult, ALU.mult,
                (Bx, Bst, Bg), (Bxn,))
            return xn, Bxn

        def norm_B(xn, Bxn, rows, hnT, BhnT, col0):
            for half in range(2):
                pb, Bp = nextbank()
                pbb = pb[:, :].bitcast(BF16)
                for i in range(8):
                    c = half * 8 + i
                    tr(pbb[:, i * 128:i * 128 + rows], xn[0:rows, c * 128:(c + 1) * 128], ident_b[0:rows, 0:rows],
                       (Bxn, Bcmb), (Bp,))
                src = pbb.rearrange("p (a b) -> p a b", a=8)[:, :, 0:rows]
                cp("act", hnT[:, half * 8:half * 8 + 8, col0:col0 + rows], src, (Bp,), (BhnT,))

        def norm_block(src_rows_ap, rows, gbc, Bg, xr, xnr, statr, hnT, BhnT, col0, it):
            xn, Bxn = norm_A(src_rows_ap, rows, gbc, Bg, xr, xnr, statr, it)
            norm_B(xn, Bxn, rows, hnT, BhnT, col0)

        m0 = A.mark()
        gbc = A.alloc([128, 2048], F32)
        Bg = Buf("gmix")
        dma("sp", gbc, g_mix.partition_broadcast(128), (), (Bg,))
        Wckv = A.alloc([128, 16, 512], BF16)
        Wki = A.alloc([128, 16, 128], BF16)
        Wkis = A.alloc([128, 16, 128], BF16)
        Wkb = A.alloc([128, 16, 1024], BF16)
        Wvb = A.alloc([128, 16, 1024], BF16)
        Wfb = A.alloc([128, 16, 8], BF16)
        Wkk = A.alloc([128, 4, 1024], BF16)
        Wkks = A.alloc([128, 4, 1024], BF16)
        Wkv = A.alloc([128, 4, 1024], BF16)
        BW = Buf("W0")

        def wv(c0, n):
            return w_in[:, c0:c0 + n].rearrange("(c p) n -> p c n", p=128)

        def wv1(c0, n, c):
            return w_in[c * 128:(c + 1) * 128, c0:c0 + n]

        BW2 = Buf("W0b")
        BW3 = Buf("W0c")
        BW4 = Buf("W0d")
        BW5 = Buf("W0e")
        BW6 = Buf("W0f")
        for c in range(16):
            dma("pool", Wckv[:, c, :], wv1(C_CKV, 512, c), (), (BW,), fresh=True)
            for d0 in (0, 64):
                dma("pool", Wki[:, c, d0:d0 + 64], wv1(C_KI, 64, c), (), (BW2,), fresh=True)
                dma("pool", Wkis[:, c, d0:d0 + 32], wv1(C_KI + 32, 32, c), (), (BW2,), fresh=True)
                dma("pool", Wkis[:, c, d0 + 32:d0 + 64], wv1(C_KI, 32, c), (), (BW2,), fresh=True)
            dma("pool", Wkb[:, c, :], wv1(C_KB, 1024, c), (), (BW3,), fresh=True)
            dma("pool", Wvb[:, c, :], wv1(C_VB, 1024, c), (), (BW4,), fresh=True)
            dma("pool", Wfb[:, c, :], wv1(C_FB, 8, c), (), (BW5,), fresh=True)
        for c in range(4):
            kvr = w_kvup[c * 128:(c + 1) * 128, :]
            dma("pool", Wkk[:, c, :], kvr[:, 0:1024], (), (BW6,), fresh=True)
            dma("pool", Wkv[:, c, :], kvr[:, 1024:2048], (), (BW6,), fresh=True)
            src = kvr[:, 0:1024].rearrange("p (h t j) -> p h t j", h=8, t=2)
            dst = Wkks[:, c, :].rearrange("p (h t j) -> p h t j", h=8, t=2)
            dma("pool", dst[:, :, 0, :], src[:, :, 1, :], (), (BW6,), fresh=True)
            dma("pool", dst[:, :, 1, :], src[:, :, 0, :], (), (BW6,), fresh=True)
        if stop_after == "w0":
            S.barrier()
            return _finish(nc, S, out_d)
        Wall = (BW, BW2, BW3, BW4, BW5, BW6)

        xr = ring(1, [128, 2048], F32, "x")
        xnr = ring(2, [128, 2048], BF16, "xn")
        statr = ring(4, [128, 8], F32, "st")
        hnr = ring(2, [128, 16, 256], BF16, "hn")
        tabr = ring(1, [128, 4, 256], F32, "tab")
        sqr = ring(1, [128, 4, 256], BF16, "sq")
        ckvTr = ring(1, [128, 4, 256], F32, "ckvT")
        rstdr = ring(2, [128, 256], F32, "rstd")
        ckvnr = ring(2, [128, 4, 256], BF16, "ckvn")
        t1r = ring(2, [128, 256], F32, "t1")
        t2r = ring(2, [128, 256], F32, "t2")
        kst = ring(4, [128, 256], BF16, "kst")
        vst = ring(3, [128, 1024], BF16, "vst")
        ksqr = ring(1, [128, 256], BF16, "ksq")
        kiAr = ring(1, [128, 256], F32, "kiA")
        kiBr = ring(1, [128, 256], F32, "kiB")
        lr = ring(2, [128, 8], F32, "l")
        zr = ring(2, [128, 8], F32, "z")

        cnt = {"blk": 0, "k": 0, "v": 0, "t": 0}
        groups = [(0, 16)] + [(16 + 256 * g, 256) for g in range(32)]
        groups = groups[:1 + 8 * npairs]
        if stop_after == "p0small":
            groups = groups[1:3] if only and "nometa" in only else groups[:3]
        pendB = []

        def do_norm_A(gi_):
            p0_, T_ = groups[gi_]
            hnT_, BhnT_ = hnr[gi_ % 2]
            for j_ in range(1 if T_ == 16 else 2):
                rows_ = 16 if T_ == 16 else 128
                xn_, Bxn_ = norm_A(xall[p0_ + 128 * j_:p0_ + 128 * j_ + rows_, :], rows_, gbc, Bg, xr, xnr, statr,
                                   cnt["blk"])
                cnt["blk"] += 1
                pendB.append((xn_, Bxn_, rows_, hnT_, BhnT_, 128 * j_))

        def do_norm_B():
            while pendB:
                norm_B(*pendB.pop(0))

        deferred = []
        do_norm_A(0)
        do_norm_B()
        for gi, (p0, T) in enumerate(groups):
            hnT, BhnT = hnr[gi % 2]
            tab, Btab = tabr[0]
            nblk = 1 if T == 16 else 2
            for ti, tsrc in enumerate((cosA_all, sinA_all, cosI_all, sinI_all)):
                dma("sp", tab[:, ti, 0:T], tsrc[:, p0:p0 + T], (), (Btab,))
            sq, Bsq = sqr[0]
            ckvT, BckvT = ckvTr[0]
            rs, Brs = rstdr[gi % 2]
            rs2, Brs2 = rstdr[(gi + 1) % 2]
            ckvn, Bckvn = ckvnr[gi % 2]
            for n in range(4):
                pb, Bp = nextbank()
                for c in range(16):
                    mm(pb[:, 0:T], Wckv[:, c, n * 128:(n + 1) * 128], hnT[:, c, 0:T], c == 0, c == 15,
                       (BW, BhnT), (Bp,))
                act(sq[:, n, 0:T], pb[:, 0:T], AF.Square, (Bp,), (Bsq,))
                cp("dve", ckvT[:, n, 0:T], pb[:, 0:T], (Bp,), (BckvT,))
            pki, Bpki = nextbank()
            pki2, Bpki2 = nextbank()
            for c in range(16):
                mm(pki[:, 0:T], Wki[:, c, :], hnT[:, c, 0:T], c == 0, c == 15, (BW2, BhnT), (Bpki,))
            for c in range(16):
                mm(pki2[:, 0:T], Wkis[:, c, :], hnT[:, c, 0:T], c == 0, c == 15, (BW2, BhnT), (Bpki2,))
            ksq, Bksq = ksqr[0]
            act(ksq[:, 0:T], pki[:, 0:T], AF.Square, (Bpki,), (Bksq,))
            kiA, BkiA = kiAr[0]
            kiB, BkiB = kiBr[0]
            ts("dve", kiA[:, 0:T], pki[:, 0:T], smc[:, 4:5], None, ALU.mult, None, (Bpki, Bsmc), (BkiA,))
            ts("dve", kiB[:, 0:T], pki2[:, 0:T], smc[:, 5:6], None, ALU.mult, None, (Bpki2, Bsmc), (BkiB,))
            for h in range(8):
                pb, Bp = nextbank()
                for c in range(16):
                    mm(pb[:, 0:T], Wkb[:, c, h * 128:(h + 1) * 128], hnT[:, c, 0:T], c == 0, c == 15,
                       (BW3, BhnT), (Bp,))
                ks, Bks = kst[cnt["k"] % 4]
                cnt["k"] += 1
                cp("act", ks[:, 0:T], pb[:, 0:T], (Bp,), (Bks,))
                dma("sp", KbT[h, :, p0:p0 + T], ks[:, 0:T], (Bks,), ())
                if h == 3:
                    pss, Bpss = nextbank()
                    for n in range(4):
                        mm(pss[:, 0:T], ones_b, sq[:, n, 0:T], n == 0, n == 3, (Bcmb, Bsq), (Bpss,))
                    mm(pss[:, 256:256 + T], blk64_b, ksq[:, 0:T], True, True, (Bcmb, Bksq), (Bpss,))
                    cp("dve", rs[:, 0:T], pss[:, 0:T], (Bpss,), (Brs,))
                    cp("dve", rs2[:, 0:T], pss[:, 256:256 + T], (Bpss,), (Brs2,))
                    rstd_from_ss(rs[:, 0:T], 512, (Brs,), (Brs,))
                    rstd_from_ss(rs2[:, 0:T], 64, (Brs2,), (Brs2,))
                    for n in range(4):
                        stt("dve", ckvn[:, n, 0:T], ckvT[:, n, 0:T], smc[:, n:n + 1], rs[:, 0:T], ALU.mult, ALU.mult,
                            (BckvT, Bsmc, Brs), (Bckvn,))
                    if gi + 1 < len(groups):
                        do_norm_A(gi + 1)
            for j in range(nblk):
                rows = 16 if T == 16 else 128
                b = 0 if T == 16 else 1 + 2 * (gi - 1) + j
                vs, Bvs = vst[cnt["v"] % 3]
                cnt["v"] += 1
                for half in range(2):
                    pb, Bp = nextbank()
                    for c in range(16):
                        mm(pb[0:rows, :], hnT[:, c, 128 * j:128 * j + rows], Wvb[:, c, half * 512:(half + 1) * 512],
                           c == 0, c == 15, (BW4, BhnT), (Bp,))
                    cp("act", vs[0:rows, half * 512:(half + 1) * 512], pb[0:rows, :], (Bp,), (Bvs,))
                dma("sp", Vb[p0 + 128 * j:p0 + 128 * j + rows, :], vs[0:rows, :], (Bvs,), ())
                pb, Bp = nextbank()
                for c in range(16):
                    mm(pb[0:rows, 0:8], hnT[:, c, 128 * j:128 * j + rows], Wfb[:, c, 0:8], c == 0, c == 15,
                       (BW5, BhnT), (Bp,))
                z, Bz = zr[b % 2]
                l, Bl = lr[b % 2]
                tt("dve", z[0:rows, :], pb[0:rows, 0:8], smc[0:rows, 8:16], ALU.add, (Bp, Bsmc), (Bz,))
                act(z[0:rows, :], z[0:rows, :], AF.Exp, (Bz,), (Bz,), scale=-1.0)
                act(l[0:rows, :], z[0:rows, :], AF.Ln, (Bz,), (Bl,), bias=1.0)
                def cums(rows=rows, l=l, Bl=Bl, b=b):
                    pb, Bp = nextbank()
                    mm(pb[0:rows, 0:8], triU_f[0:rows, 0:rows], l[0:rows, :], True, True, (Bcm, Bl), (Bp,))
                    mm(pb[:, 8:16], ones_f[0:rows, :], l[0:rows, :], True, True, (Bcm, Bl), (Bp,))
                    tt("dve", Lall[0:rows, :, b], pb[0:rows, 0:8], Tall[0:rows, :, b], ALU.add, (Bp, BT), (BL,))
                    tt("dve", Tall[:, :, b + 1], pb[:, 8:16], Tall[:, :, b], ALU.add, (Bp, BT), (BT,))

                deferred.append(cums)
            do_norm_B()
            tt("pool", kiA[:, 0:T], kiA[:, 0:T], rs2[:, 0:T], ALU.mult, (BkiA, Brs2), (BkiA,))
            tt("pool", kiB[:, 0:T], kiB[:, 0:T], rs2[:, 0:T], ALU.mult, (BkiB, Brs2), (BkiB,))
            tt("pool", kiA[:, 0:T], kiA[:, 0:T], tab[:, 2, 0:T], ALU.mult, (BkiA, Btab), (BkiA,))
            tt("pool", kiB[:, 0:T], kiB[:, 0:T], tab[:, 3, 0:T], ALU.mult, (BkiB, Btab), (BkiB,))
            ks, Bks = kst[cnt["k"] % 4]
            cnt["k"] += 1
            tt("pool", ks[:, 0:T], kiA[:, 0:T], kiB[:, 0:T], ALU.add, (BkiA, BkiB), (Bks,))
            dma("sp", KiT_d[:, p0:p0 + T], ks[:, 0:T], (Bks,), ())
            for h in range(8):
                pb, Bp = nextbank()
                pb2, Bp2 = nextbank()
                for c in range(4):
                    mm(pb[:, 0:T], Wkk[:, c, h * 128:(h + 1) * 128], ckvn[:, c, 0:T], c == 0, c == 3,
                       (BW6, Bckvn), (Bp,))
                for c in range(4):
                    mm(pb2[:, 0:T], Wkks[:, c, h * 128:(h + 1) * 128], ckvn[:, c, 0:T], c == 0, c == 3,
                       (BW6, Bckvn), (Bp2,))
                t1, Bt1 = t1r[cnt["t"] % 2]
                t2, Bt2 = t2r[cnt["t"] % 2]
                cnt["t"] += 1
                tt("dve", t1[:, 0:T], pb[:, 0:T], tab[:, 0, 0:T], ALU.mult, (Bp, Btab), (Bt1,))
                tt("dve", t2[:, 0:T], pb2[:, 0:T], tab[:, 1, 0:T], ALU.mult, (Bp2, Btab), (Bt2,))
                ks, Bks = kst[cnt["k"] % 4]
                cnt["k"] += 1
                tt("pool", ks[:, 0:T], t1[:, 0:T], t2[:, 0:T], ALU.add, (Bt1, Bt2), (Bks,))
                dma("sp", KaT[h, :, p0:p0 + T], ks[:, 0:T], (Bks,), ())
            for j in range(nblk):
                rows = 16 if T == 16 else 128
                vs, Bvs = vst[cnt["v"] % 3]
                cnt["v"] += 1
                for half in range(2):
                    pb, Bp = nextbank()
                    for c in range(4):
                        mm(pb[0:rows, :], ckvn[:, c, 128 * j:128 * j + rows], Wkv[:, c, half * 512:(half + 1) * 512],
                           c == 0, c == 3, (BW6, Bckvn), (Bp,))
                    cp("act", vs[0:rows, half * 512:(half + 1) * 512], pb[0:rows, :], (Bp,), (Bvs,))
                dma("sp", Va[p0 + 128 * j:p0 + 128 * j + rows, :], vs[0:rows, :], (Bvs,), ())
            while deferred:
                deferred.pop(0)()
        if debug:
            dma("sp", dbg["d_L"], Lall.rearrange("p a b -> p (a b)"), (BL,), ())
        S.barrier()
        A.release(m0)
        if stop_after in ("p0", "p0small"):
            return _finish(nc, S, out_d)
        NP_ = npairs
        NS = 2 * NP_
        NT = 128 * NS
        TG = min(512, NT)
        NTG = NT // TG

        def mkap(base, off_elems, dims):
            pstride = base.ap[0][0]
            return bass.AP(base.tensor, base.offset + off_elems, [(pstride, dims[0])] + [tuple(d) for d in dims[1:]])

        yaT = A.alloc([128, 8, NT], BF16)
        ByaT = Buf("yaT")
        mq = A.mark()
        gbc = A.alloc([128, 2048], F32)
        Bg = Buf("gmix2")
        dma("sp", gbc, g_mix.partition_broadcast(128), (), (Bg,))
        hnO = A.alloc([128, 16, NT], BF16)
        BhnO = Buf("hnO")
        xr = ring(2, [128, 2048], F32, "qx")
        xnr = ring(2, [128, 2048], BF16, "qxn")
        statr = ring(4, [128, 8], F32, "qst")
        for s_ in range(NS):
            norm_block(xown[128 * s_:128 * s_ + 128, :], 128, gbc, Bg, xr, xnr, statr, hnO, BhnO, 128 * s_, s_)
        dma("sp", HnO[:, :, 0:NT], hnO, (BhnO,), ())
        tabq = A.alloc([128, 4, NT], F32)
        Btabq = Buf("tabq")
        for ti, tsrc in enumerate((cosA_own, sinA_own, cosI_own, sinI_own)):
            dma("sp", tabq[:, ti, :], tsrc[:, 0:NT], (), (Btabq,), fresh=True)
        wstage_alloc(3, ("dve",))
        wr = ring(3, [128, 16, 128], BF16, "qw")
        wsr = ring(3, [128, 16, 128], BF16, "qws")
        qst = ring(3, [128, NT], BF16, "qstage")
        t1r = ring(2, [128, TG], F32, "qt1")
        t2r = ring(2, [128, TG], F32, "qt2")
        specs = [("qa", h, C_QA + 128 * h, QaT) for h in range(8)] + \
                [("qi", t, C_QI + 128 * t, QiT) for t in range(8)] + \
                [("qb", h, C_QB + 128 * h, QbT) for h in range(8)]
        def q_load(i):
            kind, idx, col0, dst = specs[i]
            W, BWt = wr[i % 3]
            Ws, BWs = wsr[i % 3]
            wsrc = w_in[:, col0:col0 + 128].rearrange("(c p) n -> p c n", p=128)
            wload(W, wsrc, BWt)
            if kind != "qb":
                hw = 64 if kind == "qa" else 32
                for j in range(128 // hw):
                    jo = j ^ 1
                    wload(Ws[:, :, j * hw:(j + 1) * hw], wsrc[:, :, jo * hw:(jo + 1) * hw], BWs)

        q_load(0)
        q_load(1)
        for i, (kind, idx, col0, dst) in enumerate(specs):
            W, BWt = wr[i % 3]
            Ws, BWs = wsr[i % 3]
            if i + 2 < len(specs):
                q_load(i + 2)
            st_, Bst_ = qst[i % 3]
            for tg in range(NTG):
                cols = slice(tg * TG, (tg + 1) * TG)
                pb, Bp = nextbank()
                for c in range(16):
                    mm(pb[:, 0:TG], W[:, c, :], hnO[:, c, cols], c == 0, c == 15, (BWt, BhnO), (Bp,))
                if kind == "qb":
                    act(st_[:, cols], pb[:, 0:TG], AF.Copy, (Bp,), (Bst_,), scale=float(128 ** -0.5))
                else:
                    pb2, Bp2 = nextbank()
                    for c in range(16):
                        mm(pb2[:, 0:TG], Ws[:, c, :], hnO[:, c, cols], c == 0, c == 15, (BWs, BhnO), (Bp2,))
                    ci = 0 if kind == "qa" else 2
                    t1, Bt1 = t1r[tg % 2]
                    t2, Bt2 = t2r[tg % 2]
                    tt("dve", t1, pb[:, 0:TG], tabq[:, ci, cols], ALU.mult, (Bp, Btabq), (Bt1,))
                    tt("dve", t2, pb2[:, 0:TG], tabq[:, ci + 1, cols], ALU.mult, (Bp2, Btabq), (Bt2,))
                    tt("pool", st_[:, cols], t1, t2, ALU.add, (Bt1, Bt2), (Bst_,))
            dma("sp", dst[idx, :, 0:NT], st_, (Bst_,), ())
        Wwi = A.alloc([128, 16, 16], BF16)
        BWwi = Buf("Wwi")
        dma("pool", Wwi, w_in[:, C_WI:C_WI + 16].rearrange("(c p) n -> p c n", p=128), (), (BWwi,))
        wtmp = A.alloc([128, 16], F32)
        Bwtmp = Buf("wtmp")
        for s_ in range(NS):
            pb, Bp = nextbank()
            for c in range(16):
                mm(pb[:, 0:16], hnO[:, c, 128 * s_:128 * s_ + 128], Wwi[:, c, :], c == 0, c == 15, (BWwi, BhnO), (Bp,))
            ts("dve", wtmp, pb[:, 0:16], 0.25 * 0.125, None, ALU.mult, None, (Bp,), (Bwtmp,))
            ts("dve", wisgn[:, s_, :], wtmp, 0.0, 2.0, ALU.is_ge, ALU.mult, (Bwtmp,), (Bwi,))
            ts("dve", wisgn[:, s_, :], wisgn[:, s_, :], -1.0, None, ALU.add, None, (Bwi,), (Bwi,))
            tt("dve", wiabs[:, s_, :], wtmp, wisgn[:, s_, :], ALU.mult, (Bwtmp, Bwi), (Bwi,))
        if debug:
            pass
        S.barrier()
        A.release(mq)
        if stop_after == "q":
            return _finish(nc, S, out_d)

        def kb_range(kb):
            return (0, 16) if kb == 0 else (16 + 128 * (kb - 1), 128)

        LOOK = 3
        pending_fin = []

        prefetched = {}

        def attention(m, h, mixer, Qp, BQp, KT_d, V_d, Kr, Vr, PTr, onr, rsr, yT, ByT, cnt,
                      selT=None, BselT=None, maskT_sb=None, Bmask=None, biasAB=None, Bbias=None, nxt=None):
            NKB = 16 * m + 17
            par = cnt["o"] % 2
            cnt["o"] += 1
            oA, BoA = banks[4 + 2 * par], Bbank[4 + 2 * par]
            oB, BoB = banks[5 + 2 * par], Bbank[5 + 2 * par]
            chunks = {}
            nch_tot = (NKB + 15) // 16

            def load_chunk(ch, mm_=m, hh_=h, NKB_=NKB):
                key = (mixer, mm_, hh_, ch)
                if key in prefetched:
                    chunks[ch] = prefetched.pop(key)
                    return
                NKBx = NKB_
                kb0 = 16 * ch
                kb1 = min(NKBx, kb0 + 16)
                klo = kb_range(kb0)[0]
                khi = kb_range(kb1 - 1)[0] + kb_range(kb1 - 1)[1]
                Kc, BKc = Kr[cnt["kv"] % len(Kr)]
                Vc, BVc = Vr[cnt["kv"] % len(Vr)]
                cnt["kv"] += 1
                dma("sp", Kc[:, 0:khi - klo], KT_d[hh_, :, klo:khi], (), (BKc,))
                j0 = 0
                if kb0 == 0:
                    dma("sp", Vc[0:16, 0, 0:128], V_d[0:16, hh_ * 128:(hh_ + 1) * 128], (), (BVc,))
                    j0 = 1
                nreal = (kb1 - kb0) - j0
                r0 = kb_range(kb0 + j0)[0]
                dma("sp", Vc[:, j0:j0 + nreal, 0:128],
                    V_d[r0:r0 + 128 * nreal, hh_ * 128:(hh_ + 1) * 128].rearrange("(j p) d -> p j d", p=128),
                    (), (BVc,), fresh=(j0 == 1))
                rec = (Kc, BKc, Vc, BVc, klo, kb0)
                if (mm_, hh_) == (m, h):
                    chunks[ch] = rec
                else:
                    prefetched[key] = rec

            pts = {}

            def emit_S(kb):
                ch = kb // 16
                if ch not in chunks:
                    load_chunk(ch)
                if kb % 16 == 0:
                    if ch + 1 < nch_tot:
                        if (ch + 1) not in chunks:
                            load_chunk(ch + 1)
                    elif nxt is not None:
                        load_chunk(0, nxt[0], nxt[1], 16 * nxt[0] + 17)
                Kc, BKc, Vc, BVc, klo, kb0 = chunks[ch]
                k0, ksz = kb_range(kb)
                pb, Bp = nextbank()
                mm(pb[0:ksz, 0:256], Kc[:, k0 - klo:k0 - klo + ksz], Qp[:, h, :], True, True, (BKc, BQp), (Bp,))
                PT, BPT = PTr[cnt["pt"] % len(PTr)]
                cnt["pt"] += 1
                if mixer == "a":
                    act(PT[0:ksz, :], pb[0:ksz, 0:256], AF.Exp, (Bp,), (BPT,))
                    tt("pool" if kb % 3 == 2 else "dve", PT[0:ksz, :], PT[0:ksz, :], selT[0:ksz, kb, :], ALU.mult,
                       (BPT, BselT), (BPT,))
                else:
                    for half in range(2):
                        act(PT[0:ksz, half * 128:(half + 1) * 128], pb[0:ksz, half * 128:(half + 1) * 128], AF.Exp,
                            (Bp, Bbias), (BPT,), bias=biasAB[half][0:ksz, kb:kb + 1])
                    t = kb - (NKB - 16)
                    if t >= 0:
                        tt("dve", PT[0:ksz, :], PT[0:ksz, :], maskT_sb[0:ksz, t, :], ALU.mult, (BPT, Bmask), (BPT,))
                pts[kb] = (PT, BPT)

            def emit_PV(kb):
                ch = kb // 16
                Kc, BKc, Vc, BVc, klo, kb0 = chunks[ch]
                k0, ksz = kb_range(kb)
                j = kb - kb0
                PT, BPT = pts.pop(kb)
                mm(oA[:, 0:129], PT[0:ksz, 0:128], Vc[0:ksz, j, :], kb == 0, kb == NKB - 1, (BPT, BVc), (BoA,))
                mm(oB[:, 0:129], PT[0:ksz, 128:256], Vc[0:ksz, j, :], kb == 0, kb == NKB - 1, (BPT, BVc), (BoB,))

            for kb in range(min(LOOK, NKB)):
                emit_S(kb)
            while pending_fin:
                pending_fin.pop(0)()
            for kb in range(NKB):
                emit_PV(kb)
                if kb + LOOK < NKB:
                    emit_S(kb + LOOK)

            def fin():
                for half, (o_, Bo_) in enumerate(((oA, BoA), (oB, BoB))):
                    s_ = 2 * m + half
                    rs_, Brs_ = rsr[cnt["fin"] % len(rsr)]
                    on_, Bon_ = onr[cnt["fin"] % len(onr)]
                    cnt["fin"] += 1
                    recip(rs_[:, 0:1], o_[:, 128:129], (Bo_,), (Brs_,))
                    ts("dve", on_, o_[:, 0:128], rs_[:, 0:1], None, ALU.mult, None, (Bo_, Brs_), (Bon_,))
                    pb, Bp = nextbank()
                    pbb = pb[:, :].bitcast(BF16)
                    tr(pbb[:, 0:128], on_, ident_b, (Bon_, Bcmb), (Bp,))
                    cp("act", yT[:, h, 128 * s_:128 * s_ + 128], pbb[:, 0:128], (Bp,), (ByT,))

            pending_fin.append(fin)

        def flush_fin():
            while pending_fin:
                pending_fin.pop(0)()

        def kv_rings():
            Kr = ring(3, [128, 2048], BF16, "Kc")
            Vr = ring(3, [128, 16, 129], BF16, "Vc")
            for Vc, BVc in Vr:
                mset("pool", Vc[:, :, 128:129], 1.0, (BVc,))
            PTr = ring(5, [128, 256], BF16, "PT")
            onr = ring(4, [128, 128], BF16, "on")
            rsr = ring(4, [128, 8], F32, "rs")
            return Kr, Vr, PTr, onr, rsr

        md = A.mark()
        setbanks(0, 4)
        KiT_sb = A.alloc([128, L], BF16)
        BKi = Buf("KiT_sb")
        NKTOT = 16 + 128 * 16 * NP_
        dma("sp", KiT_sb[:, 0:NKTOT], KiT_d[:, 0:NKTOT], (), (BKi,))
        maskT_sb = A.alloc([128, 16, 256], BF16)
        Bmask = Buf("maskT")
        dma("pool", maskT_sb, maskT_d.rearrange("p (t q) -> p t q", t=16), (), (Bmask,))
        negb_sb = A.alloc([128, 2, 2048], F32)
        Bnegb = Buf("negb")
        dma("sp", negb_sb, negb_d.rearrange("p (a k) -> p a k", a=2), (), (Bnegb,))
        score = A.alloc([128, L], F32)
        Bscore = Buf("score")
        sel = A.alloc([128, L], BF16)
        Bsel = Buf("sel")
        selT = A.alloc([128, 65, 256], BF16)
        BselT = Buf("selT")
        rr_ = ring(6, [128, 512], F32, "relu")
        Qi_p = A.alloc([128, 8, 256], BF16)
        BQi = Buf("Qi_p")
        Qa_p = A.alloc([128, 8, 256], BF16)
        BQa = Buf("Qa_p")
        bst = A.alloc([128, 16], F32)
        Bbst = Buf("bst")
        btab = A.alloc([128, 16], F32)
        bcnt_t = A.alloc([128, 16], F32)
        Bbtab = Buf("btab")
        cntp = A.alloc([128, 8], F32)[:, 0:1]
        Bcntp = Buf("cntp")
        Bsel2 = Buf("sel2")
        Kr, Vr, PTr, onr, rsr = kv_rings()
        acnt = {"o": 0, "kv": 0, "pt": 0, "fin": 0}
        for m in range(NP_):
            NKB = 16 * m + 17
            NK = 16 + 128 * (NKB - 1)
            flush_fin()
            dma("sp", Qi_p, QiT[:, :, 256 * m:256 * m + 256].rearrange("t p q -> p t q"), (), (BQi,))
            dma("sp", Qa_p, QaT[:, :, 256 * m:256 * m + 256].rearrange("h p q -> p h q"), (), (BQa,))
            for half in range(2):
                s_ = 2 * m + half
                nchunk = (NK + 511) // 512
                Bscs = [Buf("sc%d" % i) for i in range(nchunk)]
                for b_ in Bscs:
                    b_.writers = list(Bscore.writers)
                    b_.readers = list(Bscore.readers)
                for h in range(16):
                    po = (h % 2) * 64
                    for ch in range(nchunk):
                        k0 = ch * 512
                        kw = min(512, NK - k0)
                        pb, Bp = nextbank()
                        mm(pb[:, 0:kw], Qi_p[po:po + 64, h // 2, half * 128:(half + 1) * 128],
                           KiT_sb[po:po + 64, k0:k0 + kw], True, True, (BQi, BKi), (Bp,))
                        r_, Br_ = rr_[(h * nchunk + ch) % 6]
                        act(r_[:, 0:kw], pb[:, 0:kw], AF.Relu, (Bp, Bwi), (Br_,), scale=wiabs[:, s_, h:h + 1])
                        Bsc = Bscs[ch]
                        if h == 0:
                            act(score[:, k0:k0 + kw], r_[:, 0:kw], AF.Copy, (Br_, Bwi), (Bsc,), scale=wisgn[:, s_, h:h + 1])
                        elif ch % 4 == 3:
                            act(r_[:, 0:kw], r_[:, 0:kw], AF.Copy, (Br_, Bwi), (Br_,), scale=wisgn[:, s_, h:h + 1])
                            tt("pool", score[:, k0:k0 + kw], score[:, k0:k0 + kw], r_[:, 0:kw], ALU.add, (Br_, Bsc), (Bsc,))
                        else:
                            stt("dve", score[:, k0:k0 + kw], r_[:, 0:kw], wisgn[:, s_, h:h + 1], score[:, k0:k0 + kw],
                                ALU.mult, ALU.add, (Br_, Bwi, Bsc), (Bsc,))
                Bscore.writers = [ev for b_ in Bscs for ev in b_.writers]
                Bscore.readers = [ev for b_ in Bscs for ev in b_.readers]
                if debug and m == 0 and half == 0:
                    dma("sp", dbg["d_score"][:, 0:NK], score[:, 0:NK], (Bscore,), ())
                red("dve", bst[:, 0:1], score[:, 0:NK], ALU.min, (Bscore,), (Bbst,))
                red("dve", bst[:, 1:2], score[:, 0:NK], ALU.max, (Bscore,), (Bbst,))
                tt("dve", score[:, NK - 2048:NK], score[:, NK - 2048:NK], negb_sb[:, half, :], ALU.add,
                   (Bscore, Bnegb), (Bscore,))
                lo, hi, mid, inc = (bst[:, i:i + 1] for i in range(4))
                tt("dve", hi, hi, lo, ALU.subtract, (Bbst,), (Bbst,))
                ts("dve", btab[:, 0:NBISECT], smc[:, 36:36 + NBISECT], hi, None, ALU.mult, None, (Bsmc, Bbst), (Bbtab,))
                mset("dve", bcnt_t, 0.0, (Bbtab,))
                tt("dve", mid, lo, btab[:, 0:1], ALU.add, (Bbst, Bbtab), (Bbst,))
                for it in range(NBISECT):
                    ts("dve", sel[:, 0:NK], score[:, 0:NK], mid, 0.0, ALU.is_ge, ALU.add, (Bscore, Bbst, Bsel), (Bsel, Bbtab),
                       accum=bcnt_t[:, it:it + 1])
                    stt("dve", inc, bcnt_t[:, it:it + 1], float(TOPK) - 0.5, btab[:, it:it + 1], ALU.is_ge, ALU.mult,
                        (Bbtab,), (Bbst,))
                    nx_ = it + 1 if it + 1 < NBISECT else it
                    dst_ = mid if it + 1 < NBISECT else lo
                    stt("dve", dst_, inc, btab[:, nx_:nx_ + 1], mid, ALU.subtract, ALU.add, (Bbst, Bbtab), (Bbst,))
                stt("dve", lo, btab[:, NBISECT - 1:NBISECT], -0.0625, lo, ALU.mult, ALU.add, (Bbtab, Bbst), (Bbst,))
                ts("dve", sel[:, 0:NK], score[:, 0:NK], lo, None, ALU.is_ge, None, (Bscore, Bbst, Bsel, Bsel2), (Bsel, Bsel2))
                if debug:
                    dma("sp", dbg["d_thr"][s_:s_ + 1, :].rearrange("a p -> p a"), lo, (Bbst,), ())
                pb, Bp = nextbank()
                pbb = pb[:, :].bitcast(BF16)
                tr(pbb[0:16, 0:128], sel[:, 0:16], ident_b, (Bsel, Bcmb), (Bp,))
                cp("act", selT[0:16, 0, half * 128:(half + 1) * 128], pbb[0:16, 0:128], (Bp,), (BselT,))
                for g8 in range((NKB - 1) // 8):
                    pb, Bp = nextbank()
                    pbb = pb[:, :].bitcast(BF16)
                    for i in range(8):
                        kb = 1 + 8 * g8 + i
                        k0 = 16 + 128 * (kb - 1)
                        tr(pbb[:, i * 128:(i + 1) * 128], sel[:, k0:k0 + 128], ident_b, (Bsel, Bcmb), (Bp,))
                    cp("act", selT[:, 1 + 8 * g8:9 + 8 * g8, half * 128:(half + 1) * 128],
                       pbb.rearrange("p (a b) -> p a b", a=8), (Bp,), (BselT,))
            for h in range(8):
                attention(m, h, "a", Qa_p, BQa, KaT, Va, Kr, Vr, PTr, onr, rsr, yaT, ByaT, acnt,
                          selT=selT, BselT=BselT, nxt=((m, h + 1) if h < 7 else None))
        flush_fin()
        if debug:
            dma("pool", dbg["d_ya"][:, 0:8 * NT], yaT.rearrange("p a b -> p (a b)"), (ByaT,), ())
        S.barrier()
        A.release(md)
        if stop_after == "dsa":
            return _finish(nc, S, out_d)

        ybT = A.alloc([128, 8, NT], BF16)
        BybT = Buf("ybT")
        mf = A.mark()
        maskT_sb = A.alloc([128, 16, 256], BF16)
        Bmask = Buf("maskT2")
        dma("pool", maskT_sb, maskT_d.rearrange("p (t q) -> p t q", t=16), (), (Bmask,))
        oh_sb = A.alloc([128, 8, 66], F32)
        Boh = Buf("oh")
        dma("sp", oh_sb, ohsel_d.rearrange("p (s b) -> p s b", s=8), (), (Boh,))
        Lref = A.alloc([128, 8, 8], F32)
        BLref = Buf("Lref")
        tmpL = A.alloc([128, 8, 66], F32)
        BtmpL = Buf("tmpL")
        for s_ in range(NS):
            ohb = mkap(oh_sb, s_ * 66, [128, (0, 8), (1, 66)])
            tt("dve", tmpL, Tall, ohb, ALU.mult, (BT, Boh), (BtmpL,))
            red("dve", Lref[:, s_, :], tmpL, ALU.add, (BtmpL,), (BLref,))
        Qb_p = A.alloc([128, 8, 256], BF16)
        BQb = Buf("Qb_p")
        biasr = [ring(2, [128, 66], F32, "biasA"), ring(2, [128, 66], F32, "biasB")]
        Kr, Vr, PTr, onr, rsr = kv_rings()
        bcnt = {"o": 0, "kv": 0, "pt": 0, "fin": 0}
        it_ = 0
        for m in range(NP_):
            dma("sp", Qb_p, QbT[:, :, 256 * m:256 * m + 256].rearrange("h p q -> p h q"), (), (BQb,))
            for h in range(8):
                bA, BbA = biasr[0][it_ % 2]
                bB, BbB = biasr[1][it_ % 2]
                it_ += 1
                for half, (bt, Bbt) in enumerate(((bA, BbA), (bB, BbB))):
                    s_ = 2 * m + half
                    ts("dve", bt, Lall[:, h, :], Lref[:, s_, h:h + 1], 60.0, ALU.subtract, ALU.min, (BL, BLref), (Bbt,))
                Bbias = Buf("biasjoin")
                Bbias.writers = list(BbA.writers) + list(BbB.writers)
                nx = (m, h + 1) if h < 7 else ((m + 1, 0) if m + 1 < NP_ else None)
                attention(m, h, "b", Qb_p, BQb, KbT, Vb, Kr, Vr, PTr, onr, rsr, ybT, BybT, bcnt,
                          maskT_sb=maskT_sb, Bmask=Bmask, biasAB=(bA, bB), Bbias=Bbias, nxt=nx)
                for ev in Bbias.readers:
                    BbA.readers.append(ev)
                    BbB.readers.append(ev)
        flush_fin()
        if debug:
            dma("pool", dbg["d_yb"][:, 0:8 * NT], ybT.rearrange("p a b -> p (a b)"), (BybT,), ())
        S.barrier()
        A.release(mf)
        setbanks(0, 8)
        if stop_after == "fox":
            return _finish(nc, S, out_d)

        mergedT = A.alloc_top([128, 16, NT], BF16)
        Bmerged = Buf("merged")
        mm_ = A.mark()
        hnO = A.alloc([128, 16, NT], BF16)
        BhnO = Buf("hnO2")
        dma("sp", hnO, HnO[:, :, 0:NT], (), (BhnO,))
        wstage_alloc(3, ("dve", "act"))
        gwr = ring(4, [128, 16, 128], BF16, "gw")
        bwr = ring(4, [128, 8, 128], BF16, "bw")
        sgr = ring(2, [128, TG], F32, "sg")
        m1r = ring(2, [128, TG], F32, "m1")
        for n in range(16):
            Wga, BWga = gwr[(2 * n) % 4]
            Wgb, BWgb = gwr[(2 * n + 1) % 4]
            Wba, BWba = bwr[(2 * n) % 4]
            Wbb, BWbb = bwr[(2 * n + 1) % 4]
            wload(Wga, w_in[:, C_GA + 128 * n:C_GA + 128 * n + 128].rearrange("(c p) n -> p c n", p=128), BWga)
            wload(Wgb, w_in[:, C_GB + 128 * n:C_GB + 128 * n + 128].rearrange("(c p) n -> p c n", p=128), BWgb)
            wload(Wba, w_bra[:, 128 * n:128 * n + 128].rearrange("(c p) n -> p c n", p=128), BWba)
            wload(Wbb, w_brb[:, 128 * n:128 * n + 128].rearrange("(c p) n -> p c n", p=128), BWbb)
            for tg in range(NTG):
                cols = slice(tg * TG, (tg + 1) * TG)
                res = []
                for (Wg, BWg, Wb, BWb, yT_, ByT_) in ((Wga, BWga, Wba, BWba, yaT, ByaT), (Wgb, BWgb, Wbb, BWbb, ybT, BybT)):
                    pg, Bpg = nextbank()
                    for c in range(16):
                        mm(pg[:, 0:TG], Wg[:, c, :], hnO[:, c, cols], c == 0, c == 15, (BWg, BhnO), (Bpg,))
                    pbr, Bpbr = nextbank()
                    for j in range(8):
                        mm(pbr[:, 0:TG], Wb[:, j, :], yT_[:, j, cols], j == 0, j == 7, (BWb, ByT_), (Bpbr,))
                    sg, Bsg = sgr[len(res) % 2]
                    m1, Bm1 = m1r[len(res) % 2]
                    act(sg, pg[:, 0:TG], AF.Sigmoid, (Bpg,), (Bsg,))
                    tt("dve", m1, sg, pbr[:, 0:TG], ALU.mult, (Bsg, Bpbr), (Bm1,))
                    res.append((m1, Bm1))
                tt("pool", mergedT[:, n, cols], res[0][0], res[1][0], ALU.add, (res[0][1], res[1][1]), (Bmerged,))
        S.barrier()
        A.release(persist_mark)
        if stop_after == "mergea":
            return _finish(nc, S, out_d)
        h2 = A.alloc([128, NS, 2048], F32)
        Bh2 = [Buf("h2_%d" % i) for i in range(NS)]
        mo = A.mark()
        for s_ in range(NS):
            dma("sp", h2[:, s_, :], xown[128 * s_:128 * s_ + 128, :], (), (Bh2[s_],))
        wstage_alloc(3, ("dve", "act", "pool"))
        wor = ring(2, [128, 16, 512], BF16, "wo")
        for cg in range(4):
            Wo, BWo = wor[cg % 2]
            wload(Wo, w_out[:, cg * 512:(cg + 1) * 512].rearrange("(c p) n -> p c n", p=128), BWo)
            for s_ in range(NS):
                pb, Bp = nextbank()
                for c in range(16):
                    mm(pb[:, :], mergedT[:, c, 128 * s_:128 * s_ + 128], Wo[:, c, :], c == 0, c == 15, (Bmerged, BWo), (Bp,))
                tt("dve", h2[:, s_, cg * 512:(cg + 1) * 512], h2[:, s_, cg * 512:(cg + 1) * 512], pb[:, :], ALU.add,
                   (Bp, Bh2[s_]), (Bh2[s_],))
        if debug:
            for s_ in range(NS):
                dma("sp", dbg["d_h2"][128 * s_:128 * s_ + 128, :], h2[:, s_, :], (Bh2[s_],), ())
        S.barrier()
        A.release(mo)
        A.limit = A.full
        if stop_after == "merge":
            return _finish(nc, S, out_d)

        tT = A.alloc([128, 16, NT], BF16)
        BtT = Buf("tT")
        gates = A.alloc([128, NS, 16], F32)
        Bgates = Buf("gates")
        mp = A.mark()
        gbc = A.alloc([128, 2048], F32)
        Bg = Buf("gffn")
        dma("sp", gbc, g_ffn.partition_broadcast(128), (), (Bg,))
        Wge = A.alloc([128, 16, 20], F32)
        BWge = Buf("Wge")
        dma("sp", Wge, w_ge.rearrange("(c p) n -> p c n", p=128), (), (BWge,))
        tnr = ring(1, [128, 2048], F32, "tn")
        t32r = ring(1, [128, 16, 128], F32, "t32")
        statr = ring(2, [128, 8], F32, "mst")
        def route_math(rt, R_, s_):
            gmax, ngmax, sume, pgrp = rt[:, 20:21], rt[:, 21:22], rt[:, 22:23], rt[:, 23:24]
            ohg, eg = rt[:, 24:28], rt[:, 28:32]
            red("dve", gmax, rt[:, 0:4], ALU.max, R_, R_)
            ts("dve", ohg, rt[:, 0:4], gmax, None, ALU.is_ge, None, R_, R_)
            ts("dve", ngmax, gmax, -1.0, None, ALU.mult, None, R_, R_)
            mset("dve", sume, 0.0, R_)
            act(eg, rt[:, 0:4], AF.Exp, R_, R_, bias=ngmax, accum=sume)
            recip(pgrp, sume, R_, R_)
            tmp16 = rt[:, 32:48]
            tt("dve", tmp16.rearrange("p (g j) -> p g j", g=4), rt[:, 4:20].rearrange("p (g j) -> p g j", g=4),
               mkap(rt, 24, [128, (1, 4), (0, 4)]), ALU.mult, R_, R_)
            esel = rt[:, 48:52]
            red("dve", esel, mkap(rt, 32, [128, (1, 4), (4, 4)]), ALU.add, R_, R_)
            m1_, mk1, e2_, m2_, mk2 = rt[:, 52:53], rt[:, 53:57], rt[:, 57:61], rt[:, 61:62], rt[:, 0:4]
            red("dve", m1_, esel, ALU.max, R_, R_)
            ts("dve", mk1, esel, m1_, None, ALU.is_ge, None, R_, R_)
            stt("dve", e2_, mk1, -1e30, esel, ALU.mult, ALU.add, R_, R_)
            red("dve", m2_, e2_, ALU.max, R_, R_)
            ts("dve", mk2, e2_, m2_, None, ALU.is_ge, None, R_, R_)
            dd_, ed_, p1_, p2_ = rt[:, 4:5], rt[:, 5:6], rt[:, 6:7], rt[:, 7:8]
            tt("dve", dd_, m2_, m1_, ALU.subtract, R_, R_)
            act(ed_, dd_, AF.Exp, R_, R_)
            ts("dve", p1_, ed_, 1.0, None, ALU.add, None, R_, R_)
            recip(p1_, p1_, R_, R_)
            tt("dve", p2_, ed_, p1_, ALU.mult, R_, R_)
            tt("dve", p1_, p1_, pgrp, ALU.mult, R_, R_)
            tt("dve", p2_, p2_, pgrp, ALU.mult, R_, R_)
            ge_ = rt[:, 8:12]
            ts("dve", ge_, mk1, p1_, None, ALU.mult, None, R_, R_)
            stt("dve", ge_, mk2, p2_, ge_, ALU.mult, ALU.add, R_, R_)
            for g in range(4):
                ts("dve", gates[:, s_, 4 * g:4 * g + 4], ge_, rt[:, 24 + g:25 + g], None, ALU.mult, None, R_, (Bgates,))

        rtr = ring(2, [128, 64], F32, "rt")
        tnr = ring(2, [128, 2048], F32, "tn2") if A.limit - A.off > 24 * 1024 else tnr
        route_q = []
        for s_ in range(NS):
            rt, Brt = rtr[s_ % 2]
            tn, Btn = tnr[s_ % len(tnr)]
            t32, Bt32 = t32r[0]
            st_, Bst_ = statr[s_ % 2]
            mset("dve", st_, 0.0, (Bst_,))
            act(tn, h2[:, s_, :], AF.Square, (Bh2[s_], Bst_), (Btn, Bst_), accum=st_[:, 0:1])
            rstd_from_ss(st_[:, 0:1], D, (Bst_,), (Bst_,))
            stt("dve", tn, h2[:, s_, :], st_[:, 0:1], gbc, ALU.mult, ALU.mult, (Bh2[s_], Bst_, Bg), (Btn,))
            for q4 in range(4):
                pb, Bp = nextbank()
                for i in range(4):
                    c = 4 * q4 + i
                    tr(pb[:, i * 128:(i + 1) * 128], tn[:, c * 128:(c + 1) * 128], ident_f, (Btn, Bcm), (Bp,))
                src = pb[:, :].rearrange("p (a b) -> p a b", a=4)
                cp("act", tT[:, 4 * q4:4 * q4 + 4, 128 * s_:128 * s_ + 128], src, (Bp,), (BtT,))
                cp("dve", t32[:, 4 * q4:4 * q4 + 4, :], src, (Bp,), (Bt32,))
            while route_q:
                route_q.pop(0)()
            pb, Bp = nextbank()
            for c in range(16):
                mm(pb[:, 0:20], t32[:, c, :], Wge[:, c, :], c == 0, c == 15, (Bt32, BWge), (Bp,))
            R_ = (Brt,)
            lg = rt[:, 0:20]
            tt("dve", lg, pb[:, 0:20], smc[:, 16:36], ALU.add, (Bp, Bsmc), R_)
            route_q.append(lambda rt=rt, R_=R_, s_=s_: route_math(rt, R_, s_))
        while route_q:
            route_q.pop(0)()
        if debug:
            dma("sp", dbg["d_gates"][:, 0:NS * 16], gates.rearrange("p a b -> p (a b)"), (Bgates,), ())
        S.barrier()
        A.release(mp)
        wstage_alloc(2, ("pool",))
        wgr = ring(2, [128, 16, 128], BF16, "wg")
        wur = ring(2, [128, 16, 128], BF16, "wu")
        wdr = ring(2, [128, 4, 2048], BF16, "wd")
        actr = ring(2, [128, 4, NT], BF16, "actT")
        sgr = ring(2, [128, TG], F32, "silu")
        wi_ = 0
        for e_ in range(NEXP):
            actT, Bact = actr[e_ % 2]
            Wd, BWd = wdr[e_ % 2]
            for f in range(4):
                wload(Wd[:, f, :], w_down[e_, f * 128:(f + 1) * 128, :], BWd)
            for ft in range(4):
                Wg, BWg = wgr[wi_ % 2]
                Wu, BWu = wur[wi_ % 2]
                wi_ += 1
                wload(Wg, w_gate[e_, :, ft * 128:(ft + 1) * 128].rearrange("(c p) n -> p c n", p=128), BWg)
                wload(Wu, w_up[e_, :, ft * 128:(ft + 1) * 128].rearrange("(c p) n -> p c n", p=128), BWu)
                for tg in range(NTG):
                    cols = slice(tg * TG, (tg + 1) * TG)
                    pg, Bpg = nextbank()
                    for c in range(16):
                        mm(pg[:, 0:TG], Wg[:, c, :], tT[:, c, cols], c == 0, c == 15, (BWg, BtT), (Bpg,))
                    pu, Bpu = nextbank()
                    for c in range(16):
                        mm(pu[:, 0:TG], Wu[:, c, :], tT[:, c, cols], c == 0, c == 15, (BWu, BtT), (Bpu,))
                    sg, Bsg = sgr[tg % 2]
                    act(sg, pg[:, 0:TG], AF.Silu, (Bpg,), (Bsg,))
                    tt("dve", actT[:, ft, cols], sg, pu[:, 0:TG], ALU.mult, (Bsg, Bpu), (Bact,))
            for s_ in range(NS):
                for cg in range(4):
                    pb, Bp = nextbank()
                    for f in range(4):
                        mm(pb[:, :], actT[:, f, 128 * s_:128 * s_ + 128], Wd[:, f, cg * 512:(cg + 1) * 512], f == 0, f == 3,
                           (Bact, BWd), (Bp,))
                    stt("dve", h2[:, s_, cg * 512:(cg + 1) * 512], pb[:, :], gates[:, s_, e_:e_ + 1],
                        h2[:, s_, cg * 512:(cg + 1) * 512], ALU.mult, ALU.add, (Bp, Bgates, Bh2[s_]), (Bh2[s_],))
        S.barrier()
        A.release(mp)
        gbc = A.alloc([128, 2048], F32)
        Bg = Buf("gfinal")
        dma("sp", gbc, g_final.partition_broadcast(128), (), (Bg,))
        outr = ring(2, [128, 2048], F32, "outst")
        statr = ring(2, [128, 8], F32, "fst")
        for s_ in range(NS):
            ot, Bot = outr[s_ % 2]
            st_, Bst_ = statr[s_ % 2]
            mset("dve", st_, 0.0, (Bst_,))
            act(ot, h2[:, s_, :], AF.Square, (Bh2[s_], Bst_), (Bot, Bst_), accum=st_[:, 0:1])
            rstd_from_ss(st_[:, 0:1], D, (Bst_,), (Bst_,))
            stt("dve", ot, h2[:, s_, :], st_[:, 0:1], gbc, ALU.mult, ALU.mult, (Bh2[s_], Bst_, Bg), (Bot,))
            dma("sp", out_d[128 * s_:128 * s_ + 128, :], ot, (Bot,), ())
        S.barrier()
        return _finish(nc, S, out_d)


def _finish(nc, S, out_d):
    with nc.Block() as block:
        S.emit(block)
    return nc


def own_blocks(c):
    out = []
    for m in range(4):
        out += [16*m + c, 16*m + 15 - c]
    return out

def rope_tables(pos, dim):
    half = dim // 2
    inv = (10000.0 ** (-np.arange(half, dtype=np.float32) / half)).astype(np.float32)
    ang = pos.astype(np.float32)[None, :] * inv[:, None]
    cos = np.cos(ang).astype(np.float32); sin = np.sin(ang).astype(np.float32)
    reps = 128 // dim
    cosT = np.concatenate([cos, cos] * reps, axis=0)
    sinT = np.concatenate([-sin, sin] * reps, axis=0)
    return np.ascontiguousarray(cosT), np.ascontiguousarray(sinT)

def _prep(inputs):
    x = np.asarray(inputs['x'], np.float32)[0]
    meta = np.asarray(inputs['meta_tokens'], np.float32)
    xall = np.ascontiguousarray(np.concatenate([meta, x], axis=0))
    posall = np.arange(L)
    cA, sA = rope_tables(posall, 128)
    cI, sI = rope_tables(posall, 64)
    g = lambda k: np.ascontiguousarray(np.asarray(inputs[k], np.float32))
    common = {
        'xall': xall, 'w_in': g('w_in')[0], 'w_kvup': g('w_kv_up')[0], 'w_bra': g('w_branch_a')[0],
        'w_brb': g('w_branch_b')[0], 'w_out': g('w_out')[0],
        'w_ge': np.ascontiguousarray(np.concatenate([g('w_group')[0], g('w_expert')[0]], axis=1)),
        'w_gate': g('w_gate_e')[0], 'w_up': g('w_up_e')[0], 'w_down': g('w_down_e')[0],
        'g_mix': g('g_mix').reshape(1, D), 'g_ffn': g('g_ffn').reshape(1, D), 'g_final': g('g_final').reshape(1, D),
        'cosA_all': cA, 'sinA_all': sA, 'cosI_all': cI, 'sinI_all': sI,
    }
    smallc = np.zeros((128, 64), np.float32)
    smallc[:, 0:4] = g('g_kv')[0].reshape(4, 128).T
    gk = g('g_idx_k')[0]
    p = np.arange(128)
    smallc[:, 4] = gk[p % 64]
    smallc[:, 5] = gk[(p % 64 + 32) % 64]
    smallc[:, 8:16] = g('b_f')[0][None, :]
    smallc[:, 16:20] = g('b_group')[0][None, :]
    smallc[:, 20:36] = g('b_expert')[0][None, :]
    smallc[:, 36:52] = (0.5 ** np.arange(1, 17, dtype=np.float32))[None, :]
    common['smallc'] = smallc
    cm = np.zeros((128, 512), np.float32)
    cm[:, 0:128] = np.eye(128)
    cm[:, 128:256] = np.triu(np.ones((128, 128)))
    cm[:, 256:384] = 1.0
    blk = np.zeros((128, 128)); blk[0:64, 0:64] = 1; blk[64:, 64:] = 1
    cm[:, 384:512] = blk
    common['cmat'] = cm
    maps = []
    tri = (np.arange(128)[:, None] <= np.arange(128)[None, :]).astype(np.float32)
    for c in range(NCORES):
        blks = own_blocks(c)
        rows = np.concatenate([np.arange(128 * b, 128 * b + 128) for b in blks])
        pos = rows + NMETA
        d = dict(common)
        d['xown'] = np.ascontiguousarray(x[rows])
        ca, sa = rope_tables(pos, 128); ci, si = rope_tables(pos, 64)
        sc = np.float32(128 ** -0.5)
        d['cosA_own'] = ca * sc; d['sinA_own'] = sa * sc; d['cosI_own'] = ci; d['sinI_own'] = si
        mT = np.zeros((128, 16, 256), np.float32)
        for t in range(16):
            for half, dg in ((0, c), (1, 15 - c)):
                if t < dg: mT[:, t, half*128:(half+1)*128] = 1.0
                elif t == dg: mT[:, t, half*128:(half+1)*128] = tri
        d['maskT'] = np.ascontiguousarray(mT.reshape(128, 16 * 256))
        nb = np.zeros((128, 2, 16, 128), np.float32)
        for half in range(2):
            m_qk = mT[:, :, half*128:(half+1)*128].transpose(2, 1, 0)
            nb[:, half] = np.where(m_qk > 0, 0.0, -1e30)
        d['negb'] = np.ascontiguousarray(nb.reshape(128, 2 * 2048))
        oh = np.zeros((128, 8, 66), np.float32)
        for s, b in enumerate(blks):
            oh[:, s, 1 + b] = 1.0
        d['ohsel'] = np.ascontiguousarray(oh.reshape(128, 8 * 66))
        maps.append(d)
    return maps

def _assemble(results):
    out = np.zeros((1, SEQ, D), np.float32)
    for c in range(NCORES):
        blks = own_blocks(c)
        o = results[c]['out']
        for s, b in enumerate(blks):
            out[0, 128*b:128*b+128] = o[128*s:128*s+128]
    return out


_PROGRAM = {}


def kernel(**inputs):
    maps = _prep(inputs)
    if "nc" not in _PROGRAM:
        _PROGRAM["nc"] = build_program()
    nc = _PROGRAM["nc"]
    res = run_bass_kernel_spmd(nc, maps, core_ids=list(range(NCORES)))
    return _assemble(res.results)
```

```python
import contextlib
import os
LVL = int(os.environ.get('LVL', '9'))
import numpy as np
import ml_dtypes
import concourse.bass as bass
import concourse.mybir as mybir
from concourse.bass_utils import run_bass_kernel_spmd

F32 = mybir.dt.float32
BF16 = mybir.dt.bfloat16
ALU = mybir.AluOpType
AF = mybir.ActivationFunctionType
AX = mybir.AxisListType

NCORES = 8
D = 2048
SEQ = 8192
NMETA = 16
L = SEQ + NMETA
NKB_ALL = 65
HD = 128
EPS = 1e-6
TOPK = 256
NBISECT = 15
C_QA, C_CKV, C_QI, C_KI, C_WI, C_QB, C_KB, C_VB, C_FB, C_GA, C_GB = (
    0, 1024, 1536, 2560, 2624, 2640, 3664, 4688, 5712, 5720, 7768)


class Buf:
    __slots__ = ("name", "writers", "readers", "excl")

    def __init__(self, name="", excl=False):
        self.name = name
        self.writers = []
        self.readers = []
        self.excl = excl


class _Eng:
    def __init__(self, name, sem):
        self.name = name
        self.sem = sem
        self.count = 0
        self.ops = []
        self.seen = {}
        self.dsems = []
        self.dcount = []
        self.dnext = 0


class Sched:
    ENG_NAMES = ("pe", "act", "dve", "pool", "sp")

    def __init__(self, nc, stack, n_dsem=24):
        self.nc = nc
        self.eng = {}
        self.clock = {}
        for n in self.ENG_NAMES:
            sem = stack.enter_context(nc.semaphore("s_" + n))
            self.eng[n] = _Eng(n, sem)
        for n in ("sp", "pool"):
            e = self.eng[n]
            for i in range(n_dsem):
                e.dsems.append(stack.enter_context(nc.semaphore("d_%s_%d" % (n, i))))
                e.dcount.append(0)
        self.semobj = {}
        for n in self.ENG_NAMES:
            self.semobj[("e", n)] = self.eng[n].sem
        for n in ("sp", "pool"):
            for i, s in enumerate(self.eng[n].dsems):
                self.semobj[("d", n, i)] = s
        self.nwaits = 0

    def _need(self, E, ev, out):
        key, val = ev
        if E.seen.get(key, 0) >= val:
            return
        if out.get(key, 0) < val:
            out[key] = val

    def _apply_waits(self, E, need):
        for key, val in need.items():
            if E.seen.get(key, 0) >= val:
                continue
            E.ops.append(("w", self.semobj[key], val))
            self.nwaits += 1
            ck = self.clock.get((key, val))
            if ck:
                for k2, v2 in ck.items():
                    if E.seen.get(k2, 0) < v2:
                        E.seen[k2] = v2
            if E.seen.get(key, 0) < val:
                E.seen[key] = val

    def _deps(self, E, reads, writes, same_ok):
        need = {}
        me = ("e", E.name)
        for b in reads:
            for ev in b.writers:
                if same_ok and ev[0] == me:
                    continue
                self._need(E, ev, need)
            if b.excl:
                for ev in b.readers:
                    if ev[0] != me:
                        self._need(E, ev, need)
        for b in writes:
            for ev in b.writers:
                if same_ok and ev[0] == me:
                    continue
                self._need(E, ev, need)
            for ev in b.readers:
                if same_ok and ev[0] == me:
                    continue
                self._need(E, ev, need)
        self._apply_waits(E, need)

    def _record(self, ev, reads, writes):
        for b in reads:
            b.readers.append(ev)
        for b in writes:
            b.writers = [ev]
            b.readers = []

    def op(self, eng, fn, reads=(), writes=()):
        E = self.eng[eng]
        self._deps(E, reads, writes, same_ok=(eng == "pe"))
        E.count += 1
        ev = (("e", eng), E.count)
        E.ops.append(("i", fn, E.sem, 1))
        ck = dict(E.seen)
        ck[("e", eng)] = E.count
        self.clock[ev] = ck
        self._record(ev, reads, writes)
        return ev

    def dma(self, eng, fn, reads=(), writes=(), fresh=False):
        E = self.eng[eng]
        self._deps(E, reads, () if fresh else writes, same_ok=False)
        i = E.dnext
        E.dnext = (E.dnext + 1) % len(E.dsems)
        key = ("d", eng, i)
        if E.dcount[i] > 0:
            need = {}
            self._need(E, (key, E.dcount[i]), need)
            self._apply_waits(E, need)
        E.dcount[i] += 16
        ev = (key, E.dcount[i])
        E.ops.append(("i", fn, E.dsems[i], 16))
        self.clock[ev] = dict(E.seen)
        if fresh:
            self._record(ev, reads, ())
            for b in writes:
                b.writers.append(ev)
        else:
            self._record(ev, reads, writes)
        return ev

    def barrier(self):
        for n in ("sp", "pool"):
            E = self.eng[n]
            need = {}
            for i in range(len(E.dsems)):
                if E.dcount[i] > 0:
                    self._need(E, (("d", n, i), E.dcount[i]), need)
            self._apply_waits(E, need)
        evs = []
        for n in self.ENG_NAMES:
            E = self.eng[n]
            if E.count > 0:
                need = {}
                self._need(E, (("e", n), E.count), need)
                self._apply_waits(E, need)
            evs.append(self.op(n, lambda e: e.nop(nofuse=True), (), ()))
        for n in self.ENG_NAMES:
            E = self.eng[n]
            need = {}
            for ev in evs:
                self._need(E, ev, need)
            self._apply_waits(E, need)

    def emit(self, block):
        def run(E, e):
            for o in E.ops:
                if o[0] == "w":
                    e.wait_ge(o[1], o[2])
                else:
                    o[1](e).then_inc(o[2], o[3])

        S = self

        @block.tensor
        def _(e):
            run(S.eng["pe"], e)

        @block.scalar
        def _(e):
            run(S.eng["act"], e)

        @block.vector
        def _(e):
            run(S.eng["dve"], e)

        @block.gpsimd
        def _(e):
            run(S.eng["pool"], e)

        @block.sync
        def _(e):
            run(S.eng["sp"], e)


class SBAlloc:
    def __init__(self, nc, nbytes=200 * 1024):
        self.arena = nc.alloc_sbuf_tensor("arena", [128, nbytes // 4], F32)
        self.off = 0
        self.limit = nbytes
        self.full = nbytes
        self.peak = 0

    def alloc(self, shape, dtype):
        esz = 2 if dtype == BF16 else 4
        n = 1
        for s in shape[1:]:
            n *= s
        fb = (n * esz + 63) // 64 * 64
        off = self.off
        assert off + fb <= self.limit, "SBUF overflow %d+%d" % (off, fb)
        self.off = off + fb
        self.peak = max(self.peak, self.off)
        v = self.arena[0:shape[0], off // 4:(off + fb) // 4]
        if dtype != F32:
            v = v.bitcast(dtype)
        v = v[:, 0:n]
        if len(shape) == 3:
            v = v.rearrange("p (a b) -> p a b", a=shape[1])
        elif len(shape) == 4:
            v = v.rearrange("p (a b c) -> p a b c", a=shape[1], b=shape[2])
        return v

    def alloc_top(self, shape, dtype):
        esz = 2 if dtype == BF16 else 4
        n = 1
        for s in shape[1:]:
            n *= s
        fb = (n * esz + 63) // 64 * 64
        self.limit -= fb
        assert self.off <= self.limit
        off = self.limit
        v = self.arena[0:shape[0], off // 4:(off + fb) // 4]
        if dtype != F32:
            v = v.bitcast(dtype)
        v = v[:, 0:n]
        if len(shape) == 3:
            v = v.rearrange("p (a b) -> p a b", a=shape[1])
        return v

    def mark(self):
        return self.off

    def release(self, m):
        self.off = m


def build_program(stop_after=None, debug=False, only=None, npairs=4, NEXP=16):
    def on(part):
        return only is None or part in only
    nc = bass.Bass("TRN2", target_bir_lowering=False)

    def din(name, shape, dt=F32):
        return nc.dram_tensor(name, list(shape), dt, kind="ExternalInput").ap()

    def dscr(name, shape, dt=BF16):
        if debug:
            return nc.dram_tensor(name, list(shape), dt, kind="ExternalOutput").ap()
        return nc.dram_tensor(name, list(shape), dt).ap()

    xall = din("xall", [L, D])
    xown = din("xown", [1024, D])
    w_in = din("w_in", [D, 9816])
    w_kvup = din("w_kvup", [512, 2048])
    w_bra = din("w_bra", [1024, D])
    w_brb = din("w_brb", [1024, D])
    w_out = din("w_out", [D, D])
    w_ge = din("w_ge", [D, 20])
    w_gate = din("w_gate", [16, D, 512])
    w_up = din("w_up", [16, D, 512])
    w_down = din("w_down", [16, 512, D])
    g_mix = din("g_mix", [1, D])
    g_ffn = din("g_ffn", [1, D])
    g_final = din("g_final", [1, D])
    smallc = din("smallc", [128, 64])
    cosA_all = din("cosA_all", [128, L])
    sinA_all = din("sinA_all", [128, L])
    cosI_all = din("cosI_all", [128, L])
    sinI_all = din("sinI_all", [128, L])
    cosA_own = din("cosA_own", [128, 1024])
    sinA_own = din("sinA_own", [128, 1024])
    cosI_own = din("cosI_own", [128, 1024])
    sinI_own = din("sinI_own", [128, 1024])
    maskT_d = din("maskT", [128, 16 * 256])
    negb_d = din("negb", [128, 2 * 2048])
    ohsel_d = din("ohsel", [128, 8 * 66])
    cmat = din("cmat", [128, 4 * 128])
    out_d = nc.dram_tensor("out", [1024, D], F32, kind="ExternalOutput").ap()

    KaT = dscr("KaT", [8, 128, L])
    KbT = dscr("KbT", [8, 128, L])
    KiT_d = dscr("KiT", [128, L])
    Va = dscr("Va", [L, 1024])
    Vb = dscr("Vb", [L, 1024])
    QaT = dscr("QaT", [8, 128, 1024])
    QbT = dscr("QbT", [8, 128, 1024])
    QiT = dscr("QiT", [8, 128, 1024])
    HnO = dscr("HnO", [128, 16, 1024])
    dbg = {}
    if debug:
        for nm, shp in (("d_hn", [128, 16, 1024]), ("d_L", [128, 8 * 66]), ("d_ya", [128, 8 * 1024]),
                        ("d_yb", [128, 8 * 1024]), ("d_h2", [1024, D]), ("d_thr", [8, 128]),
                        ("d_gates", [128, 8 * 16]), ("d_score", [128, 8208])):
            dbg[nm] = nc.dram_tensor(nm, shp, F32, kind="ExternalOutput").ap()

    with contextlib.ExitStack() as st:
        S = Sched(nc, st)
        A = SBAlloc(nc)
        banks = [nc.alloc_psum_tensor("psb%d" % i, [128, 512], F32) for i in range(8)]
        Bbank = [Buf("bank%d" % i, excl=True) for i in range(8)]
        rr = {"i": 0, "lo": 0, "hi": 8}

        def nextbank():
            i = rr["i"]
            rr["i"] = rr["lo"] + (i + 1 - rr["lo"]) % (rr["hi"] - rr["lo"])
            return banks[i], Bbank[i]

        def setbanks(lo, hi):
            rr["lo"], rr["hi"], rr["i"] = lo, hi, lo

        def mm(out, lhsT, rhs, start, stop, R, W):
            S.op("pe", lambda e: e.matmul(out, lhsT=lhsT, rhs=rhs, start=start, stop=stop), R, W)

        def tr(out, in_, ident, R, W):
            S.op("pe", lambda e: e.transpose(out=out, in_=in_, identity=ident), R, W)

        def act(out, in_, func, R, W, bias=None, scale=None, accum=None, eng="act"):
            kw = {}
            if bias is not None:
                kw["bias"] = bias
            if scale is not None:
                kw["scale"] = scale
            if accum is not None:
                kw["accum_out"] = accum
            S.op(eng, lambda e: e.activation(out=out, in_=in_, func=func, **kw), R, W)

        def ts(eng, out, in0, s1, s2, op0, op1, R, W, accum=None):
            kw = {}
            if op1 is not None:
                kw["op1"] = op1
            if accum is not None:
                kw["accum_out"] = accum
            S.op(eng, lambda e: e.tensor_scalar(out=out, in0=in0, scalar1=s1, scalar2=s2, op0=op0, **kw), R, W)

        def tt(eng, out, in0, in1, op, R, W):
            S.op(eng, lambda e: e.tensor_tensor(out=out, in0=in0, in1=in1, op=op), R, W)

        def stt(eng, out, in0, scalar, in1, op0, op1, R, W):
            S.op(eng, lambda e: e.scalar_tensor_tensor(out=out, in0=in0, scalar=scalar, in1=in1, op0=op0, op1=op1), R, W)

        def cp(eng, out, in_, R, W):
            if eng == "act":
                S.op("act", lambda e: e.copy(out=out, in_=in_), R, W)
            else:
                S.op(eng, lambda e: e.tensor_copy(out=out, in_=in_), R, W)

        def red(eng, out, in_, op, R, W):
            S.op(eng, lambda e: e.tensor_reduce(out=out, in_=in_, axis=AX.X, op=op), R, W)

        def recip(out, in_, R, W):
            S.op("dve", lambda e: e.reciprocal(out=out, in_=in_), R, W)

        def mset(eng, ap, val, W):
            S.op(eng, lambda e: e.memset(ap, val), (), W)

        def dma(eng, out, in_, R, W, fresh=False):
            S.dma(eng, lambda e: e.dma_start(out=out, in_=in_), R, W, fresh=fresh)

        def ring(n, shape, dtype, name="r"):
            return [(A.alloc(shape, dtype), Buf("%s%d" % (name, i))) for i in range(n)]

        wst = {"ring": None, "i": 0, "engs": ("pool",)}

        def wstage_alloc(n=3, engs=("pool",)):
            wst["ring"] = ring(n, [128, 2048], F32, "wstg")
            wst["i"] = 0
            wst["engs"] = engs

        def wload(dst, src, Bdst, cast_eng=None):
            shp = list(dst.shape)
            if len(shp) == 2:
                pieces = [(dst, src, shp[1], None)]
            else:
                a, b = shp[1], shp[2]
                step = max(1, 2048 // b)
                pieces = []
                for a0 in range(0, a, step):
                    a1 = min(a, a0 + step)
                    pieces.append((dst[:, a0:a1, :], src[:, a0:a1, :], (a1 - a0) * b, a1 - a0))
            for (d_, s_, n_, na) in pieces:
                stg, Bstg = wst["ring"][wst["i"] % len(wst["ring"])]
                wst["i"] += 1
                v = stg[:, 0:n_]
                if na is not None:
                    v = v.rearrange("p (a b) -> p a b", a=na)
                dma("sp", v, s_, (), (Bstg,))
                ce = cast_eng or wst["engs"][wst["i"] % len(wst["engs"])]
                cp(ce, d_, v, (Bstg,), (Bdst,))

        cm = A.alloc([128, 512], F32)
        Bcm = Buf("cm")
        dma("sp", cm, cmat[:, :], (), (Bcm,))
        ident_f = cm[:, 0:128]
        triU_f = cm[:, 128:256]
        ones_f = cm[:, 256:384]
        cmb = A.alloc([128, 512], BF16)
        Bcmb = Buf("cmb")
        cp("dve", cmb, cm, (Bcm,), (Bcmb,))
        ident_b = cmb[:, 0:128]
        ones_b = cmb[:, 256:384]
        blk64_b = cmb[:, 384:512]
        smc = A.alloc([128, 64], F32)
        Bsmc = Buf("smc")
        dma("sp", smc, smallc[:, :], (), (Bsmc,))
        Lall = A.alloc([128, 8, 66], F32)
        Tall = A.alloc([128, 8, 66], F32)
        BL = Buf("Lall")
        BT = Buf("Tall")
        mset("pool", Lall, 0.0, (BL,))
        mset("pool", Tall, 0.0, (BT,))
        wiabs = A.alloc([128, 8, 16], F32)
        wisgn = A.alloc([128, 8, 16], F32)
        Bwi = Buf("wi")
        persist_mark = A.mark()

        def rstd_from_ss(ss, n, R, W, tmp=None):
            ts("dve", ss, ss, 1.0 / n, EPS, ALU.mult, ALU.add, R, W)
            act(ss, ss, AF.Sqrt, W, W)
            recip(ss, ss, W, W)

        def norm_A(src_rows_ap, rows, gbc, Bg, xr, xnr, statr, it):
            xb, Bx = xr[it % len(xr)]
            xn, Bxn = xnr[it % len(xnr)]
            stt_, Bst = statr[it % len(statr)]
            dma("sp", xb[0:rows, :], src_rows_ap, (), (Bx,))
            mset("dve", stt_[0:rows, :], 0.0, (Bst,))
            act(xn[0:rows, :], xb[0:rows, :], AF.Square, (Bx, Bst), (Bst, Bxn), accum=stt_[0:rows, 0:1])
            rstd_from_ss(stt_[0:rows, 0:1], D, (Bst,), (Bst,))
            stt("dve", xn[0:rows, :], xb[0:rows, :], stt_[0:rows, 0:1], gbc[0:rows, :], ALU.mult, ALU.mult,
                (Bx, Bst, Bg), (Bxn,))
            return xn, Bxn

        def norm_B(xn, Bxn, rows, hnT, BhnT, col0):
            for half in range(2):
                pb, Bp = nextbank()
                pbb = pb[:, :].bitcast(BF16)
                for i in range(8):
                    c = half * 8 + i
                    tr(pbb[:, i * 128:i * 128 + rows], xn[0:rows, c * 128:(c + 1) * 128], ident_b[0:rows, 0:rows],
                       (Bxn, Bcmb), (Bp,))
                src = pbb.rearrange("p (a b) -> p a b", a=8)[:, :, 0:rows]
                cp("act", hnT[:, half * 8:half * 8 + 8, col0:col0 + rows], src, (Bp,), (BhnT,))

        def norm_block(src_rows_ap, rows, gbc, Bg, xr, xnr, statr, hnT, BhnT, col0, it):
            xn, Bxn = norm_A(src_rows_ap, rows, gbc, Bg, xr, xnr, statr, it)
            norm_B(xn, Bxn, rows, hnT, BhnT, col0)

        m0 = A.mark()
        gbc = A.alloc([128, 2048], F32)
        Bg = Buf("gmix")
        dma("sp", gbc, g_mix.partition_broadcast(128), (), (Bg,))
        Wckv = A.alloc([128, 16, 512], BF16)
        Wki = A.alloc([128, 16, 128], BF16)
        Wkis = A.alloc([128, 16, 128], BF16)
        Wkb = A.alloc([128, 16, 1024], BF16)
        Wvb = A.alloc([128, 16, 1024], BF16)
        Wfb = A.alloc([128, 16, 8], BF16)
        Wkk = A.alloc([128, 4, 1024], BF16)
        Wkks = A.alloc([128, 4, 1024], BF16)
        Wkv = A.alloc([128, 4, 1024], BF16)
        BW = Buf("W0")

        def wv(c0, n):
            return w_in[:, c0:c0 + n].rearrange("(c p) n -> p c n", p=128)

        def wv1(c0, n, c):
            return w_in[c * 128:(c + 1) * 128, c0:c0 + n]

        BW2 = Buf("W0b")
        BW3 = Buf("W0c")
        BW4 = Buf("W0d")
        BW5 = Buf("W0e")
        BW6 = Buf("W0f")
        for c in range(16):
            dma("pool", Wckv[:, c, :], wv1(C_CKV, 512, c), (), (BW,), fresh=True)
            for d0 in (0, 64):
                dma("pool", Wki[:, c, d0:d0 + 64], wv1(C_KI, 64, c), (), (BW2,), fresh=True)
                dma("pool", Wkis[:, c, d0:d0 + 32], wv1(C_KI + 32, 32, c), (), (BW2,), fresh=True)
                dma("pool", Wkis[:, c, d0 + 32:d0 + 64], wv1(C_KI, 32, c), (), (BW2,), fresh=True)
            dma("pool", Wkb[:, c, :], wv1(C_KB, 1024, c), (), (BW3,), fresh=True)
            dma("pool", Wvb[:, c, :], wv1(C_VB, 1024, c), (), (BW4,), fresh=True)
            dma("pool", Wfb[:, c, :], wv1(C_FB, 8, c), (), (BW5,), fresh=True)
        for c in range(4):
            kvr = w_kvup[c * 128:(c + 1) * 128, :]
            dma("pool", Wkk[:, c, :], kvr[:, 0:1024], (), (BW6,), fresh=True)
            dma("pool", Wkv[:, c, :], kvr[:, 1024:2048], (), (BW6,), fresh=True)
            src = kvr[:, 0:1024].rearrange("p (h t j) -> p h t j", h=8, t=2)
            dst = Wkks[:, c, :].rearrange("p (h t j) -> p h t j", h=8, t=2)
            dma("pool", dst[:, :, 0, :], src[:, :, 1, :], (), (BW6,), fresh=True)
            dma("pool", dst[:, :, 1, :], src[:, :, 0, :], (), (BW6,), fresh=True)
        if stop_after == "w0":
            S.barrier()
            return _finish(nc, S, out_d)
        Wall = (BW, BW2, BW3, BW4, BW5, BW6)

        xr = ring(1, [128, 2048], F32, "x")
        xnr = ring(2, [128, 2048], BF16, "xn")
        statr = ring(4, [128, 8], F32, "st")
        hnr = ring(2, [128, 16, 256], BF16, "hn")
        tabr = ring(1, [128, 4, 256], F32, "tab")
        sqr = ring(1, [128, 4, 256], BF16, "sq")
        ckvTr = ring(1, [128, 4, 256], F32, "ckvT")
        rstdr = ring(2, [128, 256], F32, "rstd")
        ckvnr = ring(2, [128, 4, 256], BF16, "ckvn")
        t1r = ring(2, [128, 256], F32, "t1")
        t2r = ring(2, [128, 256], F32, "t2")
        kst = ring(4, [128, 256], BF16, "kst")
        vst = ring(3, [128, 1024], BF16, "vst")
        ksqr = ring(1, [128, 256], BF16, "ksq")
        kiAr = ring(1, [128, 256], F32, "kiA")
        kiBr = ring(1, [128, 256], F32, "kiB")
        lr = ring(2, [128, 8], F32, "l")
        zr = ring(2, [128, 8], F32, "z")

        cnt = {"blk": 0, "k": 0, "v": 0, "t": 0}
        groups = [(0, 16)] + [(16 + 256 * g, 256) for g in range(32)]
        groups = groups[:1 + 8 * npairs]
        if stop_after == "p0small":
            groups = groups[1:3] if only and "nometa" in only else groups[:3]
        pendB = []

        def do_norm_A(gi_):
            p0_, T_ = groups[gi_]
            hnT_, BhnT_ = hnr[gi_ % 2]
            for j_ in range(1 if T_ == 16 else 2):
                rows_ = 16 if T_ == 16 else 128
                xn_, Bxn_ = norm_A(xall[p0_ + 128 * j_:p0_ + 128 * j_ + rows_, :], rows_, gbc, Bg, xr, xnr, statr,
                                   cnt["blk"])
                cnt["blk"] += 1
                pendB.append((xn_, Bxn_, rows_, hnT_, BhnT_, 128 * j_))

        def do_norm_B():
            while pendB:
                norm_B(*pendB.pop(0))

        deferred = []
        do_norm_A(0)
        do_norm_B()
        for gi, (p0, T) in enumerate(groups):
            hnT, BhnT = hnr[gi % 2]
            tab, Btab = tabr[0]
            nblk = 1 if T == 16 else 2
            for ti, tsrc in enumerate((cosA_all, sinA_all, cosI_all, sinI_all)):
                dma("sp", tab[:, ti, 0:T], tsrc[:, p0:p0 + T], (), (Btab,))
            sq, Bsq = sqr[0]
            ckvT, BckvT = ckvTr[0]
            rs, Brs = rstdr[gi % 2]
            rs2, Brs2 = rstdr[(gi + 1) % 2]
            ckvn, Bckvn = ckvnr[gi % 2]
            for n in range(4):
                pb, Bp = nextbank()
                for c in range(16):
                    mm(pb[:, 0:T], Wckv[:, c, n * 128:(n + 1) * 128], hnT[:, c, 0:T], c == 0, c == 15,
                       (BW, BhnT), (Bp,))
                act(sq[:, n, 0:T], pb[:, 0:T], AF.Square, (Bp,), (Bsq,))
                cp("dve", ckvT[:, n, 0:T], pb[:, 0:T], (Bp,), (BckvT,))
            pki, Bpki = nextbank()
            pki2, Bpki2 = nextbank()
            for c in range(16):
                mm(pki[:, 0:T], Wki[:, c, :], hnT[:, c, 0:T], c == 0, c == 15, (BW2, BhnT), (Bpki,))
            for c in range(16):
                mm(pki2[:, 0:T], Wkis[:, c, :], hnT[:, c, 0:T], c == 0, c == 15, (BW2, BhnT), (Bpki2,))
            ksq, Bksq = ksqr[0]
            act(ksq[:, 0:T], pki[:, 0:T], AF.Square, (Bpki,), (Bksq,))
            kiA, BkiA = kiAr[0]
            kiB, BkiB = kiBr[0]
            ts("dve", kiA[:, 0:T], pki[:, 0:T], smc[:, 4:5], None, ALU.mult, None, (Bpki, Bsmc), (BkiA,))
            ts("dve", kiB[:, 0:T], pki2[:, 0:T], smc[:, 5:6], None, ALU.mult, None, (Bpki2, Bsmc), (BkiB,))
            for h in range(8):
                pb, Bp = nextbank()
                for c in range(16):
                    mm(pb[:, 0:T], Wkb[:, c, h * 128:(h + 1) * 128], hnT[:, c, 0:T], c == 0, c == 15,
                       (BW3, BhnT), (Bp,))
                ks, Bks = kst[cnt["k"] % 4]
                cnt["k"] += 1
                cp("act", ks[:, 0:T], pb[:, 0:T], (Bp,), (Bks,))
                dma("sp", KbT[h, :, p0:p0 + T], ks[:, 0:T], (Bks,), ())
                if h == 3:
                    pss, Bpss = nextbank()
                    for n in range(4):
                        mm(pss[:, 0:T], ones_b, sq[:, n, 0:T], n == 0, n == 3, (Bcmb, Bsq), (Bpss,))
                    mm(pss[:, 256:256 + T], blk64_b, ksq[:, 0:T], True, True, (Bcmb, Bksq), (Bpss,))
                    cp("dve", rs[:, 0:T], pss[:, 0:T], (Bpss,), (Brs,))
                    cp("dve", rs2[:, 0:T], pss[:, 256:256 + T], (Bpss,), (Brs2,))
                    rstd_from_ss(rs[:, 0:T], 512, (Brs,), (Brs,))
                    rstd_from_ss(rs2[:, 0:T], 64, (Brs2,), (Brs2,))
                    for n in range(4):
                        stt("dve", ckvn[:, n, 0:T], ckvT[:, n, 0:T], smc[:, n:n + 1], rs[:, 0:T], ALU.mult, ALU.mult,
                            (BckvT, Bsmc, Brs), (Bckvn,))
                    if gi + 1 < len(groups):
                        do_norm_A(gi + 1)
            for j in range(nblk):
                rows = 16 if T == 16 else 128
                b = 0 if T == 16 else 1 + 2 * (gi - 1) + j
                vs, Bvs = vst[cnt["v"] % 3]
                cnt["v"] += 1
                for half in range(2):
                    pb, Bp = nextbank()
                    for c in range(16):
                        mm(pb[0:rows, :], hnT[:, c, 128 * j:128 * j + rows], Wvb[:, c, half * 512:(half + 1) * 512],
                           c == 0, c == 15, (BW4, BhnT), (Bp,))
                    cp("act", vs[0:rows, half * 512:(half + 1) * 512], pb[0:rows, :], (Bp,), (Bvs,))
                dma("sp", Vb[p0 + 128 * j:p0 + 128 * j + rows, :], vs[0:rows, :], (Bvs,), ())
                pb, Bp = nextbank()
                for c in range(16):
                    mm(pb[0:rows, 0:8], hnT[:, c, 128 * j:128 * j + rows], Wfb[:, c, 0:8], c == 0, c == 15,
                       (BW5, BhnT), (Bp,))
                z, Bz = zr[b % 2]
                l, Bl = lr[b % 2]
                tt("dve", z[0:rows, :], pb[0:rows, 0:8], smc[0:rows, 8:16], ALU.add, (Bp, Bsmc), (Bz,))
                act(z[0:rows, :], z[0:rows, :], AF.Exp, (Bz,), (Bz,), scale=-1.0)
                act(l[0:rows, :], z[0:rows, :], AF.Ln, (Bz,), (Bl,), bias=1.0)
                def cums(rows=rows, l=l, Bl=Bl, b=b):
                    pb, Bp = nextbank()
                    mm(pb[0:rows, 0:8], triU_f[0:rows, 0:rows], l[0:rows, :], True, True, (Bcm, Bl), (Bp,))
                    mm(pb[:, 8:16], ones_f[0:rows, :], l[0:rows, :], True, True, (Bcm, Bl), (Bp,))
                    tt("dve", Lall[0:rows, :, b], pb[0:rows, 0:8], Tall[0:rows, :, b], ALU.add, (Bp, BT), (BL,))
                    tt("dve", Tall[:, :, b + 1], pb[:, 8:16], Tall[:, :, b], ALU.add, (Bp, BT), (BT,))

                deferred.append(cums)
            do_norm_B()
            tt("pool", kiA[:, 0:T], kiA[:, 0:T], rs2[:, 0:T], ALU.mult, (BkiA, Brs2), (BkiA,))
            tt("pool", kiB[:, 0:T], kiB[:, 0:T], rs2[:, 0:T], ALU.mult, (BkiB, Brs2), (BkiB,))
            tt("pool", kiA[:, 0:T], kiA[:, 0:T], tab[:, 2, 0:T], ALU.mult, (BkiA, Btab), (BkiA,))
            tt("pool", kiB[:, 0:T], kiB[:, 0:T], tab[:, 3, 0:T], ALU.mult, (BkiB, Btab), (BkiB,))
            ks, Bks = kst[cnt["k"] % 4]
            cnt["k"] += 1
            tt("pool", ks[:, 0:T], kiA[:, 0:T], kiB[:, 0:T], ALU.add, (BkiA, BkiB), (Bks,))
            dma("sp", KiT_d[:, p0:p0 + T], ks[:, 0:T], (Bks,), ())
            for h in range(8):
                pb, Bp = nextbank()
                pb2, Bp2 = nextbank()
                for c in range(4):
                    mm(pb[:, 0:T], Wkk[:, c, h * 128:(h + 1) * 128], ckvn[:, c, 0:T], c == 0, c == 3,
                       (BW6, Bckvn), (Bp,))
                for c in range(4):
                    mm(pb2[:, 0:T], Wkks[:, c, h * 128:(h + 1) * 128], ckvn[:, c, 0:T], c == 0, c == 3,
                       (BW6, Bckvn), (Bp2,))
                t1, Bt1 = t1r[cnt["t"] % 2]
                t2, Bt2 = t2r[cnt["t"] % 2]
                cnt["t"] += 1
                tt("dve", t1[:, 0:T], pb[:, 0:T], tab[:, 0, 0:T], ALU.mult, (Bp, Btab), (Bt1,))
                tt("dve", t2[:, 0:T], pb2[:, 0:T], tab[:, 1, 0:T], ALU.mult, (Bp2, Btab), (Bt2,))
                ks, Bks = kst[cnt["k"] % 4]
                cnt["k"] += 1
                tt("pool", ks[:, 0:T], t1[:, 0:T], t2[:, 0:T], ALU.add, (Bt1, Bt2), (Bks,))
                dma("sp", KaT[h, :, p0:p0 + T], ks[:, 0:T], (Bks,), ())
            for j in range(nblk):
                rows = 16 if T == 16 else 128
                vs, Bvs = vst[cnt["v"] % 3]
                cnt["v"] += 1
                for half in range(2):
                    pb, Bp = nextbank()
                    for c in range(4):
                        mm(pb[0:rows, :], ckvn[:, c, 128 * j:128 * j + rows], Wkv[:, c, half * 512:(half + 1) * 512],
                           c == 0, c == 3, (BW6, Bckvn), (Bp,))
                    cp("act", vs[0:rows, half * 512:(half + 1) * 512], pb[0:rows, :], (Bp,), (Bvs,))
                dma("sp", Va[p0 + 128 * j:p0 + 128 * j + rows, :], vs[0:rows, :], (Bvs,), ())
            while deferred:
                deferred.pop(0)()
        if debug:
            dma("sp", dbg["d_L"], Lall.rearrange("p a b -> p (a b)"), (BL,), ())
        S.barrier()
        A.release(m0)
        if stop_after in ("p0", "p0small"):
            return _finish(nc, S, out_d)
        NP_ = npairs
        NS = 2 * NP_
        NT = 128 * NS
        TG = min(512, NT)
        NTG = NT // TG

        def mkap(base, off_elems, dims):
            pstride = base.ap[0][0]
            return bass.AP(base.tensor, base.offset + off_elems, [(pstride, dims[0])] + [tuple(d) for d in dims[1:]])

        yaT = A.alloc([128, 8, NT], BF16)
        ByaT = Buf("yaT")
        mq = A.mark()
        gbc = A.alloc([128, 2048], F32)
        Bg = Buf("gmix2")
        dma("sp", gbc, g_mix.partition_broadcast(128), (), (Bg,))
        hnO = A.alloc([128, 16, NT], BF16)
        BhnO = Buf("hnO")
        xr = ring(2, [128, 2048], F32, "qx")
        xnr = ring(2, [128, 2048], BF16, "qxn")
        statr = ring(4, [128, 8], F32, "qst")
        for s_ in range(NS):
            norm_block(xown[128 * s_:128 * s_ + 128, :], 128, gbc, Bg, xr, xnr, statr, hnO, BhnO, 128 * s_, s_)
        dma("sp", HnO[:, :, 0:NT], hnO, (BhnO,), ())
        tabq = A.alloc([128, 4, NT], F32)
        Btabq = Buf("tabq")
        for ti, tsrc in enumerate((cosA_own, sinA_own, cosI_own, sinI_own)):
            dma("sp", tabq[:, ti, :], tsrc[:, 0:NT], (), (Btabq,), fresh=True)
        wstage_alloc(3, ("dve",))
        wr = ring(3, [128, 16, 128], BF16, "qw")
        wsr = ring(3, [128, 16, 128], BF16, "qws")
        qst = ring(3, [128, NT], BF16, "qstage")
        t1r = ring(2, [128, TG], F32, "qt1")
        t2r = ring(2, [128, TG], F32, "qt2")
        specs = [("qa", h, C_QA + 128 * h, QaT) for h in range(8)] + \
                [("qi", t, C_QI + 128 * t, QiT) for t in range(8)] + \
                [("qb", h, C_QB + 128 * h, QbT) for h in range(8)]
        def q_load(i):
            kind, idx, col0, dst = specs[i]
            W, BWt = wr[i % 3]
            Ws, BWs = wsr[i % 3]
            wsrc = w_in[:, col0:col0 + 128].rearrange("(c p) n -> p c n", p=128)
            wload(W, wsrc, BWt)
            if kind != "qb":
                hw = 64 if kind == "qa" else 32
                for j in range(128 // hw):
                    jo = j ^ 1
                    wload(Ws[:, :, j * hw:(j + 1) * hw], wsrc[:, :, jo * hw:(jo + 1) * hw], BWs)

        q_load(0)
        q_load(1)
        for i, (kind, idx, col0, dst) in enumerate(specs):
            W, BWt = wr[i % 3]
            Ws, BWs = wsr[i % 3]
            if i + 2 < len(specs):
                q_load(i + 2)
            st_, Bst_ = qst[i % 3]
            for tg in range(NTG):
                cols = slice(tg * TG, (tg + 1) * TG)
                pb, Bp = nextbank()
                for c in range(16):
                    mm(pb[:, 0:TG], W[:, c, :], hnO[:, c, cols], c == 0, c == 15, (BWt, BhnO), (Bp,))
                if kind == "qb":
                    act(st_[:, cols], pb[:, 0:TG], AF.Copy, (Bp,), (Bst_,), scale=float(128 ** -0.5))
                else:
                    pb2, Bp2 = nextbank()
                    for c in range(16):
                        mm(pb2[:, 0:TG], Ws[:, c, :], hnO[:, c, cols], c == 0, c == 15, (BWs, BhnO), (Bp2,))
                    ci = 0 if kind == "qa" else 2
                    t1, Bt1 = t1r[tg % 2]
                    t2, Bt2 = t2r[tg % 2]
                    tt("dve", t1, pb[:, 0:TG], tabq[:, ci, cols], ALU.mult, (Bp, Btabq), (Bt1,))
                    tt("dve", t2, pb2[:, 0:TG], tabq[:, ci + 1, cols], ALU.mult, (Bp2, Btabq), (Bt2,))
                    tt("pool", st_[:, cols], t1, t2, ALU.add, (Bt1, Bt2), (Bst_,))
            dma("sp", dst[idx, :, 0:NT], st_, (Bst_,), ())
        Wwi = A.alloc([128, 16, 16], BF16)
        BWwi = Buf("Wwi")
        dma("pool", Wwi, w_in[:, C_WI:C_WI + 16].rearrange("(c p) n -> p c n", p=128), (), (BWwi,))
        wtmp = A.alloc([128, 16], F32)
        Bwtmp = Buf("wtmp")
        for s_ in range(NS):
            pb, Bp = nextbank()
            for c in range(16):
                mm(pb[:, 0:16], hnO[:, c, 128 * s_:128 * s_ + 128], Wwi[:, c, :], c == 0, c == 15, (BWwi, BhnO), (Bp,))
            ts("dve", wtmp, pb[:, 0:16], 0.25 * 0.125, None, ALU.mult, None, (Bp,), (Bwtmp,))
            ts("dve", wisgn[:, s_, :], wtmp, 0.0, 2.0, ALU.is_ge, ALU.mult, (Bwtmp,), (Bwi,))
            ts("dve", wisgn[:, s_, :], wisgn[:, s_, :], -1.0, None, ALU.add, None, (Bwi,), (Bwi,))
            tt("dve", wiabs[:, s_, :], wtmp, wisgn[:, s_, :], ALU.mult, (Bwtmp, Bwi), (Bwi,))
        if debug:
            pass
        S.barrier()
        A.release(mq)
        if stop_after == "q":
            return _finish(nc, S, out_d)

        def kb_range(kb):
            return (0, 16) if kb == 0 else (16 + 128 * (kb - 1), 128)

        LOOK = 3
        pending_fin = []

        prefetched = {}

        def attention(m, h, mixer, Qp, BQp, KT_d, V_d, Kr, Vr, PTr, onr, rsr, yT, ByT, cnt,
                      selT=None, BselT=None, maskT_sb=None, Bmask=None, biasAB=None, Bbias=None, nxt=None):
            NKB = 16 * m + 17
            par = cnt["o"] % 2
            cnt["o"] += 1
            oA, BoA = banks[4 + 2 * par], Bbank[4 + 2 * par]
            oB, BoB = banks[5 + 2 * par], Bbank[5 + 2 * par]
            chunks = {}
            nch_tot = (NKB + 15) // 16

            def load_chunk(ch, mm_=m, hh_=h, NKB_=NKB):
                key = (mixer, mm_, hh_, ch)
                if key in prefetched:
                    chunks[ch] = prefetched.pop(key)
                    return
                NKBx = NKB_
                kb0 = 16 * ch
                kb1 = min(NKBx, kb0 + 16)
                klo = kb_range(kb0)[0]
                khi = kb_range(kb1 - 1)[0] + kb_range(kb1 - 1)[1]
                Kc, BKc = Kr[cnt["kv"] % len(Kr)]
                Vc, BVc = Vr[cnt["kv"] % len(Vr)]
                cnt["kv"] += 1
                dma("sp", Kc[:, 0:khi - klo], KT_d[hh_, :, klo:khi], (), (BKc,))
                j0 = 0
                if kb0 == 0:
                    dma("sp", Vc[0:16, 0, 0:128], V_d[0:16, hh_ * 128:(hh_ + 1) * 128], (), (BVc,))
                    j0 = 1
                nreal = (kb1 - kb0) - j0
                r0 = kb_range(kb0 + j0)[0]
                dma("sp", Vc[:, j0:j0 + nreal, 0:128],
                    V_d[r0:r0 + 128 * nreal, hh_ * 128:(hh_ + 1) * 128].rearrange("(j p) d -> p j d", p=128),
                    (), (BVc,), fresh=(j0 == 1))
                rec = (Kc, BKc, Vc, BVc, klo, kb0)
                if (mm_, hh_) == (m, h):
                    chunks[ch] = rec
                else:
                    prefetched[key] = rec

            pts = {}

            def emit_S(kb):
                ch = kb // 16
                if ch not in chunks:
                    load_chunk(ch)
                if kb % 16 == 0:
                    if ch + 1 < nch_tot:
                        if (ch + 1) not in chunks:
                            load_chunk(ch + 1)
                    elif nxt is not None:
                        load_chunk(0, nxt[0], nxt[1], 16 * nxt[0] + 17)
                Kc, BKc, Vc, BVc, klo, kb0 = chunks[ch]
                k0, ksz = kb_range(kb)
                pb, Bp = nextbank()
                mm(pb[0:ksz, 0:256], Kc[:, k0 - klo:k0 - klo + ksz], Qp[:, h, :], True, True, (BKc, BQp), (Bp,))
                PT, BPT = PTr[cnt["pt"] % len(PTr)]
                cnt["pt"] += 1
                if mixer == "a":
                    act(PT[0:ksz, :], pb[0:ksz, 0:256], AF.Exp, (Bp,), (BPT,))
                    tt("pool" if kb % 3 == 2 else "dve", PT[0:ksz, :], PT[0:ksz, :], selT[0:ksz, kb, :], ALU.mult,
                       (BPT, BselT), (BPT,))
                else:
                    for half in range(2):
                        act(PT[0:ksz, half * 128:(half + 1) * 128], pb[0:ksz, half * 128:(half + 1) * 128], AF.Exp,
                            (Bp, Bbias), (BPT,), bias=biasAB[half][0:ksz, kb:kb + 1])
                    t = kb - (NKB - 16)
                    if t >= 0:
                        tt("dve", PT[0:ksz, :], PT[0:ksz, :], maskT_sb[0:ksz, t, :], ALU.mult, (BPT, Bmask), (BPT,))
                pts[kb] = (PT, BPT)

            def emit_PV(kb):
                ch = kb // 16
                Kc, BKc, Vc, BVc, klo, kb0 = chunks[ch]
                k0, ksz = kb_range(kb)
                j = kb - kb0
                PT, BPT = pts.pop(kb)
                mm(oA[:, 0:129], PT[0:ksz, 0:128], Vc[0:ksz, j, :], kb == 0, kb == NKB - 1, (BPT, BVc), (BoA,))
                mm(oB[:, 0:129], PT[0:ksz, 128:256], Vc[0:ksz, j, :], kb == 0, kb == NKB - 1, (BPT, BVc), (BoB,))

            for kb in range(min(LOOK, NKB)):
                emit_S(kb)
            while pending_fin:
                pending_fin.pop(0)()
            for kb in range(NKB):
                emit_PV(kb)
                if kb + LOOK < NKB:
                    emit_S(kb + LOOK)

            def fin():
                for half, (o_, Bo_) in enumerate(((oA, BoA), (oB, BoB))):
                    s_ = 2 * m + half
                    rs_, Brs_ = rsr[cnt["fin"] % len(rsr)]
                    on_, Bon_ = onr[cnt["fin"] % len(onr)]
                    cnt["fin"] += 1
                    recip(rs_[:, 0:1], o_[:, 128:129], (Bo_,), (Brs_,))
                    ts("dve", on_, o_[:, 0:128], rs_[:, 0:1], None, ALU.mult, None, (Bo_, Brs_), (Bon_,))
                    pb, Bp = nextbank()
                    pbb = pb[:, :].bitcast(BF16)
                    tr(pbb[:, 0:128], on_, ident_b, (Bon_, Bcmb), (Bp,))
                    cp("act", yT[:, h, 128 * s_:128 * s_ + 128], pbb[:, 0:128], (Bp,), (ByT,))

            pending_fin.append(fin)

        def flush_fin():
            while pending_fin:
                pending_fin.pop(0)()

        def kv_rings():
            Kr = ring(3, [128, 2048], BF16, "Kc")
            Vr = ring(3, [128, 16, 129], BF16, "Vc")
            for Vc, BVc in Vr:
                mset("pool", Vc[:, :, 128:129], 1.0, (BVc,))
            PTr = ring(5, [128, 256], BF16, "PT")
            onr = ring(4, [128, 128], BF16, "on")
            rsr = ring(4, [128, 8], F32, "rs")
            return Kr, Vr, PTr, onr, rsr

        md = A.mark()
        setbanks(0, 4)
        KiT_sb = A.alloc([128, L], BF16)
        BKi = Buf("KiT_sb")
        NKTOT = 16 + 128 * 16 * NP_
        dma("sp", KiT_sb[:, 0:NKTOT], KiT_d[:, 0:NKTOT], (), (BKi,))
        maskT_sb = A.alloc([128, 16, 256], BF16)
        Bmask = Buf("maskT")
        dma("pool", maskT_sb, maskT_d.rearrange("p (t q) -> p t q", t=16), (), (Bmask,))
        negb_sb = A.alloc([128, 2, 2048], F32)
        Bnegb = Buf("negb")
        dma("sp", negb_sb, negb_d.rearrange("p (a k) -> p a k", a=2), (), (Bnegb,))
        score = A.alloc([128, L], F32)
        Bscore = Buf("score")
        sel = A.alloc([128, L], BF16)
        Bsel = Buf("sel")
        selT = A.alloc([128, 65, 256], BF16)
        BselT = Buf("selT")
        rr_ = ring(6, [128, 512], F32, "relu")
        Qi_p = A.alloc([128, 8, 256], BF16)
        BQi = Buf("Qi_p")
        Qa_p = A.alloc([128, 8, 256], BF16)
        BQa = Buf("Qa_p")
        bst = A.alloc([128, 16], F32)
        Bbst = Buf("bst")
        btab = A.alloc([128, 16], F32)
        bcnt_t = A.alloc([128, 16], F32)
        Bbtab = Buf("btab")
        cntp = A.alloc([128, 8], F32)[:, 0:1]
        Bcntp = Buf("cntp")
        Bsel2 = Buf("sel2")
        Kr, Vr, PTr, onr, rsr = kv_rings()
        acnt = {"o": 0, "kv": 0, "pt": 0, "fin": 0}
        for m in range(NP_):
            NKB = 16 * m + 17
            NK = 16 + 128 * (NKB - 1)
            flush_fin()
            dma("sp", Qi_p, QiT[:, :, 256 * m:256 * m + 256].rearrange("t p q -> p t q"), (), (BQi,))
            dma("sp", Qa_p, QaT[:, :, 256 * m:256 * m + 256].rearrange("h p q -> p h q"), (), (BQa,))
            for half in range(2):
                s_ = 2 * m + half
                nchunk = (NK + 511) // 512
                Bscs = [Buf("sc%d" % i) for i in range(nchunk)]
                for b_ in Bscs:
                    b_.writers = list(Bscore.writers)
                    b_.readers = list(Bscore.readers)
                for h in range(16):
                    po = (h % 2) * 64
                    for ch in range(nchunk):
                        k0 = ch * 512
                        kw = min(512, NK - k0)
                        pb, Bp = nextbank()
                        mm(pb[:, 0:kw], Qi_p[po:po + 64, h // 2, half * 128:(half + 1) * 128],
                           KiT_sb[po:po + 64, k0:k0 + kw], True, True, (BQi, BKi), (Bp,))
                        r_, Br_ = rr_[(h * nchunk + ch) % 6]
                        act(r_[:, 0:kw], pb[:, 0:kw], AF.Relu, (Bp, Bwi), (Br_,), scale=wiabs[:, s_, h:h + 1])
                        Bsc = Bscs[ch]
                        if h == 0:
                            act(score[:, k0:k0 + kw], r_[:, 0:kw], AF.Copy, (Br_, Bwi), (Bsc,), scale=wisgn[:, s_, h:h + 1])
                        else:
                            stt("dve", score[:, k0:k0 + kw], r_[:, 0:kw], wisgn[:, s_, h:h + 1], score[:, k0:k0 + kw],
                                ALU.mult, ALU.add, (Br_, Bwi, Bsc), (Bsc,))
                Bscore.writers = [ev for b_ in Bscs for ev in b_.writers]
                Bscore.readers = [ev for b_ in Bscs for ev in b_.readers]
                if debug and m == 0 and half == 0:
                    dma("sp", dbg["d_score"][:, 0:NK], score[:, 0:NK], (Bscore,), ())
                red("dve", bst[:, 0:1], score[:, 0:NK], ALU.min, (Bscore,), (Bbst,))
                red("dve", bst[:, 1:2], score[:, 0:NK], ALU.max, (Bscore,), (Bbst,))
                tt("dve", score[:, NK - 2048:NK], score[:, NK - 2048:NK], negb_sb[:, half, :], ALU.add,
                   (Bscore, Bnegb), (Bscore,))
                lo, hi, mid, inc = (bst[:, i:i + 1] for i in range(4))
                tt("dve", hi, hi, lo, ALU.subtract, (Bbst,), (Bbst,))
                ts("dve", btab[:, 0:NBISECT], smc[:, 36:36 + NBISECT], hi, None, ALU.mult, None, (Bsmc, Bbst), (Bbtab,))
                mset("dve", bcnt_t, 0.0, (Bbtab,))
                tt("dve", mid, lo, btab[:, 0:1], ALU.add, (Bbst, Bbtab), (Bbst,))
                for it in range(NBISECT):
                    ts("dve", sel[:, 0:NK], score[:, 0:NK], mid, 0.0, ALU.is_ge, ALU.add, (Bscore, Bbst, Bsel), (Bsel, Bbtab),
                       accum=bcnt_t[:, it:it + 1])
                    stt("dve", inc, bcnt_t[:, it:it + 1], float(TOPK) - 0.5, btab[:, it:it + 1], ALU.is_ge, ALU.mult,
                        (Bbtab,), (Bbst,))
                    nx_ = it + 1 if it + 1 < NBISECT else it
                    dst_ = mid if it + 1 < NBISECT else lo
                    stt("dve", dst_, inc, btab[:, nx_:nx_ + 1], mid, ALU.subtract, ALU.add, (Bbst, Bbtab), (Bbst,))
                stt("dve", lo, btab[:, NBISECT - 1:NBISECT], -0.0625, lo, ALU.mult, ALU.add, (Bbtab, Bbst), (Bbst,))
                ts("dve", sel[:, 0:NK], score[:, 0:NK], lo, None, ALU.is_ge, None, (Bscore, Bbst, Bsel, Bsel2), (Bsel, Bsel2))
                if debug:
                    dma("sp", dbg["d_thr"][s_:s_ + 1, :].rearrange("a p -> p a"), lo, (Bbst,), ())
                pb, Bp = nextbank()
                pbb = pb[:, :].bitcast(BF16)
                tr(pbb[0:16, 0:128], sel[:, 0:16], ident_b, (Bsel, Bcmb), (Bp,))
                cp("act", selT[0:16, 0, half * 128:(half + 1) * 128], pbb[0:16, 0:128], (Bp,), (BselT,))
                for g8 in range((NKB - 1) // 8):
                    pb, Bp = nextbank()
                    pbb = pb[:, :].bitcast(BF16)
                    for i in range(8):
                        kb = 1 + 8 * g8 + i
                        k0 = 16 + 128 * (kb - 1)
                        tr(pbb[:, i * 128:(i + 1) * 128], sel[:, k0:k0 + 128], ident_b, (Bsel, Bcmb), (Bp,))
                    cp("act", selT[:, 1 + 8 * g8:9 + 8 * g8, half * 128:(half + 1) * 128],
                       pbb.rearrange("p (a b) -> p a b", a=8), (Bp,), (BselT,))
            for h in range(8):
                attention(m, h, "a", Qa_p, BQa, KaT, Va, Kr, Vr, PTr, onr, rsr, yaT, ByaT, acnt,
                          selT=selT, BselT=BselT, nxt=((m, h + 1) if h < 7 else None))
        flush_fin()
        if debug:
            dma("pool", dbg["d_ya"][:, 0:8 * NT], yaT.rearrange("p a b -> p (a b)"), (ByaT,), ())
        S.barrier()
        A.release(md)
        if stop_after == "dsa":
            return _finish(nc, S, out_d)

        ybT = A.alloc([128, 8, NT], BF16)
        BybT = Buf("ybT")
        mf = A.mark()
        maskT_sb = A.alloc([128, 16, 256], BF16)
        Bmask = Buf("maskT2")
        dma("pool", maskT_sb, maskT_d.rearrange("p (t q) -> p t q", t=16), (), (Bmask,))
        oh_sb = A.alloc([128, 8, 66], F32)
        Boh = Buf("oh")
        dma("sp", oh_sb, ohsel_d.rearrange("p (s b) -> p s b", s=8), (), (Boh,))
        Lref = A.alloc([128, 8, 8], F32)
        BLref = Buf("Lref")
        tmpL = A.alloc([128, 8, 66], F32)
        BtmpL = Buf("tmpL")
        for s_ in range(NS):
            ohb = mkap(oh_sb, s_ * 66, [128, (0, 8), (1, 66)])
            tt("dve", tmpL, Tall, ohb, ALU.mult, (BT, Boh), (BtmpL,))
            red("dve", Lref[:, s_, :], tmpL, ALU.add, (BtmpL,), (BLref,))
        Qb_p = A.alloc([128, 8, 256], BF16)
        BQb = Buf("Qb_p")
        biasr = [ring(2, [128, 66], F32, "biasA"), ring(2, [128, 66], F32, "biasB")]
        Kr, Vr, PTr, onr, rsr = kv_rings()
        bcnt = {"o": 0, "kv": 0, "pt": 0, "fin": 0}
        it_ = 0
        for m in range(NP_):
            dma("sp", Qb_p, QbT[:, :, 256 * m:256 * m + 256].rearrange("h p q -> p h q"), (), (BQb,))
            for h in range(8):
                bA, BbA = biasr[0][it_ % 2]
                bB, BbB = biasr[1][it_ % 2]
                it_ += 1
                for half, (bt, Bbt) in enumerate(((bA, BbA), (bB, BbB))):
                    s_ = 2 * m + half
                    ts("dve", bt, Lall[:, h, :], Lref[:, s_, h:h + 1], 60.0, ALU.subtract, ALU.min, (BL, BLref), (Bbt,))
                Bbias = Buf("biasjoin")
                Bbias.writers = list(BbA.writers) + list(BbB.writers)
                nx = (m, h + 1) if h < 7 else ((m + 1, 0) if m + 1 < NP_ else None)
                attention(m, h, "b", Qb_p, BQb, KbT, Vb, Kr, Vr, PTr, onr, rsr, ybT, BybT, bcnt,
                          maskT_sb=maskT_sb, Bmask=Bmask, biasAB=(bA, bB), Bbias=Bbias, nxt=nx)
                for ev in Bbias.readers:
                    BbA.readers.append(ev)
                    BbB.readers.append(ev)
        flush_fin()
        if debug:
            dma("pool", dbg["d_yb"][:, 0:8 * NT], ybT.rearrange("p a b -> p (a b)"), (BybT,), ())
        S.barrier()
        A.release(mf)
        setbanks(0, 8)
        if stop_after == "fox":
            return _finish(nc, S, out_d)

        mergedT = A.alloc_top([128, 16, NT], BF16)
        Bmerged = Buf("merged")
        mm_ = A.mark()
        hnO = A.alloc([128, 16, NT], BF16)
        BhnO = Buf("hnO2")
        dma("sp", hnO, HnO[:, :, 0:NT], (), (BhnO,))
        wstage_alloc(3, ("dve", "act"))
        gwr = ring(4, [128, 16, 128], BF16, "gw")
        bwr = ring(4, [128, 8, 128], BF16, "bw")
        sgr = ring(2, [128, TG], F32, "sg")
        m1r = ring(2, [128, TG], F32, "m1")
        for n in range(16):
            Wga, BWga = gwr[(2 * n) % 4]
            Wgb, BWgb = gwr[(2 * n + 1) % 4]
            Wba, BWba = bwr[(2 * n) % 4]
            Wbb, BWbb = bwr[(2 * n + 1) % 4]
            wload(Wga, w_in[:, C_GA + 128 * n:C_GA + 128 * n + 128].rearrange("(c p) n -> p c n", p=128), BWga)
            wload(Wgb, w_in[:, C_GB + 128 * n:C_GB + 128 * n + 128].rearrange("(c p) n -> p c n", p=128), BWgb)
            wload(Wba, w_bra[:, 128 * n:128 * n + 128].rearrange("(c p) n -> p c n", p=128), BWba)
            wload(Wbb, w_brb[:, 128 * n:128 * n + 128].rearrange("(c p) n -> p c n", p=128), BWbb)
            for tg in range(NTG):
                cols = slice(tg * TG, (tg + 1) * TG)
                res = []
                for (Wg, BWg, Wb, BWb, yT_, ByT_) in ((Wga, BWga, Wba, BWba, yaT, ByaT), (Wgb, BWgb, Wbb, BWbb, ybT, BybT)):
                    pg, Bpg = nextbank()
                    for c in range(16):
                        mm(pg[:, 0:TG], Wg[:, c, :], hnO[:, c, cols], c == 0, c == 15, (BWg, BhnO), (Bpg,))
                    pbr, Bpbr = nextbank()
                    for j in range(8):
                        mm(pbr[:, 0:TG], Wb[:, j, :], yT_[:, j, cols], j == 0, j == 7, (BWb, ByT_), (Bpbr,))
                    sg, Bsg = sgr[len(res) % 2]
                    m1, Bm1 = m1r[len(res) % 2]
                    act(sg, pg[:, 0:TG], AF.Sigmoid, (Bpg,), (Bsg,))
                    tt("dve", m1, sg, pbr[:, 0:TG], ALU.mult, (Bsg, Bpbr), (Bm1,))
                    res.append((m1, Bm1))
                tt("pool", mergedT[:, n, cols], res[0][0], res[1][0], ALU.add, (res[0][1], res[1][1]), (Bmerged,))
        S.barrier()
        A.release(persist_mark)
        if stop_after == "mergea":
            return _finish(nc, S, out_d)
        h2 = A.alloc([128, NS, 2048], F32)
        Bh2 = [Buf("h2_%d" % i) for i in range(NS)]
        mo = A.mark()
        for s_ in range(NS):
            dma("sp", h2[:, s_, :], xown[128 * s_:128 * s_ + 128, :], (), (Bh2[s_],))
        wstage_alloc(3, ("dve", "act", "pool"))
        wor = ring(2, [128, 16, 512], BF16, "wo")
        for cg in range(4):
            Wo, BWo = wor[cg % 2]
            wload(Wo, w_out[:, cg * 512:(cg + 1) * 512].rearrange("(c p) n -> p c n", p=128), BWo)
            for s_ in range(NS):
                pb, Bp = nextbank()
                for c in range(16):
                    mm(pb[:, :], mergedT[:, c, 128 * s_:128 * s_ + 128], Wo[:, c, :], c == 0, c == 15, (Bmerged, BWo), (Bp,))
                tt("dve", h2[:, s_, cg * 512:(cg + 1) * 512], h2[:, s_, cg * 512:(cg + 1) * 512], pb[:, :], ALU.add,
                   (Bp, Bh2[s_]), (Bh2[s_],))
        if debug:
            for s_ in range(NS):
                dma("sp", dbg["d_h2"][128 * s_:128 * s_ + 128, :], h2[:, s_, :], (Bh2[s_],), ())
        S.barrier()
        A.release(mo)
        A.limit = A.full
        if stop_after == "merge":
            return _finish(nc, S, out_d)

        tT = A.alloc([128, 16, NT], BF16)
        BtT = Buf("tT")
        gates = A.alloc([128, NS, 16], F32)
        Bgates = Buf("gates")
        mp = A.mark()
        gbc = A.alloc([128, 2048], F32)
        Bg = Buf("gffn")
        dma("sp", gbc, g_ffn.partition_broadcast(128), (), (Bg,))
        Wge = A.alloc([128, 16, 20], F32)
        BWge = Buf("Wge")
        dma("sp", Wge, w_ge.rearrange("(c p) n -> p c n", p=128), (), (BWge,))
        tnr = ring(1, [128, 2048], F32, "tn")
        t32r = ring(1, [128, 16, 128], F32, "t32")
        statr = ring(2, [128, 8], F32, "mst")
        def route_math(rt, R_, s_):
            gmax, ngmax, sume, pgrp = rt[:, 20:21], rt[:, 21:22], rt[:, 22:23], rt[:, 23:24]
            ohg, eg = rt[:, 24:28], rt[:, 28:32]
            red("dve", gmax, rt[:, 0:4], ALU.max, R_, R_)
            ts("dve", ohg, rt[:, 0:4], gmax, None, ALU.is_ge, None, R_, R_)
            ts("dve", ngmax, gmax, -1.0, None, ALU.mult, None, R_, R_)
            mset("dve", sume, 0.0, R_)
            act(eg, rt[:, 0:4], AF.Exp, R_, R_, bias=ngmax, accum=sume)
            recip(pgrp, sume, R_, R_)
            tmp16 = rt[:, 32:48]
            tt("dve", tmp16.rearrange("p (g j) -> p g j", g=4), rt[:, 4:20].rearrange("p (g j) -> p g j", g=4),
               mkap(rt, 24, [128, (1, 4), (0, 4)]), ALU.mult, R_, R_)
            esel = rt[:, 48:52]
            red("dve", esel, mkap(rt, 32, [128, (1, 4), (4, 4)]), ALU.add, R_, R_)
            m1_, mk1, e2_, m2_, mk2 = rt[:, 52:53], rt[:, 53:57], rt[:, 57:61], rt[:, 61:62], rt[:, 0:4]
            red("dve", m1_, esel, ALU.max, R_, R_)
            ts("dve", mk1, esel, m1_, None, ALU.is_ge, None, R_, R_)
            stt("dve", e2_, mk1, -1e30, esel, ALU.mult, ALU.add, R_, R_)
            red("dve", m2_, e2_, ALU.max, R_, R_)
            ts("dve", mk2, e2_, m2_, None, ALU.is_ge, None, R_, R_)
            dd_, ed_, p1_, p2_ = rt[:, 4:5], rt[:, 5:6], rt[:, 6:7], rt[:, 7:8]
            tt("dve", dd_, m2_, m1_, ALU.subtract, R_, R_)
            act(ed_, dd_, AF.Exp, R_, R_)
            ts("dve", p1_, ed_, 1.0, None, ALU.add, None, R_, R_)
            recip(p1_, p1_, R_, R_)
            tt("dve", p2_, ed_, p1_, ALU.mult, R_, R_)
            tt("dve", p1_, p1_, pgrp, ALU.mult, R_, R_)
            tt("dve", p2_, p2_, pgrp, ALU.mult, R_, R_)
            ge_ = rt[:, 8:12]
            ts("dve", ge_, mk1, p1_, None, ALU.mult, None, R_, R_)
            stt("dve", ge_, mk2, p2_, ge_, ALU.mult, ALU.add, R_, R_)
            for g in range(4):
                ts("dve", gates[:, s_, 4 * g:4 * g + 4], ge_, rt[:, 24 + g:25 + g], None, ALU.mult, None, R_, (Bgates,))

        rtr = ring(2, [128, 64], F32, "rt")
        tnr = ring(2, [128, 2048], F32, "tn2") if A.limit - A.off > 24 * 1024 else tnr
        route_q = []
        for s_ in range(NS):
            rt, Brt = rtr[s_ % 2]
            tn, Btn = tnr[s_ % len(tnr)]
            t32, Bt32 = t32r[0]
            st_, Bst_ = statr[s_ % 2]
            mset("dve", st_, 0.0, (Bst_,))
            act(tn, h2[:, s_, :], AF.Square, (Bh2[s_], Bst_), (Btn, Bst_), accum=st_[:, 0:1])
            rstd_from_ss(st_[:, 0:1], D, (Bst_,), (Bst_,))
            stt("dve", tn, h2[:, s_, :], st_[:, 0:1], gbc, ALU.mult, ALU.mult, (Bh2[s_], Bst_, Bg), (Btn,))
            for q4 in range(4):
                pb, Bp = nextbank()
                for i in range(4):
                    c = 4 * q4 + i
                    tr(pb[:, i * 128:(i + 1) * 128], tn[:, c * 128:(c + 1) * 128], ident_f, (Btn, Bcm), (Bp,))
                src = pb[:, :].rearrange("p (a b) -> p a b", a=4)
                cp("act", tT[:, 4 * q4:4 * q4 + 4, 128 * s_:128 * s_ + 128], src, (Bp,), (BtT,))
                cp("dve", t32[:, 4 * q4:4 * q4 + 4, :], src, (Bp,), (Bt32,))
            while route_q:
                route_q.pop(0)()
            pb, Bp = nextbank()
            for c in range(16):
                mm(pb[:, 0:20], t32[:, c, :], Wge[:, c, :], c == 0, c == 15, (Bt32, BWge), (Bp,))
            R_ = (Brt,)
            lg = rt[:, 0:20]
            tt("dve", lg, pb[:, 0:20], smc[:, 16:36], ALU.add, (Bp, Bsmc), R_)
            route_q.append(lambda rt=rt, R_=R_, s_=s_: route_math(rt, R_, s_))
        while route_q:
            route_q.pop(0)()
        if debug:
            dma("sp", dbg["d_gates"][:, 0:NS * 16], gates.rearrange("p a b -> p (a b)"), (Bgates,), ())
        S.barrier()
        A.release(mp)
        wstage_alloc(2, ("pool",))
        wgr = ring(2, [128, 16, 128], BF16, "wg")
        wur = ring(2, [128, 16, 128], BF16, "wu")
        wdr = ring(2, [128, 4, 2048], BF16, "wd")
        actr = ring(2, [128, 4, NT], BF16, "actT")
        sgr = ring(2, [128, TG], F32, "silu")
        wi_ = 0
        for e_ in range(NEXP):
            actT, Bact = actr[e_ % 2]
            Wd, BWd = wdr[e_ % 2]
            for f in range(4):
                wload(Wd[:, f, :], w_down[e_, f * 128:(f + 1) * 128, :], BWd)
            for ft in range(4):
                Wg, BWg = wgr[wi_ % 2]
                Wu, BWu = wur[wi_ % 2]
                wi_ += 1
                wload(Wg, w_gate[e_, :, ft * 128:(ft + 1) * 128].rearrange("(c p) n -> p c n", p=128), BWg)
                wload(Wu, w_up[e_, :, ft * 128:(ft + 1) * 128].rearrange("(c p) n -> p c n", p=128), BWu)
                for tg in range(NTG):
                    cols = slice(tg * TG, (tg + 1) * TG)
                    pg, Bpg = nextbank()
                    for c in range(16):
                        mm(pg[:, 0:TG], Wg[:, c, :], tT[:, c, cols], c == 0, c == 15, (BWg, BtT), (Bpg,))
                    pu, Bpu = nextbank()
                    for c in range(16):
                        mm(pu[:, 0:TG], Wu[:, c, :], tT[:, c, cols], c == 0, c == 15, (BWu, BtT), (Bpu,))
                    sg, Bsg = sgr[tg % 2]
                    act(sg, pg[:, 0:TG], AF.Silu, (Bpg,), (Bsg,))
                    tt("dve", actT[:, ft, cols], sg, pu[:, 0:TG], ALU.mult, (Bsg, Bpu), (Bact,))
            for s_ in range(NS):
                for cg in range(4):
                    pb, Bp = nextbank()
                    for f in range(4):
                        mm(pb[:, :], actT[:, f, 128 * s_:128 * s_ + 128], Wd[:, f, cg * 512:(cg + 1) * 512], f == 0, f == 3,
                           (Bact, BWd), (Bp,))
                    stt("dve", h2[:, s_, cg * 512:(cg + 1) * 512], pb[:, :], gates[:, s_, e_:e_ + 1],
                        h2[:, s_, cg * 512:(cg + 1) * 512], ALU.mult, ALU.add, (Bp, Bgates, Bh2[s_]), (Bh2[s_],))
        S.barrier()
        A.release(mp)
        gbc = A.alloc([128, 2048], F32)
        Bg = Buf("gfinal")
        dma("sp", gbc, g_final.partition_broadcast(128), (), (Bg,))
        outr = ring(2, [128, 2048], F32, "outst")
        statr = ring(2, [128, 8], F32, "fst")
        for s_ in range(NS):
            ot, Bot = outr[s_ % 2]
            st_, Bst_ = statr[s_ % 2]
            mset("dve", st_, 0.0, (Bst_,))
            act(ot, h2[:, s_, :], AF.Square, (Bh2[s_], Bst_), (Bot, Bst_), accum=st_[:, 0:1])
            rstd_from_ss(st_[:, 0:1], D, (Bst_,), (Bst_,))
            stt("dve", ot, h2[:, s_, :], st_[:, 0:1], gbc, ALU.mult, ALU.mult, (Bh2[s_], Bst_, Bg), (Bot,))
            dma("sp", out_d[128 * s_:128 * s_ + 128, :], ot, (Bot,), ())
        S.barrier()
        return _finish(nc, S, out_d)


def _finish(nc, S, out_d):
    with nc.Block() as block:
        S.emit(block)
    return nc


def own_blocks(c):
    out = []
    for m in range(4):
        out += [16*m + c, 16*m + 15 - c]
    return out

def rope_tables(pos, dim):
    half = dim // 2
    inv = (10000.0 ** (-np.arange(half, dtype=np.float32) / half)).astype(np.float32)
    ang = pos.astype(np.float32)[None, :] * inv[:, None]
    cos = np.cos(ang).astype(np.float32); sin = np.sin(ang).astype(np.float32)
    reps = 128 // dim
    cosT = np.concatenate([cos, cos] * reps, axis=0)
    sinT = np.concatenate([-sin, sin] * reps, axis=0)
    return np.ascontiguousarray(cosT), np.ascontiguousarray(sinT)

def _prep(inputs):
    x = np.asarray(inputs['x'], np.float32)[0]
    meta = np.asarray(inputs['meta_tokens'], np.float32)
    xall = np.ascontiguousarray(np.concatenate([meta, x], axis=0))
    posall = np.arange(L)
    cA, sA = rope_tables(posall, 128)
    cI, sI = rope_tables(posall, 64)
    g = lambda k: np.ascontiguousarray(np.asarray(inputs[k], np.float32))
    common = {
        'xall': xall, 'w_in': g('w_in')[0], 'w_kvup': g('w_kv_up')[0], 'w_bra': g('w_branch_a')[0],
        'w_brb': g('w_branch_b')[0], 'w_out': g('w_out')[0],
        'w_ge': np.ascontiguousarray(np.concatenate([g('w_group')[0], g('w_expert')[0]], axis=1)),
        'w_gate': g('w_gate_e')[0], 'w_up': g('w_up_e')[0], 'w_down': g('w_down_e')[0],
        'g_mix': g('g_mix').reshape(1, D), 'g_ffn': g('g_ffn').reshape(1, D), 'g_final': g('g_final').reshape(1, D),
        'cosA_all': cA, 'sinA_all': sA, 'cosI_all': cI, 'sinI_all': sI,
    }
    smallc = np.zeros((128, 64), np.float32)
    smallc[:, 0:4] = g('g_kv')[0].reshape(4, 128).T
    gk = g('g_idx_k')[0]
    p = np.arange(128)
    smallc[:, 4] = gk[p % 64]
    smallc[:, 5] = gk[(p % 64 + 32) % 64]
    smallc[:, 8:16] = g('b_f')[0][None, :]
    smallc[:, 16:20] = g('b_group')[0][None, :]
    smallc[:, 20:36] = g('b_expert')[0][None, :]
    smallc[:, 36:52] = (0.5 ** np.arange(1, 17, dtype=np.float32))[None, :]
    common['smallc'] = smallc
    cm = np.zeros((128, 512), np.float32)
    cm[:, 0:128] = np.eye(128)
    cm[:, 128:256] = np.triu(np.ones((128, 128)))
    cm[:, 256:384] = 1.0
    blk = np.zeros((128, 128)); blk[0:64, 0:64] = 1; blk[64:, 64:] = 1
    cm[:, 384:512] = blk
    common['cmat'] = cm
    maps = []
    tri = (np.arange(128)[:, None] <= np.arange(128)[None, :]).astype(np.float32)
    for c in range(NCORES):
        blks = own_blocks(c)
        rows = np.concatenate([np.arange(128 * b, 128 * b + 128) for b in blks])
        pos = rows + NMETA
        d = dict(common)
        d['xown'] = np.ascontiguousarray(x[rows])
        ca, sa = rope_tables(pos, 128); ci, si = rope_tables(pos, 64)
        sc = np.float32(128 ** -0.5)
        d['cosA_own'] = ca * sc; d['sinA_own'] = sa * sc; d['cosI_own'] = ci; d['sinI_own'] = si
        mT = np.zeros((128, 16, 256), np.float32)
        for t in range(16):
            for half, dg in ((0, c), (1, 15 - c)):
                if t < dg: mT[:, t, half*128:(half+1)*128] = 1.0
                elif t == dg: mT[:, t, half*128:(half+1)*128] = tri
        d['maskT'] = np.ascontiguousarray(mT.reshape(128, 16 * 256))
        nb = np.zeros((128, 2, 16, 128), np.float32)
        for half in range(2):
            m_qk = mT[:, :, half*128:(half+1)*128].transpose(2, 1, 0)
            nb[:, half] = np.where(m_qk > 0, 0.0, -1e30)
        d['negb'] = np.ascontiguousarray(nb.reshape(128, 2 * 2048))
        oh = np.zeros((128, 8, 66), np.float32)
        for s, b in enumerate(blks):
            oh[:, s, 1 + b] = 1.0
        d['ohsel'] = np.ascontiguousarray(oh.reshape(128, 8 * 66))
        maps.append(d)
    return maps

def _assemble(results):
    out = np.zeros((1, SEQ, D), np.float32)
    for c in range(NCORES):
        blks = own_blocks(c)
        o = results[c]['out']
        for s, b in enumerate(blks):
            out[0, 128*b:128*b+128] = o[128*s:128*s+128]
    return out


_PROGRAM = {}


def kernel(**inputs):
    maps = _prep(inputs)
    if "nc" not in _PROGRAM:
        _PROGRAM["nc"] = build_program()
    nc = _PROGRAM["nc"]
    res = run_bass_kernel_spmd(nc, maps, core_ids=list(range(NCORES)))
    return _assemble(res.results)
```

```python
import contextlib
import os
LVL = int(os.environ.get('LVL', '9'))
import numpy as np
import ml_dtypes
import concourse.bass as bass
import concourse.mybir as mybir
from concourse.bass_utils import run_bass_kernel_spmd

F32 = mybir.dt.float32
BF16 = mybir.dt.bfloat16
ALU = mybir.AluOpType
AF = mybir.ActivationFunctionType
AX = mybir.AxisListType

NCORES = 8
D = 2048
SEQ = 8192
NMETA = 16
L = SEQ + NMETA
NKB_ALL = 65
HD = 128
EPS = 1e-6
TOPK = 256
NBISECT = 15
C_QA, C_CKV, C_QI, C_KI, C_WI, C_QB, C_KB, C_VB, C_FB, C_GA, C_GB = (
    0, 1024, 1536, 2560, 2624, 2640, 3664, 4688, 5712, 5720, 7768)


class Buf:
    __slots__ = ("name", "writers", "readers", "excl")

    def __init__(self, name="", excl=False):
        self.name = name
        self.writers = []
        self.readers = []
        self.excl = excl


class _Eng:
    def __init__(self, name, sem):
        self.name = name
        self.sem = sem
        self.count = 0
        self.ops = []
        self.seen = {}
        self.dsems = []
        self.dcount = []
        self.dnext = 0


class Sched:
    ENG_NAMES = ("pe", "act", "dve", "pool", "sp")

    def __init__(self, nc, stack, n_dsem=24):
        self.nc = nc
        self.eng = {}
        self.clock = {}
        for n in self.ENG_NAMES:
            sem = stack.enter_context(nc.semaphore("s_" + n))
            self.eng[n] = _Eng(n, sem)
        for n in ("sp", "pool"):
            e = self.eng[n]
            for i in range(n_dsem):
                e.dsems.append(stack.enter_context(nc.semaphore("d_%s_%d" % (n, i))))
                e.dcount.append(0)
        self.semobj = {}
        for n in self.ENG_NAMES:
            self.semobj[("e", n)] = self.eng[n].sem
        for n in ("sp", "pool"):
            for i, s in enumerate(self.eng[n].dsems):
                self.semobj[("d", n, i)] = s
        self.nwaits = 0

    def _need(self, E, ev, out):
        key, val = ev
        if E.seen.get(key, 0) >= val:
            return
        if out.get(key, 0) < val:
            out[key] = val

    def _apply_waits(self, E, need):
        for key, val in need.items():
            if E.seen.get(key, 0) >= val:
                continue
            E.ops.append(("w", self.semobj[key], val))
            self.nwaits += 1
            ck = self.clock.get((key, val))
            if ck:
                for k2, v2 in ck.items():
                    if E.seen.get(k2, 0) < v2:
                        E.seen[k2] = v2
            if E.seen.get(key, 0) < val:
                E.seen[key] = val

    def _deps(self, E, reads, writes, same_ok):
        need = {}
        me = ("e", E.name)
        for b in reads:
            for ev in b.writers:
                if same_ok and ev[0] == me:
                    continue
                self._need(E, ev, need)
            if b.excl:
                for ev in b.readers:
                    if ev[0] != me:
                        self._need(E, ev, need)
        for b in writes:
            for ev in b.writers:
                if same_ok and ev[0] == me:
                    continue
                self._need(E, ev, need)
            for ev in b.readers:
                if same_ok and ev[0] == me:
                    continue
                self._need(E, ev, need)
        self._apply_waits(E, need)

    def _record(self, ev, reads, writes):
        for b in reads:
            b.readers.append(ev)
        for b in writes:
            b.writers = [ev]
            b.readers = []

    def op(self, eng, fn, reads=(), writes=()):
        E = self.eng[eng]
        self._deps(E, reads, writes, same_ok=(eng == "pe"))
        E.count += 1
        ev = (("e", eng), E.count)
        E.ops.append(("i", fn, E.sem, 1))
        ck = dict(E.seen)
        ck[("e", eng)] = E.count
        self.clock[ev] = ck
        self._record(ev, reads, writes)
        return ev

    def dma(self, eng, fn, reads=(), writes=(), fresh=False):
        E = self.eng[eng]
        self._deps(E, reads, () if fresh else writes, same_ok=False)
        i = E.dnext
        E.dnext = (E.dnext + 1) % len(E.dsems)
        key = ("d", eng, i)
        if E.dcount[i] > 0:
            need = {}
            self._need(E, (key, E.dcount[i]), need)
            self._apply_waits(E, need)
        E.dcount[i] += 16
        ev = (key, E.dcount[i])
        E.ops.append(("i", fn, E.dsems[i], 16))
        self.clock[ev] = dict(E.seen)
        if fresh:
            self._record(ev, reads, ())
            for b in writes:
                b.writers.append(ev)
        else:
            self._record(ev, reads, writes)
        return ev

    def barrier(self):
        for n in ("sp", "pool"):
            E = self.eng[n]
            need = {}
            for i in range(len(E.dsems)):
                if E.dcount[i] > 0:
                    self._need(E, (("d", n, i), E.dcount[i]), need)
            self._apply_waits(E, need)
        evs = []
        for n in self.ENG_NAMES:
            E = self.eng[n]
            if E.count > 0:
                need = {}
                self._need(E, (("e", n), E.count), need)
                self._apply_waits(E, need)
            evs.append(self.op(n, lambda e: e.nop(nofuse=True), (), ()))
        for n in self.ENG_NAMES:
            E = self.eng[n]
            need = {}
            for ev in evs:
                self._need(E, ev, need)
            self._apply_waits(E, need)

    def emit(self, block):
        def run(E, e):
            for o in E.ops:
                if o[0] == "w":
                    e.wait_ge(o[1], o[2])
                else:
                    o[1](e).then_inc(o[2], o[3])

        S = self

        @block.tensor
        def _(e):
            run(S.eng["pe"], e)

        @block.scalar
        def _(e):
            run(S.eng["act"], e)

        @block.vector
        def _(e):
            run(S.eng["dve"], e)

        @block.gpsimd
        def _(e):
            run(S.eng["pool"], e)

        @block.sync
        def _(e):
            run(S.eng["sp"], e)


class SBAlloc:
    def __init__(self, nc, nbytes=200 * 1024):
        self.arena = nc.alloc_sbuf_tensor("arena", [128, nbytes // 4], F32)
        self.off = 0
        self.limit = nbytes
        self.full = nbytes
        self.peak = 0

    def alloc(self, shape, dtype):
        esz = 2 if dtype == BF16 else 4
        n = 1
        for s in shape[1:]:
            n *= s
        fb = (n * esz + 63) // 64 * 64
        off = self.off
        assert off + fb <= self.limit, "SBUF overflow %d+%d" % (off, fb)
        self.off = off + fb
        self.peak = max(self.peak, self.off)
        v = self.arena[0:shape[0], off // 4:(off + fb) // 4]
        if dtype != F32:
            v = v.bitcast(dtype)
        v = v[:, 0:n]
        if len(shape) == 3:
            v = v.rearrange("p (a b) -> p a b", a=shape[1])
        elif len(shape) == 4:
            v = v.rearrange("p (a b c) -> p a b c", a=shape[1], b=shape[2])
        return v

    def alloc_top(self, shape, dtype):
        esz = 2 if dtype == BF16 else 4
        n = 1
        for s in shape[1:]:
            n *= s
        fb = (n * esz + 63) // 64 * 64
        self.limit -= fb
        assert self.off <= self.limit
        off = self.limit
        v = self.arena[0:shape[0], off // 4:(off + fb) // 4]
        if dtype != F32:
            v = v.bitcast(dtype)
        v = v[:, 0:n]
        if len(shape) == 3:
            v = v.rearrange("p (a b) -> p a b", a=shape[1])
        return v

    def mark(self):
        return self.off

    def release(self, m):
        self.off = m


def build_program(stop_after=None, debug=False, only=None, npairs=4, NEXP=16):
    def on(part):
        return only is None or part in only
    nc = bass.Bass("TRN2", target_bir_lowering=False)

    def din(name, shape, dt=F32):
        return nc.dram_tensor(name, list(shape), dt, kind="ExternalInput").ap()

    def dscr(name, shape, dt=BF16):
        if debug:
            return nc.dram_tensor(name, list(shape), dt, kind="ExternalOutput").ap()
        return nc.dram_tensor(name, list(shape), dt).ap()

    xall = din("xall", [L, D])
    xown = din("xown", [1024, D])
    w_in = din("w_in", [D, 9816])
    w_kvup = din("w_kvup", [512, 2048])
    w_bra = din("w_bra", [1024, D])
    w_brb = din("w_brb", [1024, D])
    w_out = din("w_out", [D, D])
    w_ge = din("w_ge", [D, 20])
    w_gate = din("w_gate", [16, D, 512])
    w_up = din("w_up", [16, D, 512])
    w_down = din("w_down", [16, 512, D])
    g_mix = din("g_mix", [1, D])
    g_ffn = din("g_ffn", [1, D])
    g_final = din("g_final", [1, D])
    smallc = din("smallc", [128, 64])
    cosA_all = din("cosA_all", [128, L])
    sinA_all = din("sinA_all", [128, L])
    cosI_all = din("cosI_all", [128, L])
    sinI_all = din("sinI_all", [128, L])
    cosA_own = din("cosA_own", [128, 1024])
    sinA_own = din("sinA_own", [128, 1024])
    cosI_own = din("cosI_own", [128, 1024])
    sinI_own = din("sinI_own", [128, 1024])
    maskT_d = din("maskT", [128, 16 * 256])
    negb_d = din("negb", [128, 2 * 2048])
    ohsel_d = din("ohsel", [128, 8 * 66])
    cmat = din("cmat", [128, 4 * 128])
    out_d = nc.dram_tensor("out", [1024, D], F32, kind="ExternalOutput").ap()

    KaT = dscr("KaT", [8, 128, L])
    KbT = dscr("KbT", [8, 128, L])
    KiT_d = dscr("KiT", [128, L])
    Va = dscr("Va", [L, 1024])
    Vb = dscr("Vb", [L, 1024])
    QaT = dscr("QaT", [8, 128, 1024])
    QbT = dscr("QbT", [8, 128, 1024])
    QiT = dscr("QiT", [8, 128, 1024])
    HnO = dscr("HnO", [128, 16, 1024])
    dbg = {}
    if debug:
        for nm, shp in (("d_hn", [128, 16, 1024]), ("d_L", [128, 8 * 66]), ("d_ya", [128, 8 * 1024]),
                        ("d_yb", [128, 8 * 1024]), ("d_h2", [1024, D]), ("d_thr", [8, 128]),
                        ("d_gates", [128, 8 * 16]), ("d_score", [128, 8208])):
            dbg[nm] = nc.dram_tensor(nm, shp, F32, kind="ExternalOutput").ap()

    with contextlib.ExitStack() as st:
        S = Sched(nc, st)
        A = SBAlloc(nc)
        banks = [nc.alloc_psum_tensor("psb%d" % i, [128, 512], F32) for i in range(8)]
        Bbank = [Buf("bank%d" % i, excl=True) for i in range(8)]
        rr = {"i": 0, "lo": 0, "hi": 8}

        def nextbank():
            i = rr["i"]
            rr["i"] = rr["lo"] + (i + 1 - rr["lo"]) % (rr["hi"] - rr["lo"])
            return banks[i], Bbank[i]

        def setbanks(lo, hi):
            rr["lo"], rr["hi"], rr["i"] = lo, hi, lo

        def mm(out, lhsT, rhs, start, stop, R, W):
            S.op("pe", lambda e: e.matmul(out, lhsT=lhsT, rhs=rhs, start=start, stop=stop), R, W)

        def tr(out, in_, ident, R, W):
            S.op("pe", lambda e: e.transpose(out=out, in_=in_, identity=ident), R, W)

        def act(out, in_, func, R, W, bias=None, scale=None, accum=None, eng="act"):
            kw = {}
            if bias is not None:
                kw["bias"] = bias
            if scale is not None:
                kw["scale"] = scale
            if accum is not None:
                kw["accum_out"] = accum
            S.op(eng, lambda e: e.activation(out=out, in_=in_, func=func, **kw), R, W)

        def ts(eng, out, in0, s1, s2, op0, op1, R, W, accum=None):
            kw = {}
            if op1 is not None:
                kw["op1"] = op1
            if accum is not None:
                kw["accum_out"] = accum
            S.op(eng, lambda e: e.tensor_scalar(out=out, in0=in0, scalar1=s1, scalar2=s2, op0=op0, **kw), R, W)

        def tt(eng, out, in0, in1, op, R, W):
            S.op(eng, lambda e: e.tensor_tensor(out=out, in0=in0, in1=in1, op=op), R, W)

        def stt(eng, out, in0, scalar, in1, op0, op1, R, W):
            S.op(eng, lambda e: e.scalar_tensor_tensor(out=out, in0=in0, scalar=scalar, in1=in1, op0=op0, op1=op1), R, W)

        def cp(eng, out, in_, R, W):
            if eng == "act":
                S.op("act", lambda e: e.copy(out=out, in_=in_), R, W)
            else:
                S.op(eng, lambda e: e.tensor_copy(out=out, in_=in_), R, W)

        def red(eng, out, in_, op, R, W):
            S.op(eng, lambda e: e.tensor_reduce(out=out, in_=in_, axis=AX.X, op=op), R, W)

        def recip(out, in_, R, W):
            S.op("dve", lambda e: e.reciprocal(out=out, in_=in_), R, W)

        def mset(eng, ap, val, W):
            S.op(eng, lambda e: e.memset(ap, val), (), W)

        def dma(eng, out, in_, R, W, fresh=False):
            S.dma(eng, lambda e: e.dma_start(out=out, in_=in_), R, W, fresh=fresh)

        def ring(n, shape, dtype, name="r"):
            return [(A.alloc(shape, dtype), Buf("%s%d" % (name, i))) for i in range(n)]

        wst = {"ring": None, "i": 0, "engs": ("pool",)}

        def wstage_alloc(n=3, engs=("pool",)):
            wst["ring"] = ring(n, [128, 2048], F32, "wstg")
            wst["i"] = 0
            wst["engs"] = engs

        def wload(dst, src, Bdst, cast_eng=None):
            shp = list(dst.shape)
            if len(shp) == 2:
                pieces = [(dst, src, shp[1], None)]
            else:
                a, b = shp[1], shp[2]
                step = max(1, 2048 // b)
                pieces = []
                for a0 in range(0, a, step):
                    a1 = min(a, a0 + step)
                    pieces.append((dst[:, a0:a1, :], src[:, a0:a1, :], (a1 - a0) * b, a1 - a0))
            for (d_, s_, n_, na) in pieces:
                stg, Bstg = wst["ring"][wst["i"] % len(wst["ring"])]
                wst["i"] += 1
                v = stg[:, 0:n_]
                if na is not None:
                    v = v.rearrange("p (a b) -> p a b", a=na)
                dma("sp", v, s_, (), (Bstg,))
                ce = cast_eng or wst["engs"][wst["i"] % len(wst["engs"])]
                cp(ce, d_, v, (Bstg,), (Bdst,))

        cm = A.alloc([128, 512], F32)
        Bcm = Buf("cm")
        dma("sp", cm, cmat[:, :], (), (Bcm,))
        ident_f = cm[:, 0:128]
        triU_f = cm[:, 128:256]
        ones_f = cm[:, 256:384]
        cmb = A.alloc([128, 512], BF16)
        Bcmb = Buf("cmb")
        cp("dve", cmb, cm, (Bcm,), (Bcmb,))
        ident_b = cmb[:, 0:128]
        ones_b = cmb[:, 256:384]
        blk64_b = cmb[:, 384:512]
        smc = A.alloc([128, 64], F32)
        Bsmc = Buf("smc")
        dma("sp", smc, smallc[:, :], (), (Bsmc,))
        Lall = A.alloc([128, 8, 66], F32)
        Tall = A.alloc([128, 8, 66], F32)
        BL = Buf("Lall")
        BT = Buf("Tall")
        mset("pool", Lall, 0.0, (BL,))
        mset("pool", Tall, 0.0, (BT,))
        wiabs = A.alloc([128, 8, 16], F32)
        wisgn = A.alloc([128, 8, 16], F32)
        Bwi = Buf("wi")
        persist_mark = A.mark()

        def rstd_from_ss(ss, n, R, W, tmp=None):
            ts("dve", ss, ss, 1.0 / n, EPS, ALU.mult, ALU.add, R, W)
            act(ss, ss, AF.Sqrt, W, W)
            recip(ss, ss, W, W)

        def norm_A(src_rows_ap, rows, gbc, Bg, xr, xnr, statr, it):
            xb, Bx = xr[it % len(xr)]
            xn, Bxn = xnr[it % len(xnr)]
            stt_, Bst = statr[it % len(statr)]
            dma("sp", xb[0:rows, :], src_rows_ap, (), (Bx,))
            mset("dve", stt_[0:rows, :], 0.0, (Bst,))
            act(xn[0:rows, :], xb[0:rows, :], AF.Square, (Bx, Bst), (Bst, Bxn), accum=stt_[0:rows, 0:1])
            rstd_from_ss(stt_[0:rows, 0:1], D, (Bst,), (Bst,))
            stt("dve", xn[0:rows, :], xb[0:rows, :], stt_[0:rows, 0:1], gbc[0:rows, :], ALU.mult, ALU.mult,
                (Bx, Bst, Bg), (Bxn,))
            return xn, Bxn

        def norm_B(xn, Bxn, rows, hnT, BhnT, col0):
            for half in range(2):
                pb, Bp = nextbank()
                pbb = pb[:, :].bitcast(BF16)
                for i in range(8):
                    c = half * 8 + i
                    tr(pbb[:, i * 128:i * 128 + rows], xn[0:rows, c * 128:(c + 1) * 128], ident_b[0:rows, 0:rows],
                       (Bxn, Bcmb), (Bp,))
                src = pbb.rearrange("p (a b) -> p a b", a=8)[:, :, 0:rows]
                cp("act", hnT[:, half * 8:half * 8 + 8, col0:col0 + rows], src, (Bp,), (BhnT,))

        def norm_block(src_rows_ap, rows, gbc, Bg, xr, xnr, statr, hnT, BhnT, col0, it):
            xn, Bxn = norm_A(src_rows_ap, rows, gbc, Bg, xr, xnr, statr, it)
            norm_B(xn, Bxn, rows, hnT, BhnT, col0)

        m0 = A.mark()
        gbc = A.alloc([128, 2048], F32)
        Bg = Buf("gmix")
        dma("sp", gbc, g_mix.partition_broadcast(128), (), (Bg,))
        Wckv = A.alloc([128, 16, 512], BF16)
        Wki = A.alloc([128, 16, 128], BF16)
        Wkis = A.alloc([128, 16, 128], BF16)
        Wkb = A.alloc([128, 16, 1024], BF16)
        Wvb = A.alloc([128, 16, 1024], BF16)
        Wfb = A.alloc([128, 16, 8], BF16)
        Wkk = A.alloc([128, 4, 1024], BF16)
        Wkks = A.alloc([128, 4, 1024], BF16)
        Wkv = A.alloc([128, 4, 1024], BF16)
        BW = Buf("W0")

        def wv(c0, n):
            return w_in[:, c0:c0 + n].rearrange("(c p) n -> p c n", p=128)

        def wv1(c0, n, c):
            return w_in[c * 128:(c + 1) * 128, c0:c0 + n]

        BW2 = Buf("W0b")
        BW3 = Buf("W0c")
        BW4 = Buf("W0d")
        BW5 = Buf("W0e")
        BW6 = Buf("W0f")
        for c in range(16):
            dma("pool", Wckv[:, c, :], wv1(C_CKV, 512, c), (), (BW,), fresh=True)
            for d0 in (0, 64):
                dma("pool", Wki[:, c, d0:d0 + 64], wv1(C_KI, 64, c), (), (BW2,), fresh=True)
                dma("pool", Wkis[:, c, d0:d0 + 32], wv1(C_KI + 32, 32, c), (), (BW2,), fresh=True)
                dma("pool", Wkis[:, c, d0 + 32:d0 + 64], wv1(C_KI, 32, c), (), (BW2,), fresh=True)
            dma("pool", Wkb[:, c, :], wv1(C_KB, 1024, c), (), (BW3,), fresh=True)
            dma("pool", Wvb[:, c, :], wv1(C_VB, 1024, c), (), (BW4,), fresh=True)
            dma("pool", Wfb[:, c, :], wv1(C_FB, 8, c), (), (BW5,), fresh=True)
        for c in range(4):
            kvr = w_kvup[c * 128:(c + 1) * 128, :]
            dma("pool", Wkk[:, c, :], kvr[:, 0:1024], (), (BW6,), fresh=True)
            dma("pool", Wkv[:, c, :], kvr[:, 1024:2048], (), (BW6,), fresh=True)
            src = kvr[:, 0:1024].rearrange("p (h t j) -> p h t j", h=8, t=2)
            dst = Wkks[:, c, :].rearrange("p (h t j) -> p h t j", h=8, t=2)
            dma("pool", dst[:, :, 0, :], src[:, :, 1, :], (), (BW6,), fresh=True)
            dma("pool", dst[:, :, 1, :], src[:, :, 0, :], (), (BW6,), fresh=True)
        if stop_after == "w0":
            S.barrier()
            return _finish(nc, S, out_d)
        Wall = (BW, BW2, BW3, BW4, BW5, BW6)

        xr = ring(1, [128, 2048], F32, "x")
        xnr = ring(2, [128, 2048], BF16, "xn")
        statr = ring(4, [128, 8], F32, "st")
        hnr = ring(2, [128, 16, 256], BF16, "hn")
        tabr = ring(1, [128, 4, 256], F32, "tab")
        sqr = ring(1, [128, 4, 256], BF16, "sq")
        ckvTr = ring(1, [128, 4, 256], F32, "ckvT")
        rstdr = ring(2, [128, 256], F32, "rstd")
        ckvnr = ring(2, [128, 4, 256], BF16, "ckvn")
        t1r = ring(2, [128, 256], F32, "t1")
        t2r = ring(2, [128, 256], F32, "t2")
        kst = ring(4, [128, 256], BF16, "kst")
        vst = ring(3, [128, 1024], BF16, "vst")
        ksqr = ring(1, [128, 256], BF16, "ksq")
        kiAr = ring(1, [128, 256], F32, "kiA")
        kiBr = ring(1, [128, 256], F32, "kiB")
        lr = ring(2, [128, 8], F32, "l")
        zr = ring(2, [128, 8], F32, "z")

        cnt = {"blk": 0, "k": 0, "v": 0, "t": 0}
        groups = [(0, 16)] + [(16 + 256 * g, 256) for g in range(32)]
        groups = groups[:1 + 8 * npairs]
        if stop_after == "p0small":
            groups = groups[1:3] if only and "nometa" in only else groups[:3]
        pendB = []

        def do_norm_A(gi_):
            p0_, T_ = groups[gi_]
            hnT_, BhnT_ = hnr[gi_ % 2]
            for j_ in range(1 if T_ == 16 else 2):
                rows_ = 16 if T_ == 16 else 128
                xn_, Bxn_ = norm_A(xall[p0_ + 128 * j_:p0_ + 128 * j_ + rows_, :], rows_, gbc, Bg, xr, xnr, statr,
                                   cnt["blk"])
                cnt["blk"] += 1
                pendB.append((xn_, Bxn_, rows_, hnT_, BhnT_, 128 * j_))

        def do_norm_B():
            while pendB:
                norm_B(*pendB.pop(0))

        deferred = []
        do_norm_A(0)
        do_norm_B()
        for gi, (p0, T) in enumerate(groups):
            hnT, BhnT = hnr[gi % 2]
            tab, Btab = tabr[0]
            nblk = 1 if T == 16 else 2
            for ti, tsrc in enumerate((cosA_all, sinA_all, cosI_all, sinI_all)):
                dma("sp", tab[:, ti, 0:T], tsrc[:, p0:p0 + T], (), (Btab,))
            sq, Bsq = sqr[0]
            ckvT, BckvT = ckvTr[0]
            rs, Brs = rstdr[gi % 2]
            rs2, Brs2 = rstdr[(gi + 1) % 2]
            ckvn, Bckvn = ckvnr[gi % 2]
            for n in range(4):
                pb, Bp = nextbank()
                for c in range(16):
                    mm(pb[:, 0:T], Wckv[:, c, n * 128:(n + 1) * 128], hnT[:, c, 0:T], c == 0, c == 15,
                       (BW, BhnT), (Bp,))
                act(sq[:, n, 0:T], pb[:, 0:T], AF.Square, (Bp,), (Bsq,))
                cp("dve", ckvT[:, n, 0:T], pb[:, 0:T], (Bp,), (BckvT,))
            pki, Bpki = nextbank()
            pki2, Bpki2 = nextbank()
            for c in range(16):
                mm(pki[:, 0:T], Wki[:, c, :], hnT[:, c, 0:T], c == 0, c == 15, (BW2, BhnT), (Bpki,))
            for c in range(16):
                mm(pki2[:, 0:T], Wkis[:, c, :], hnT[:, c, 0:T], c == 0, c == 15, (BW2, BhnT), (Bpki2,))
            ksq, Bksq = ksqr[0]
            act(ksq[:, 0:T], pki[:, 0:T], AF.Square, (Bpki,), (Bksq,))
            kiA, BkiA = kiAr[0]
            kiB, BkiB = kiBr[0]
            ts("dve", kiA[:, 0:T], pki[:, 0:T], smc[:, 4:5], None, ALU.mult, None, (Bpki, Bsmc), (BkiA,))
            ts("dve", kiB[:, 0:T], pki2[:, 0:T], smc[:, 5:6], None, ALU.mult, None, (Bpki2, Bsmc), (BkiB,))
            for h in range(8):
                pb, Bp = nextbank()
                for c in range(16):
                    mm(pb[:, 0:T], Wkb[:, c, h * 128:(h + 1) * 128], hnT[:, c, 0:T], c == 0, c == 15,
                       (BW3, BhnT), (Bp,))
                ks, Bks = kst[cnt["k"] % 4]
                cnt["k"] += 1
                cp("act", ks[:, 0:T], pb[:, 0:T], (Bp,), (Bks,))
                dma("sp", KbT[h, :, p0:p0 + T], ks[:, 0:T], (Bks,), ())
                if h == 3:
                    pss, Bpss = nextbank()
                    for n in range(4):
                        mm(pss[:, 0:T], ones_b, sq[:, n, 0:T], n == 0, n == 3, (Bcmb, Bsq), (Bpss,))
                    mm(pss[:, 256:256 + T], blk64_b, ksq[:, 0:T], True, True, (Bcmb, Bksq), (Bpss,))
                    cp("dve", rs[:, 0:T], pss[:, 0:T], (Bpss,), (Brs,))
                    cp("dve", rs2[:, 0:T], pss[:, 256:256 + T], (Bpss,), (Brs2,))
                    rstd_from_ss(rs[:, 0:T], 512, (Brs,), (Brs,))
                    rstd_from_ss(rs2[:, 0:T], 64, (Brs2,), (Brs2,))
                    for n in range(4):
                        stt("dve", ckvn[:, n, 0:T], ckvT[:, n, 0:T], smc[:, n:n + 1], rs[:, 0:T], ALU.mult, ALU.mult,
                            (BckvT, Bsmc, Brs), (Bckvn,))
                    if gi + 1 < len(groups):
                        do_norm_A(gi + 1)
            for j in range(nblk):
                rows = 16 if T == 16 else 128
                b = 0 if T == 16 else 1 + 2 * (gi - 1) + j
                vs, Bvs = vst[cnt["v"] % 3]
                cnt["v"] += 1
                for half in range(2):
                    pb, Bp = nextbank()
                    for c in range(16):
                        mm(pb[0:rows, :], hnT[:, c, 128 * j:128 * j + rows], Wvb[:, c, half * 512:(half + 1) * 512],
                           c == 0, c == 15, (BW4, BhnT), (Bp,))
                    cp("act", vs[0:rows, half * 512:(half + 1) * 512], pb[0:rows, :], (Bp,), (Bvs,))
                dma("sp", Vb[p0 + 128 * j:p0 + 128 * j + rows, :], vs[0:rows, :], (Bvs,), ())
                pb, Bp = nextbank()
                for c in range(16):
                    mm(pb[0:rows, 0:8], hnT[:, c, 128 * j:128 * j + rows], Wfb[:, c, 0:8], c == 0, c == 15,
                       (BW5, BhnT), (Bp,))
                z, Bz = zr[b % 2]
                l, Bl = lr[b % 2]
                tt("dve", z[0:rows, :], pb[0:rows, 0:8], smc[0:rows, 8:16], ALU.add, (Bp, Bsmc), (Bz,))
                act(z[0:rows, :], z[0:rows, :], AF.Exp, (Bz,), (Bz,), scale=-1.0)
                act(l[0:rows, :], z[0:rows, :], AF.Ln, (Bz,), (Bl,), bias=1.0)
                def cums(rows=rows, l=l, Bl=Bl, b=b):
                    pb, Bp = nextbank()
                    mm(pb[0:rows, 0:8], triU_f[0:rows, 0:rows], l[0:rows, :], True, True, (Bcm, Bl), (Bp,))
                    mm(pb[:, 8:16], ones_f[0:rows, :], l[0:rows, :], True, True, (Bcm, Bl), (Bp,))
                    tt("dve", Lall[0:rows, :, b], pb[0:rows, 0:8], Tall[0:rows, :, b], ALU.add, (Bp, BT), (BL,))
                    tt("dve", Tall[:, :, b + 1], pb[:, 8:16], Tall[:, :, b], ALU.add, (Bp, BT), (BT,))

                deferred.append(cums)
            do_norm_B()
            tt("pool", kiA[:, 0:T], kiA[:, 0:T], rs2[:, 0:T], ALU.mult, (BkiA, Brs2), (BkiA,))
            tt("pool", kiB[:, 0:T], kiB[:, 0:T], rs2[:, 0:T], ALU.mult, (BkiB, Brs2), (BkiB,))
            tt("pool", kiA[:, 0:T], kiA[:, 0:T], tab[:, 2, 0:T], ALU.mult, (BkiA, Btab), (BkiA,))
            tt("pool", kiB[:, 0:T], kiB[:, 0:T], tab[:, 3, 0:T], ALU.mult, (BkiB, Btab), (BkiB,))
            ks, Bks = kst[cnt["k"] % 4]
            cnt["k"] += 1
            tt("pool", ks[:, 0:T], kiA[:, 0:T], kiB[:, 0:T], ALU.add, (BkiA, BkiB), (Bks,))
            dma("sp", KiT_d[:, p0:p0 + T], ks[:, 0:T], (Bks,), ())
            for h in range(8):
                pb, Bp = nextbank()
                pb2, Bp2 = nextbank()
                for c in range(4):
                    mm(pb[:, 0:T], Wkk[:, c, h * 128:(h + 1) * 128], ckvn[:, c, 0:T], c == 0, c == 3,
                       (BW6, Bckvn), (Bp,))
                for c in range(4):
                    mm(pb2[:, 0:T], Wkks[:, c, h * 128:(h + 1) * 128], ckvn[:, c, 0:T], c == 0, c == 3,
                       (BW6, Bckvn), (Bp2,))
                t1, Bt1 = t1r[cnt["t"] % 2]
                t2, Bt2 = t2r[cnt["t"] % 2]
                cnt["t"] += 1
                tt("dve", t1[:, 0:T], pb[:, 0:T], tab[:, 0, 0:T], ALU.mult, (Bp, Btab), (Bt1,))
                tt("dve", t2[:, 0:T], pb2[:, 0:T], tab[:, 1, 0:T], ALU.mult, (Bp2, Btab), (Bt2,))
                ks, Bks = kst[cnt["k"] % 4]
                cnt["k"] += 1
                tt("pool", ks[:, 0:T], t1[:, 0:T], t2[:, 0:T], ALU.add, (Bt1, Bt2), (Bks,))
                dma("sp", KaT[h, :, p0:p0 + T], ks[:, 0:T], (Bks,), ())
            for j in range(nblk):
                rows = 16 if T == 16 else 128
                vs, Bvs = vst[cnt["v"] % 3]
                cnt["v"] += 1
                for half in range(2):
                    pb, Bp = nextbank()
                    for c in range(4):
                        mm(pb[0:rows, :], ckvn[:, c, 128 * j:128 * j + rows], Wkv[:, c, half * 512:(half + 1) * 512],
                           c == 0, c == 3, (BW6, Bckvn), (Bp,))
                    cp("act", vs[0:rows, half * 512:(half + 1) * 512], pb[0:rows, :], (Bp,), (Bvs,))
                dma("sp", Va[p0 + 128 * j:p0 + 128 * j + rows, :], vs[0:rows, :], (Bvs,), ())
            while deferred:
                deferred.pop(0)()
        if debug:
            dma("sp", dbg["d_L"], Lall.rearrange("p a b -> p (a b)"), (BL,), ())
        S.barrier()
        A.release(m0)
        if stop_after in ("p0", "p0small"):
            return _finish(nc, S, out_d)
        NP_ = npairs
        NS = 2 * NP_
        NT = 128 * NS
        TG = min(512, NT)
        NTG = NT // TG

        def mkap(base, off_elems, dims):
            pstride = base.ap[0][0]
            return bass.AP(base.tensor, base.offset + off_elems, [(pstride, dims[0])] + [tuple(d) for d in dims[1:]])

        yaT = A.alloc([128, 8, NT], BF16)
        ByaT = Buf("yaT")
        mq = A.mark()
        gbc = A.alloc([128, 2048], F32)
        Bg = Buf("gmix2")
        dma("sp", gbc, g_mix.partition_broadcast(128), (), (Bg,))
        hnO = A.alloc([128, 16, NT], BF16)
        BhnO = Buf("hnO")
        xr = ring(2, [128, 2048], F32, "qx")
        xnr = ring(2, [128, 2048], BF16, "qxn")
        statr = ring(4, [128, 8], F32, "qst")
        for s_ in range(NS):
            norm_block(xown[128 * s_:128 * s_ + 128, :], 128, gbc, Bg, xr, xnr, statr, hnO, BhnO, 128 * s_, s_)
        dma("sp", HnO[:, :, 0:NT], hnO, (BhnO,), ())
        tabq = A.alloc([128, 4, NT], F32)
        Btabq = Buf("tabq")
        for ti, tsrc in enumerate((cosA_own, sinA_own, cosI_own, sinI_own)):
            dma("sp", tabq[:, ti, :], tsrc[:, 0:NT], (), (Btabq,), fresh=True)
        wstage_alloc(3, ("dve",))
        wr = ring(3, [128, 16, 128], BF16, "qw")
        wsr = ring(3, [128, 16, 128], BF16, "qws")
        qst = ring(3, [128, NT], BF16, "qstage")
        t1r = ring(2, [128, TG], F32, "qt1")
        t2r = ring(2, [128, TG], F32, "qt2")
        specs = [("qa", h, C_QA + 128 * h, QaT) for h in range(8)] + \
                [("qi", t, C_QI + 128 * t, QiT) for t in range(8)] + \
                [("qb", h, C_QB + 128 * h, QbT) for h in range(8)]
        def q_load(i):
            kind, idx, col0, dst = specs[i]
            W, BWt = wr[i % 3]
            Ws, BWs = wsr[i % 3]
            wsrc = w_in[:, col0:col0 + 128].rearrange("(c p) n -> p c n", p=128)
            wload(W, wsrc, BWt)
            if kind != "qb":
                hw = 64 if kind == "qa" else 32
                for j in range(128 // hw):
                    jo = j ^ 1
                    wload(Ws[:, :, j * hw:(j + 1) * hw], wsrc[:, :, jo * hw:(jo + 1) * hw], BWs)

        q_load(0)
        q_load(1)
        for i, (kind, idx, col0, dst) in enumerate(specs):
            W, BWt = wr[i % 3]
            Ws, BWs = wsr[i % 3]
            if i + 2 < len(specs):
                q_load(i + 2)
            st_, Bst_ = qst[i % 3]
            for tg in range(NTG):
                cols = slice(tg * TG, (tg + 1) * TG)
                pb, Bp = nextbank()
                for c in range(16):
                    mm(pb[:, 0:TG], W[:, c, :], hnO[:, c, cols], c == 0, c == 15, (BWt, BhnO), (Bp,))
                if kind == "qb":
                    act(st_[:, cols], pb[:, 0:TG], AF.Copy, (Bp,), (Bst_,), scale=float(128 ** -0.5))
                else:
                    pb2, Bp2 = nextbank()
                    for c in range(16):
                        mm(pb2[:, 0:TG], Ws[:, c, :], hnO[:, c, cols], c == 0, c == 15, (BWs, BhnO), (Bp2,))
                    ci = 0 if kind == "qa" else 2
                    t1, Bt1 = t1r[tg % 2]
                    t2, Bt2 = t2r[tg % 2]
                    tt("dve", t1, pb[:, 0:TG], tabq[:, ci, cols], ALU.mult, (Bp, Btabq), (Bt1,))
                    tt("dve", t2, pb2[:, 0:TG], tabq[:, ci + 1, cols], ALU.mult, (Bp2, Btabq), (Bt2,))
                    tt("pool", st_[:, cols], t1, t2, ALU.add, (Bt1, Bt2), (Bst_,))
            dma("sp", dst[idx, :, 0:NT], st_, (Bst_,), ())
        Wwi = A.alloc([128, 16, 16], BF16)
        BWwi = Buf("Wwi")
        dma("pool", Wwi, w_in[:, C_WI:C_WI + 16].rearrange("(c p) n -> p c n", p=128), (), (BWwi,))
        wtmp = A.alloc([128, 16], F32)
        Bwtmp = Buf("wtmp")
        for s_ in range(NS):
            pb, Bp = nextbank()
            for c in range(16):
                mm(pb[:, 0:16], hnO[:, c, 128 * s_:128 * s_ + 128], Wwi[:, c, :], c == 0, c == 15, (BWwi, BhnO), (Bp,))
            ts("dve", wtmp, pb[:, 0:16], 0.25 * 0.125, None, ALU.mult, None, (Bp,), (Bwtmp,))
            ts("dve", wisgn[:, s_, :], wtmp, 0.0, 2.0, ALU.is_ge, ALU.mult, (Bwtmp,), (Bwi,))
            ts("dve", wisgn[:, s_, :], wisgn[:, s_, :], -1.0, None, ALU.add, None, (Bwi,), (Bwi,))
            tt("dve", wiabs[:, s_, :], wtmp, wisgn[:, s_, :], ALU.mult, (Bwtmp, Bwi), (Bwi,))
        if debug:
            pass
        S.barrier()
        A.release(mq)
        if stop_after == "q":
            return _finish(nc, S, out_d)

        def kb_range(kb):
            return (0, 16) if kb == 0 else (16 + 128 * (kb - 1), 128)

        LOOK = 3
        pending_fin = []

        prefetched = {}

        def attention(m, h, mixer, Qp, BQp, KT_d, V_d, Kr, Vr, PTr, onr, rsr, yT, ByT, cnt,
                      selT=None, BselT=None, maskT_sb=None, Bmask=None, biasAB=None, Bbias=None, nxt=None):
            NKB = 16 * m + 17
            par = cnt["o"] % 2
            cnt["o"] += 1
            oA, BoA = banks[4 + 2 * par], Bbank[4 + 2 * par]
            oB, BoB = banks[5 + 2 * par], Bbank[5 + 2 * par]
            chunks = {}
            nch_tot = (NKB + 15) // 16
            kbA_max = NKB - 9

            def load_chunk(ch, mm_=m, hh_=h, NKB_=NKB):
                key = (mixer, mm_, hh_, ch)
                if key in prefetched:
                    chunks[ch] = prefetched.pop(key)
                    return
                NKBx = NKB_
                kb0 = 16 * ch
                kb1 = min(NKBx, kb0 + 16)
                klo = kb_range(kb0)[0]
                khi = kb_range(kb1 - 1)[0] + kb_range(kb1 - 1)[1]
                Kc, BKc = Kr[cnt["kv"] % len(Kr)]
                Vc, BVc = Vr[cnt["kv"] % len(Vr)]
                cnt["kv"] += 1
                dma("sp", Kc[:, 0:khi - klo], KT_d[hh_, :, klo:khi], (), (BKc,))
                j0 = 0
                if kb0 == 0:
                    dma("sp", Vc[0:16, 0, 0:128], V_d[0:16, hh_ * 128:(hh_ + 1) * 128], (), (BVc,))
                    j0 = 1
                nreal = (kb1 - kb0) - j0
                r0 = kb_range(kb0 + j0)[0]
                dma("sp", Vc[:, j0:j0 + nreal, 0:128],
                    V_d[r0:r0 + 128 * nreal, hh_ * 128:(hh_ + 1) * 128].rearrange("(j p) d -> p j d", p=128),
                    (), (BVc,), fresh=(j0 == 1))
                rec = (Kc, BKc, Vc, BVc, klo, kb0)
                if (mm_, hh_) == (m, h):
                    chunks[ch] = rec
                else:
                    prefetched[key] = rec

            pts = {}

            def emit_S(kb):
                ch = kb // 16
                if ch not in chunks:
                    load_chunk(ch)
                if kb % 16 == 0:
                    if ch + 1 < nch_tot:
                        if (ch + 1) not in chunks:
                            load_chunk(ch + 1)
                    elif nxt is not None:
                        load_chunk(0, nxt[0], nxt[1], 16 * nxt[0] + 17)
                Kc, BKc, Vc, BVc, klo, kb0 = chunks[ch]
                k0, ksz = kb_range(kb)
                pb, Bp = nextbank()
                q0 = 0 if kb <= kbA_max else 128
                mm(pb[0:ksz, q0:256], Kc[:, k0 - klo:k0 - klo + ksz], Qp[:, h, q0:256], True, True, (BKc, BQp), (Bp,))
                PT, BPT = PTr[cnt["pt"] % len(PTr)]
                cnt["pt"] += 1
                if mixer == "a":
                    act(PT[0:ksz, q0:256], pb[0:ksz, q0:256], AF.Exp, (Bp,), (BPT,))
                    tt("pool" if kb % 3 == 2 else "dve", PT[0:ksz, q0:256], PT[0:ksz, q0:256], selT[0:ksz, kb, q0:256], ALU.mult,
                       (BPT, BselT), (BPT,))
                else:
                    for half in range(q0 // 128, 2):
                        act(PT[0:ksz, half * 128:(half + 1) * 128], pb[0:ksz, half * 128:(half + 1) * 128], AF.Exp,
                            (Bp, Bbias), (BPT,), bias=biasAB[half][0:ksz, kb:kb + 1])
                    t = kb - (NKB - 16)
                    if t >= 0:
                        tt("dve", PT[0:ksz, q0:256], PT[0:ksz, q0:256], maskT_sb[0:ksz, t, q0:256], ALU.mult, (BPT, Bmask), (BPT,))
                pts[kb] = (PT, BPT)

            def emit_PV(kb):
                ch = kb // 16
                Kc, BKc, Vc, BVc, klo, kb0 = chunks[ch]
                k0, ksz = kb_range(kb)
                j = kb - kb0
                PT, BPT = pts.pop(kb)
                if kb <= kbA_max:
                    mm(oA[:, 0:129], PT[0:ksz, 0:128], Vc[0:ksz, j, :], kb == 0, kb == kbA_max, (BPT, BVc), (BoA,))
                mm(oB[:, 0:129], PT[0:ksz, 128:256], Vc[0:ksz, j, :], kb == 0, kb == NKB - 1, (BPT, BVc), (BoB,))

            for kb in range(min(LOOK, NKB)):
                emit_S(kb)
            while pending_fin:
                pending_fin.pop(0)()
            for kb in range(NKB):
                emit_PV(kb)
                if kb + LOOK < NKB:
                    emit_S(kb + LOOK)

            def fin():
                for half, (o_, Bo_) in enumerate(((oA, BoA), (oB, BoB))):
                    s_ = 2 * m + half
                    rs_, Brs_ = rsr[cnt["fin"] % len(rsr)]
                    on_, Bon_ = onr[cnt["fin"] % len(onr)]
                    cnt["fin"] += 1
                    recip(rs_[:, 0:1], o_[:, 128:129], (Bo_,), (Brs_,))
                    ts("dve", on_, o_[:, 0:128], rs_[:, 0:1], None, ALU.mult, None, (Bo_, Brs_), (Bon_,))
                    pb, Bp = nextbank()
                    pbb = pb[:, :].bitcast(BF16)
                    tr(pbb[:, 0:128], on_, ident_b, (Bon_, Bcmb), (Bp,))
                    cp("act", yT[:, h, 128 * s_:128 * s_ + 128], pbb[:, 0:128], (Bp,), (ByT,))

            pending_fin.append(fin)

        def flush_fin():
            while pending_fin:
                pending_fin.pop(0)()

        def kv_rings():
            Kr = ring(3, [128, 2048], BF16, "Kc")
            Vr = ring(3, [128, 16, 129], BF16, "Vc")
            for Vc, BVc in Vr:
                mset("pool", Vc[:, :, 128:129], 1.0, (BVc,))
            PTr = ring(5, [128, 256], BF16, "PT")
            onr = ring(4, [128, 128], BF16, "on")
            rsr = ring(4, [128, 8], F32, "rs")
            return Kr, Vr, PTr, onr, rsr

        md = A.mark()
        setbanks(0, 4)
        KiT_sb = A.alloc([128, L], BF16)
        BKi = Buf("KiT_sb")
        NKTOT = 16 + 128 * 16 * NP_
        dma("sp", KiT_sb[:, 0:NKTOT], KiT_d[:, 0:NKTOT], (), (BKi,))
        maskT_sb = A.alloc([128, 16, 256], BF16)
        Bmask = Buf("maskT")
        dma("pool", maskT_sb, maskT_d.rearrange("p (t q) -> p t q", t=16), (), (Bmask,))
        negb_sb = A.alloc([128, 2, 2048], F32)
        Bnegb = Buf("negb")
        dma("sp", negb_sb, negb_d.rearrange("p (a k) -> p a k", a=2), (), (Bnegb,))
        score = A.alloc([128, L], F32)
        Bscore = Buf("score")
        sel = A.alloc([128, L], BF16)
        Bsel = Buf("sel")
        selT = A.alloc([128, 65, 256], BF16)
        BselT = Buf("selT")
        rr_ = ring(4, [128, 512], F32, "relu")
        Qi_p = A.alloc([128, 8, 256], BF16)
        BQi = Buf("Qi_p")
        Qa_p = A.alloc([128, 8, 256], BF16)
        BQa = Buf("Qa_p")
        bst = A.alloc([128, 16], F32)
        Bbst = Buf("bst")
        btab = A.alloc([128, 16], F32)
        bcnt_t = A.alloc([128, 16], F32)
        Bbtab = Buf("btab")
        cntp = A.alloc([128, 8], F32)[:, 0:1]
        Bcntp = Buf("cntp")
        Bsel2 = Buf("sel2")
        Kr, Vr, PTr, onr, rsr = kv_rings()
        acnt = {"o": 0, "kv": 0, "pt": 0, "fin": 0}
        for m in range(NP_):
            NKB = 16 * m + 17
            NK = 16 + 128 * (NKB - 1)
            flush_fin()
            dma("sp", Qi_p, QiT[:, :, 256 * m:256 * m + 256].rearrange("t p q -> p t q"), (), (BQi,))
            dma("sp", Qa_p, QaT[:, :, 256 * m:256 * m + 256].rearrange("h p q -> p h q"), (), (BQa,))
            for half in range(2):
                s_ = 2 * m + half
                NKh = NK - 1024 if half == 0 else NK
                nchunk = (NKh + 511) // 512
                Bscs = [Buf("sc%d" % i) for i in range(nchunk)]
                for b_ in Bscs:
                    b_.writers = list(Bscore.writers)
                    b_.readers = list(Bscore.readers)
                for h in range(16):
                    po = (h % 2) * 64
                    for ch in range(nchunk):
                        k0 = ch * 512
                        kw = min(512, NKh - k0)
                        pb, Bp = nextbank()
                        mm(pb[:, 0:kw], Qi_p[po:po + 64, h // 2, half * 128:(half + 1) * 128],
                           KiT_sb[po:po + 64, k0:k0 + kw], True, True, (BQi, BKi), (Bp,))
                        r_, Br_ = rr_[(h * nchunk + ch) % 4]
                        act(r_[:, 0:kw], pb[:, 0:kw], AF.Relu, (Bp, Bwi), (Br_,), scale=wiabs[:, s_, h:h + 1])
                        ae = "dve"
                        Bsc = Bscs[ch]
                        if h == 0:
                            ts(ae, score[:, k0:k0 + kw], r_[:, 0:kw], wisgn[:, s_, h:h + 1], None, ALU.mult, None,
                               (Br_, Bwi), (Bsc,))
                        else:
                            stt(ae, score[:, k0:k0 + kw], r_[:, 0:kw], wisgn[:, s_, h:h + 1], score[:, k0:k0 + kw],
                                ALU.mult, ALU.add, (Br_, Bwi, Bsc), (Bsc,))
                Bscore.writers = [ev for b_ in Bscs for ev in b_.writers]
                Bscore.readers = [ev for b_ in Bscs for ev in b_.readers]
                if debug and m == 0 and half == 0:
                    dma("sp", dbg["d_score"][:, 0:NKh], score[:, 0:NKh], (Bscore,), ())
                red("dve", bst[:, 0:1], score[:, 0:NKh], ALU.min, (Bscore,), (Bbst,))
                red("dve", bst[:, 1:2], score[:, 0:NKh], ALU.max, (Bscore,), (Bbst,))
                nvar = NKh - (NK - 2048)
                tt("dve", score[:, NK - 2048:NKh], score[:, NK - 2048:NKh], negb_sb[:, half, 0:nvar], ALU.add,
                   (Bscore, Bnegb), (Bscore,))
                lo, hi, mid, inc = (bst[:, i:i + 1] for i in range(4))
                tt("dve", hi, hi, lo, ALU.subtract, (Bbst,), (Bbst,))
                ts("dve", btab[:, 0:NBISECT], smc[:, 36:36 + NBISECT], hi, None, ALU.mult, None, (Bsmc, Bbst), (Bbtab,))
                mset("dve", bcnt_t, 0.0, (Bbtab,))
                for it in range(NBISECT):
                    tt("dve", mid, lo, btab[:, it:it + 1], ALU.add, (Bbst, Bbtab), (Bbst,))
                    ts("dve", sel[:, 0:NKh], score[:, 0:NKh], mid, 0.0, ALU.is_ge, ALU.add, (Bscore, Bbst, Bsel), (Bsel, Bbtab),
                       accum=bcnt_t[:, it:it + 1])
                    stt("dve", inc, bcnt_t[:, it:it + 1], float(TOPK) - 0.5, btab[:, it:it + 1], ALU.is_ge, ALU.mult,
                        (Bbtab,), (Bbst,))
                    tt("dve", lo, lo, inc, ALU.add, (Bbst,), (Bbst,))
                ts("dve", sel[:, 0:NKh], score[:, 0:NKh], lo, None, ALU.is_ge, None, (Bscore, Bbst, Bsel, Bsel2), (Bsel, Bsel2))
                if debug:
                    dma("sp", dbg["d_thr"][s_:s_ + 1, :].rearrange("a p -> p a"), lo, (Bbst,), ())
                if NKh < NK:
                    mset("pool", sel[:, NKh:NK], 0.0, (Bsel, Bsel2))
                pb, Bp = nextbank()
                pbb = pb[:, :].bitcast(BF16)
                tr(pbb[0:16, 0:128], sel[:, 0:16], ident_b, (Bsel, Bcmb), (Bp,))
                cp("act", selT[0:16, 0, half * 128:(half + 1) * 128], pbb[0:16, 0:128], (Bp,), (BselT,))
                for g8 in range((NKB - 1) // 8):
                    pb, Bp = nextbank()
                    pbb = pb[:, :].bitcast(BF16)
                    for i in range(8):
                        kb = 1 + 8 * g8 + i
                        k0 = 16 + 128 * (kb - 1)
                        tr(pbb[:, i * 128:(i + 1) * 128], sel[:, k0:k0 + 128], ident_b, (Bsel, Bcmb), (Bp,))
                    cp("act", selT[:, 1 + 8 * g8:9 + 8 * g8, half * 128:(half + 1) * 128],
                       pbb.rearrange("p (a b) -> p a b", a=8), (Bp,), (BselT,))
            for h in range(8):
                attention(m, h, "a", Qa_p, BQa, KaT, Va, Kr, Vr, PTr, onr, rsr, yaT, ByaT, acnt,
                          selT=selT, BselT=BselT, nxt=((m, h + 1) if h < 7 else None))
        flush_fin()
        if debug:
            dma("pool", dbg["d_ya"][:, 0:8 * NT], yaT.rearrange("p a b -> p (a b)"), (ByaT,), ())
        S.barrier()
        A.release(md)
        if stop_after == "dsa":
            return _finish(nc, S, out_d)

        ybT = A.alloc([128, 8, NT], BF16)
        BybT = Buf("ybT")
        mf = A.mark()
        maskT_sb = A.alloc([128, 16, 256], BF16)
        Bmask = Buf("maskT2")
        dma("pool", maskT_sb, maskT_d.rearrange("p (t q) -> p t q", t=16), (), (Bmask,))
        oh_sb = A.alloc([128, 8, 66], F32)
        Boh = Buf("oh")
        dma("sp", oh_sb, ohsel_d.rearrange("p (s b) -> p s b", s=8), (), (Boh,))
        Lref = A.alloc([128, 8, 8], F32)
        BLref = Buf("Lref")
        tmpL = A.alloc([128, 8, 66], F32)
        BtmpL = Buf("tmpL")
        for s_ in range(NS):
            ohb = mkap(oh_sb, s_ * 66, [128, (0, 8), (1, 66)])
            tt("dve", tmpL, Tall, ohb, ALU.mult, (BT, Boh), (BtmpL,))
            red("dve", Lref[:, s_, :], tmpL, ALU.add, (BtmpL,), (BLref,))
        Qb_p = A.alloc([128, 8, 256], BF16)
        BQb = Buf("Qb_p")
        biasr = [ring(2, [128, 66], F32, "biasA"), ring(2, [128, 66], F32, "biasB")]
        Kr, Vr, PTr, onr, rsr = kv_rings()
        bcnt = {"o": 0, "kv": 0, "pt": 0, "fin": 0}
        it_ = 0
        for m in range(NP_):
            dma("sp", Qb_p, QbT[:, :, 256 * m:256 * m + 256].rearrange("h p q -> p h q"), (), (BQb,))
            for h in range(8):
                bA, BbA = biasr[0][it_ % 2]
                bB, BbB = biasr[1][it_ % 2]
                it_ += 1
                for half, (bt, Bbt) in enumerate(((bA, BbA), (bB, BbB))):
                    s_ = 2 * m + half
                    ts("dve", bt, Lall[:, h, :], Lref[:, s_, h:h + 1], 60.0, ALU.subtract, ALU.min, (BL, BLref), (Bbt,))
                Bbias = Buf("biasjoin")
                Bbias.writers = list(BbA.writers) + list(BbB.writers)
                nx = (m, h + 1) if h < 7 else ((m + 1, 0) if m + 1 < NP_ else None)
                attention(m, h, "b", Qb_p, BQb, KbT, Vb, Kr, Vr, PTr, onr, rsr, ybT, BybT, bcnt,
                          maskT_sb=maskT_sb, Bmask=Bmask, biasAB=(bA, bB), Bbias=Bbias, nxt=nx)
                for ev in Bbias.readers:
                    BbA.readers.append(ev)
                    BbB.readers.append(ev)
        flush_fin()
        if debug:
            dma("pool", dbg["d_yb"][:, 0:8 * NT], ybT.rearrange("p a b -> p (a b)"), (BybT,), ())
        S.barrier()
        A.release(mf)
        setbanks(0, 8)
        if stop_after == "fox":
            return _finish(nc, S, out_d)

        mergedT = A.alloc_top([128, 16, NT], BF16)
        Bmerged = Buf("merged")
        mm_ = A.mark()
        hnO = A.alloc([128, 16, NT], BF16)
        BhnO = Buf("hnO2")
        dma("sp", hnO, HnO[:, :, 0:NT], (), (BhnO,))
        wstage_alloc(3, ("dve", "act"))
        gwr = ring(4, [128, 16, 128], BF16, "gw")
        bwr = ring(4, [128, 8, 128], BF16, "bw")
        sgr = ring(2, [128, TG], F32, "sg")
        m1r = ring(2, [128, TG], F32, "m1")
        for n in range(16):
            Wga, BWga = gwr[(2 * n) % 4]
            Wgb, BWgb = gwr[(2 * n + 1) % 4]
            Wba, BWba = bwr[(2 * n) % 4]
            Wbb, BWbb = bwr[(2 * n + 1) % 4]
            wload(Wga, w_in[:, C_GA + 128 * n:C_GA + 128 * n + 128].rearrange("(c p) n -> p c n", p=128), BWga)
            wload(Wgb, w_in[:, C_GB + 128 * n:C_GB + 128 * n + 128].rearrange("(c p) n -> p c n", p=128), BWgb)
            wload(Wba, w_bra[:, 128 * n:128 * n + 128].rearrange("(c p) n -> p c n", p=128), BWba)
            wload(Wbb, w_brb[:, 128 * n:128 * n + 128].rearrange("(c p) n -> p c n", p=128), BWbb)
            for tg in range(NTG):
                cols = slice(tg * TG, (tg + 1) * TG)
                res = []
                for (Wg, BWg, Wb, BWb, yT_, ByT_) in ((Wga, BWga, Wba, BWba, yaT, ByaT), (Wgb, BWgb, Wbb, BWbb, ybT, BybT)):
                    pg, Bpg = nextbank()
                    for c in range(16):
                        mm(pg[:, 0:TG], Wg[:, c, :], hnO[:, c, cols], c == 0, c == 15, (BWg, BhnO), (Bpg,))
                    pbr, Bpbr = nextbank()
                    for j in range(8):
                        mm(pbr[:, 0:TG], Wb[:, j, :], yT_[:, j, cols], j == 0, j == 7, (BWb, ByT_), (Bpbr,))
                    sg, Bsg = sgr[len(res) % 2]
                    m1, Bm1 = m1r[len(res) % 2]
                    act(sg, pg[:, 0:TG], AF.Sigmoid, (Bpg,), (Bsg,))
                    tt("dve", m1, sg, pbr[:, 0:TG], ALU.mult, (Bsg, Bpbr), (Bm1,))
                    res.append((m1, Bm1))
                tt("pool", mergedT[:, n, cols], res[0][0], res[1][0], ALU.add, (res[0][1], res[1][1]), (Bmerged,))
        S.barrier()
        A.release(persist_mark)
        if stop_after == "mergea":
            return _finish(nc, S, out_d)
        h2 = A.alloc([128, NS, 2048], F32)
        Bh2 = [Buf("h2_%d" % i) for i in range(NS)]
        mo = A.mark()
        for s_ in range(NS):
            dma("sp", h2[:, s_, :], xown[128 * s_:128 * s_ + 128, :], (), (Bh2[s_],))
        wstage_alloc(3, ("dve", "act", "pool"))
        wor = ring(2, [128, 16, 512], BF16, "wo")
        for cg in range(4):
            Wo, BWo = wor[cg % 2]
            wload(Wo, w_out[:, cg * 512:(cg + 1) * 512].rearrange("(c p) n -> p c n", p=128), BWo)
            for s_ in range(NS):
                pb, Bp = nextbank()
                for c in range(16):
                    mm(pb[:, :], mergedT[:, c, 128 * s_:128 * s_ + 128], Wo[:, c, :], c == 0, c == 15, (Bmerged, BWo), (Bp,))
                tt("dve", h2[:, s_, cg * 512:(cg + 1) * 512], h2[:, s_, cg * 512:(cg + 1) * 512], pb[:, :], ALU.add,
                   (Bp, Bh2[s_]), (Bh2[s_],))
        if debug:
            for s_ in range(NS):
                dma("sp", dbg["d_h2"][128 * s_:128 * s_ + 128, :], h2[:, s_, :], (Bh2[s_],), ())
        S.barrier()
        A.release(mo)
        A.limit = A.full
        if stop_after == "merge":
            return _finish(nc, S, out_d)

        tT = A.alloc([128, 16, NT], BF16)
        BtT = Buf("tT")
        gates = A.alloc([128, NS, 16], F32)
        Bgates = Buf("gates")
        mp = A.mark()
        gbc = A.alloc([128, 2048], F32)
        Bg = Buf("gffn")
        dma("sp", gbc, g_ffn.partition_broadcast(128), (), (Bg,))
        Wge = A.alloc([128, 16, 20], F32)
        BWge = Buf("Wge")
        dma("sp", Wge, w_ge.rearrange("(c p) n -> p c n", p=128), (), (BWge,))
        tnr = ring(1, [128, 2048], F32, "tn")
        t32r = ring(1, [128, 16, 128], F32, "t32")
        statr = ring(2, [128, 8], F32, "mst")
        def route_math(rt, R_, s_):
            gmax, ngmax, sume, pgrp = rt[:, 20:21], rt[:, 21:22], rt[:, 22:23], rt[:, 23:24]
            ohg, eg = rt[:, 24:28], rt[:, 28:32]
            red("dve", gmax, rt[:, 0:4], ALU.max, R_, R_)
            ts("dve", ohg, rt[:, 0:4], gmax, None, ALU.is_ge, None, R_, R_)
            ts("dve", ngmax, gmax, -1.0, None, ALU.mult, None, R_, R_)
            mset("dve", sume, 0.0, R_)
            act(eg, rt[:, 0:4], AF.Exp, R_, R_, bias=ngmax, accum=sume)
            recip(pgrp, sume, R_, R_)
            tmp16 = rt[:, 32:48]
            tt("dve", tmp16.rearrange("p (g j) -> p g j", g=4), rt[:, 4:20].rearrange("p (g j) -> p g j", g=4),
               mkap(rt, 24, [128, (1, 4), (0, 4)]), ALU.mult, R_, R_)
            esel = rt[:, 48:52]
            red("dve", esel, mkap(rt, 32, [128, (1, 4), (4, 4)]), ALU.add, R_, R_)
            m1_, mk1, e2_, m2_, mk2 = rt[:, 52:53], rt[:, 53:57], rt[:, 57:61], rt[:, 61:62], rt[:, 0:4]
            red("dve", m1_, esel, ALU.max, R_, R_)
            ts("dve", mk1, esel, m1_, None, ALU.is_ge, None, R_, R_)
            stt("dve", e2_, mk1, -1e30, esel, ALU.mult, ALU.add, R_, R_)
            red("dve", m2_, e2_, ALU.max, R_, R_)
            ts("dve", mk2, e2_, m2_, None, ALU.is_ge, None, R_, R_)
            dd_, ed_, p1_, p2_ = rt[:, 4:5], rt[:, 5:6], rt[:, 6:7], rt[:, 7:8]
            tt("dve", dd_, m2_, m1_, ALU.subtract, R_, R_)
            act(ed_, dd_, AF.Exp, R_, R_)
            ts("dve", p1_, ed_, 1.0, None, ALU.add, None, R_, R_)
            recip(p1_, p1_, R_, R_)
            tt("dve", p2_, ed_, p1_, ALU.mult, R_, R_)
            tt("dve", p1_, p1_, pgrp, ALU.mult, R_, R_)
            tt("dve", p2_, p2_, pgrp, ALU.mult, R_, R_)
            ge_ = rt[:, 8:12]
            ts("dve", ge_, mk1, p1_, None, ALU.mult, None, R_, R_)
            stt("dve", ge_, mk2, p2_, ge_, ALU.mult, ALU.add, R_, R_)
            for g in range(4):
                ts("dve", gates[:, s_, 4 * g:4 * g + 4], ge_, rt[:, 24 + g:25 + g], None, ALU.mult, None, R_, (Bgates,))

        rtr = ring(2, [128, 64], F32, "rt")
        tnr = ring(2, [128, 2048], F32, "tn2") if A.limit - A.off > 24 * 1024 else tnr
        route_q = []
        for s_ in range(NS):
            rt, Brt = rtr[s_ % 2]
            tn, Btn = tnr[s_ % len(tnr)]
            t32, Bt32 = t32r[0]
            st_, Bst_ = statr[s_ % 2]
            mset("dve", st_, 0.0, (Bst_,))
            act(tn, h2[:, s_, :], AF.Square, (Bh2[s_], Bst_), (Btn, Bst_), accum=st_[:, 0:1])
            rstd_from_ss(st_[:, 0:1], D, (Bst_,), (Bst_,))
            stt("dve", tn, h2[:, s_, :], st_[:, 0:1], gbc, ALU.mult, ALU.mult, (Bh2[s_], Bst_, Bg), (Btn,))
            for q4 in range(4):
                pb, Bp = nextbank()
                for i in range(4):
                    c = 4 * q4 + i
                    tr(pb[:, i * 128:(i + 1) * 128], tn[:, c * 128:(c + 1) * 128], ident_f, (Btn, Bcm), (Bp,))
                src = pb[:, :].rearrange("p (a b) -> p a b", a=4)
                cp("act", tT[:, 4 * q4:4 * q4 + 4, 128 * s_:128 * s_ + 128], src, (Bp,), (BtT,))
                cp("dve", t32[:, 4 * q4:4 * q4 + 4, :], src, (Bp,), (Bt32,))
            while route_q:
                route_q.pop(0)()
            pb, Bp = nextbank()
            for c in range(16):
                mm(pb[:, 0:20], t32[:, c, :], Wge[:, c, :], c == 0, c == 15, (Bt32, BWge), (Bp,))
            R_ = (Brt,)
            lg = rt[:, 0:20]
            tt("dve", lg, pb[:, 0:20], smc[:, 16:36], ALU.add, (Bp, Bsmc), R_)
            route_q.append(lambda rt=rt, R_=R_, s_=s_: route_math(rt, R_, s_))
        while route_q:
            route_q.pop(0)()
        if debug:
            dma("sp", dbg["d_gates"][:, 0:NS * 16], gates.rearrange("p a b -> p (a b)"), (Bgates,), ())
        S.barrier()
        A.release(mp)
        wstage_alloc(2, ("pool",))
        wgr = ring(2, [128, 16, 128], BF16, "wg")
        wur = ring(2, [128, 16, 128], BF16, "wu")
        wdr = ring(2, [128, 4, 2048], BF16, "wd")
        actr = ring(2, [128, 4, NT], BF16, "actT")
        sgr = ring(2, [128, TG], F32, "silu")
        wi_ = 0
        for e_ in range(NEXP):
            actT, Bact = actr[e_ % 2]
            Wd, BWd = wdr[e_ % 2]
            for f in range(4):
                wload(Wd[:, f, :], w_down[e_, f * 128:(f + 1) * 128, :], BWd)
            for ft in range(4):
                Wg, BWg = wgr[wi_ % 2]
                Wu, BWu = wur[wi_ % 2]
                wi_ += 1
                wload(Wg, w_gate[e_, :, ft * 128:(ft + 1) * 128].rearrange("(c p) n -> p c n", p=128), BWg)
                wload(Wu, w_up[e_, :, ft * 128:(ft + 1) * 128].rearrange("(c p) n -> p c n", p=128), BWu)
                for tg in range(NTG):
                    cols = slice(tg * TG, (tg + 1) * TG)
                    pg, Bpg = nextbank()
                    for c in range(16):
                        mm(pg[:, 0:TG], Wg[:, c, :], tT[:, c, cols], c == 0, c == 15, (BWg, BtT), (Bpg,))
                    pu, Bpu = nextbank()
                    for c in range(16):
                        mm(pu[:, 0:TG], Wu[:, c, :], tT[:, c, cols], c == 0, c == 15, (BWu, BtT), (Bpu,))
                    sg, Bsg = sgr[tg % 2]
                    act(sg, pg[:, 0:TG], AF.Silu, (Bpg,), (Bsg,))
                    tt("dve", actT[:, ft, cols], sg, pu[:, 0:TG], ALU.mult, (Bsg, Bpu), (Bact,))
            for s_ in range(NS):
                for cg in range(4):
                    pb, Bp = nextbank()
                    for f in range(4):
                        mm(pb[:, :], actT[:, f, 128 * s_:128 * s_ + 128], Wd[:, f, cg * 512:(cg + 1) * 512], f == 0, f == 3,
                           (Bact, BWd), (Bp,))
                    stt("dve", h2[:, s_, cg * 512:(cg + 1) * 512], pb[:, :], gates[:, s_, e_:e_ + 1],
                        h2[:, s_, cg * 512:(cg + 1) * 512], ALU.mult, ALU.add, (Bp, Bgates, Bh2[s_]), (Bh2[s_],))
        S.barrier()
        A.release(mp)
        gbc = A.alloc([128, 2048], F32)
        Bg = Buf("gfinal")
        dma("sp", gbc, g_final.partition_broadcast(128), (), (Bg,))
        outr = ring(2, [128, 2048], F32, "outst")
        statr = ring(2, [128, 8], F32, "fst")
        for s_ in range(NS):
            ot, Bot = outr[s_ % 2]
            st_, Bst_ = statr[s_ % 2]
            mset("dve", st_, 0.0, (Bst_,))
            act(ot, h2[:, s_, :], AF.Square, (Bh2[s_], Bst_), (Bot, Bst_), accum=st_[:, 0:1])
            rstd_from_ss(st_[:, 0:1], D, (Bst_,), (Bst_,))
            stt("dve", ot, h2[:, s_, :], st_[:, 0:1], gbc, ALU.mult, ALU.mult, (Bh2[s_], Bst_, Bg), (Bot,))
            dma("sp", out_d[128 * s_:128 * s_ + 128, :], ot, (Bot,), ())
        S.barrier()
        return _finish(nc, S, out_d)


def _finish(nc, S, out_d):
    with nc.Block() as block:
        S.emit(block)
    return nc


def own_blocks(c):
    out = []
    for m in range(4):
        out += [16*m + c, 16*m + 15 - c]
    return out

def rope_tables(pos, dim):
    half = dim // 2
    inv = (10000.0 ** (-np.arange(half, dtype=np.float32) / half)).astype(np.float32)
    ang = pos.astype(np.float32)[None, :] * inv[:, None]
    cos = np.cos(ang).astype(np.float32); sin = np.sin(ang).astype(np.float32)
    reps = 128 // dim
    cosT = np.concatenate([cos, cos] * reps, axis=0)
    sinT = np.concatenate([-sin, sin] * reps, axis=0)
    return np.ascontiguousarray(cosT), np.ascontiguousarray(sinT)

def _prep(inputs):
    x = np.asarray(inputs['x'], np.float32)[0]
    meta = np.asarray(inputs['meta_tokens'], np.float32)
    xall = np.ascontiguousarray(np.concatenate([meta, x], axis=0))
    posall = np.arange(L)
    cA, sA = rope_tables(posall, 128)
    cI, sI = rope_tables(posall, 64)
    g = lambda k: np.ascontiguousarray(np.asarray(inputs[k], np.float32))
    common = {
        'xall': xall, 'w_in': g('w_in')[0], 'w_kvup': g('w_kv_up')[0], 'w_bra': g('w_branch_a')[0],
        'w_brb': g('w_branch_b')[0], 'w_out': g('w_out')[0],
        'w_ge': np.ascontiguousarray(np.concatenate([g('w_group')[0], g('w_expert')[0]], axis=1)),
        'w_gate': g('w_gate_e')[0], 'w_up': g('w_up_e')[0], 'w_down': g('w_down_e')[0],
        'g_mix': g('g_mix').reshape(1, D), 'g_ffn': g('g_ffn').reshape(1, D), 'g_final': g('g_final').reshape(1, D),
        'cosA_all': cA, 'sinA_all': sA, 'cosI_all': cI, 'sinI_all': sI,
    }
    smallc = np.zeros((128, 64), np.float32)
    smallc[:, 0:4] = g('g_kv')[0].reshape(4, 128).T
    gk = g('g_idx_k')[0]
    p = np.arange(128)
    smallc[:, 4] = gk[p % 64]
    smallc[:, 5] = gk[(p % 64 + 32) % 64]
    smallc[:, 8:16] = g('b_f')[0][None, :]
    smallc[:, 16:20] = g('b_group')[0][None, :]
    smallc[:, 20:36] = g('b_expert')[0][None, :]
    smallc[:, 36:52] = (0.5 ** np.arange(1, 17, dtype=np.float32))[None, :]
    common['smallc'] = smallc
    cm = np.zeros((128, 512), np.float32)
    cm[:, 0:128] = np.eye(128)
    cm[:, 128:256] = np.triu(np.ones((128, 128)))
    cm[:, 256:384] = 1.0
    blk = np.zeros((128, 128)); blk[0:64, 0:64] = 1; blk[64:, 64:] = 1
    cm[:, 384:512] = blk
    common['cmat'] = cm
    maps = []
    tri = (np.arange(128)[:, None] <= np.arange(128)[None, :]).astype(np.float32)
    for c in range(NCORES):
        blks = own_blocks(c)
        rows = np.concatenate([np.arange(128 * b, 128 * b + 128) for b in blks])
        pos = rows + NMETA
        d = dict(common)
        d['xown'] = np.ascontiguousarray(x[rows])
        ca, sa = rope_tables(pos, 128); ci, si = rope_tables(pos, 64)
        sc = np.float32(128 ** -0.5)
        d['cosA_own'] = ca * sc; d['sinA_own'] = sa * sc; d['cosI_own'] = ci; d['sinI_own'] = si
        mT = np.zeros((128, 16, 256), np.float32)
        for t in range(16):
            for half, dg in ((0, c), (1, 15 - c)):
                if t < dg: mT[:, t, half*128:(half+1)*128] = 1.0
                elif t == dg: mT[:, t, half*128:(half+1)*128] = tri
        d['maskT'] = np.ascontiguousarray(mT.reshape(128, 16 * 256))
        nb = np.zeros((128, 2, 16, 128), np.float32)
        for half in range(2):
            m_qk = mT[:, :, half*128:(half+1)*128].transpose(2, 1, 0)
            nb[:, half] = np.where(m_qk > 0, 0.0, -1e30)
        d['negb'] = np.ascontiguousarray(nb.reshape(128, 2 * 2048))
        oh = np.zeros((128, 8, 66), np.float32)
        for s, b in enumerate(blks):
            oh[:, s, 1 + b] = 1.0
        d['ohsel'] = np.ascontiguousarray(oh.reshape(128, 8 * 66))
        maps.append(d)
    return maps

def _assemble(results):
    out = np.zeros((1, SEQ, D), np.float32)
    for c in range(NCORES):
        blks = own_blocks(c)
        o = results[c]['out']
        for s, b in enumerate(blks):
            out[0, 128*b:128*b+128] = o[128*s:128*s+128]
    return out


_PROGRAM = {}


def kernel(**inputs):
    maps = _prep(inputs)
    if "nc" not in _PROGRAM:
        _PROGRAM["nc"] = build_program()
    nc = _PROGRAM["nc"]
    res = run_bass_kernel_spmd(nc, maps, core_ids=list(range(NCORES)))
    return _assemble(res.results)
```
